# Optimizing a Trainium2 kernel written in Bass

```python
import math
import jax, jax.numpy as jnp
from jax import lax
import numpy as np


D_MODEL = 1024
BATCH = 4
SEQ = 4096
DEPTH = 1

PLE_DIM = 256
GDN_HEADS = 4
GDN_DK = 128
GDN_DV = 128
CONV_WIDTH = 4
CHUNK = 64
DIFF_HEADS = 4
DIFF_D = 64
DIFF_DV = 2 * DIFF_D
Q_BLOCK = 128
ROPE_THETA = 500000.0
ROPE_DIM = DIFF_D // 4
N_EXPERTS = 32
TOP_K = 4
D_FF = D_MODEL
SWIGLU_LIMIT = 7.0
SWIGLU_ALPHA = 1.702
MOE_BLOCK = 128
DN_ALPHA = (2 * DEPTH) ** 0.25
DN_BETA = (8 * DEPTH) ** -0.25
LN_EPS = 1e-5
RMS_EPS = 1e-6

GDN_QK_W = GDN_HEADS * GDN_DK
GDN_V_W = GDN_HEADS * GDN_DV
DIFF_QK_W = DIFF_HEADS * 2 * DIFF_D
DIFF_V_W = DIFF_HEADS * DIFF_DV
MIX_W = GDN_V_W + DIFF_V_W
IN_SIZES = (GDN_QK_W, GDN_QK_W, GDN_V_W, GDN_V_W, GDN_HEADS, GDN_HEADS, DIFF_QK_W, DIFF_QK_W, DIFF_V_W)
IN_OFFSETS = tuple(int(o) for o in np.cumsum(IN_SIZES)[:-1])
IN_W = int(sum(IN_SIZES))
CONV_CH = 2 * GDN_QK_W + GDN_V_W

kernel_name = 'hybrid_gdn_diffattn_moe_deepnorm'

F32 = jnp.float32


def layer_norm(x, g, b):
    xf = x.astype(F32)
    mu = jnp.mean(xf, -1, keepdims=True)
    var = jnp.mean(jnp.square(xf - mu), -1, keepdims=True)
    return ((xf - mu) * lax.rsqrt(var + LN_EPS) * g.astype(F32) + b.astype(F32)).astype(x.dtype)


def rms_norm(x, g):
    xf = x.astype(F32)
    return (xf * lax.rsqrt(jnp.mean(xf * xf, -1, keepdims=True) + RMS_EPS) * g.astype(F32)).astype(x.dtype)


def l2_normalize(x):
    xf = x.astype(F32)
    return xf * lax.rsqrt(jnp.sum(xf * xf, -1, keepdims=True) + 1e-6)


def causal_depthwise_conv(x, w):
    c = x.shape[-1]
    return lax.conv_general_dilated(x, w[:, None, :].astype(x.dtype), window_strides=(1,),
                                    padding=[(CONV_WIDTH - 1, 0)],
                                    dimension_numbers=('NWC', 'WIO', 'NWC'),
                                    feature_group_count=c)


def partial_rotary(x, cos, sin):
    xr, xp = x[..., :ROPE_DIM], x[..., ROPE_DIM:]
    x1, x2 = xr[..., :ROPE_DIM // 2], xr[..., ROPE_DIM // 2:]
    rot = jnp.concatenate([-x2, x1], -1)
    return jnp.concatenate([xr * cos + rot * sin, xp], -1)


def gated_delta_rule_chunked(q, k, v, g, beta):
    b_, s_, h_, dk = q.shape
    dv = v.shape[-1]
    n = s_ // CHUNK

    def chunks(t):
        t = jnp.moveaxis(t, 2, 1)
        return t.reshape((b_, h_, n, CHUNK) + t.shape[3:])

    q = chunks(l2_normalize(q) * (dk ** -0.5))
    k = chunks(l2_normalize(k))
    v = chunks(v.astype(F32))
    beta = chunks(beta.astype(F32))
    g = jnp.cumsum(chunks(g.astype(F32)), axis=-1)
    idx = jnp.arange(CHUNK)
    incl = idx[:, None] >= idx[None, :]
    strict = idx[:, None] > idx[None, :]
    decay = jnp.exp(jnp.where(incl, g[..., :, None] - g[..., None, :], -jnp.inf))
    k_beta = k * beta[..., None]
    a_mat = jnp.where(strict, jnp.einsum('bhncd,bhnjd->bhncj', k_beta, k) * decay, 0.0)
    lhs = a_mat + jnp.eye(CHUNK, dtype=F32)
    rhs = jnp.concatenate([v * beta[..., None], k_beta * jnp.exp(g)[..., None]], -1)
    sol = lax.linalg.triangular_solve(lhs, rhs, left_side=True, lower=True, unit_diagonal=True)
    u, w = sol[..., :dv], sol[..., dv:]
    qk = jnp.where(incl, jnp.einsum('bhncd,bhnjd->bhncj', q, k) * decay, 0.0)
    q_dec = q * jnp.exp(g)[..., None]
    k_dec = k * jnp.exp(g[..., -1:] - g)[..., None]
    g_last = jnp.exp(g[..., -1])

    def step(state, xs):
        u_c, w_c, qk_c, qd_c, kd_c, gl_c = xs
        v_new = u_c - jnp.einsum('bhck,bhkv->bhcv', w_c, state)
        o = jnp.einsum('bhck,bhkv->bhcv', qd_c, state) + jnp.einsum('bhcj,bhjv->bhcv', qk_c, v_new)
        state = state * gl_c[..., None, None] + jnp.einsum('bhck,bhcv->bhkv', kd_c, v_new)
        return state, o

    xs = tuple(jnp.moveaxis(t, 2, 0) for t in (u, w, qk, q_dec, k_dec, g_last))
    state0 = jnp.zeros((b_, h_, dk, dv), F32)
    _, o = lax.scan(step, state0, xs)
    o = jnp.moveaxis(o, 0, 2).reshape(b_, h_, s_, dv)
    return jnp.moveaxis(o, 1, 2)


def diff_attention(q, k, v, lam):
    b_, s_, h_, _, d = q.shape
    dv = v.shape[-1]
    nb = s_ // Q_BLOCK
    kh = jnp.transpose(k, (0, 2, 3, 1, 4))
    vh = jnp.transpose(v, (0, 2, 1, 3))
    qb = jnp.transpose(q, (0, 2, 3, 1, 4)).reshape(b_, h_, 2, nb, Q_BLOCK, d)
    qb = jnp.moveaxis(qb, 3, 0)
    kpos = jnp.arange(s_)
    scale = d ** -0.5

    def block(args):
        q_blk, i = args
        sc = jnp.einsum('bhcqd,bhckd->bhcqk', q_blk, kh, preferred_element_type=F32) * scale
        qpos = i * Q_BLOCK + jnp.arange(Q_BLOCK)
        sc = jnp.where(kpos[None, :] <= qpos[:, None], sc, -jnp.inf)
        pr = jax.nn.softmax(sc, axis=-1)
        a = pr[:, :, 0] - lam * pr[:, :, 1]
        return jnp.einsum('bhqk,bhkv->bhqv', a.astype(vh.dtype), vh)

    o = lax.map(block, (qb, jnp.arange(nb)))
    o = jnp.moveaxis(o, 0, 2).reshape(b_, h_, s_, dv)
    return jnp.transpose(o, (0, 2, 1, 3))


def hybrid_mixer(h, cos, sin, w_in, conv_w, a_log, dt_bias, gdn_norm_w,
                 lam_q1, lam_k1, lam_q2, lam_k2, diff_norm_w, w_out, lam_init):
    b_, s_, _ = h.shape
    proj = h @ w_in
    gq, gk, gv, gz, gb, ga, dq, dk, dv = jnp.split(proj, IN_OFFSETS, axis=-1)
    qkv = jax.nn.silu(causal_depthwise_conv(jnp.concatenate([gq, gk, gv], -1), conv_w))
    gq, gk, gv = jnp.split(qkv, [GDN_QK_W, 2 * GDN_QK_W], axis=-1)
    beta = jax.nn.sigmoid(gb.astype(F32))
    log_decay = -jnp.exp(a_log.astype(F32)) * jax.nn.softplus(ga.astype(F32) + dt_bias.astype(F32))
    o_gdn = gated_delta_rule_chunked(gq.reshape(b_, s_, GDN_HEADS, GDN_DK),
                                     gk.reshape(b_, s_, GDN_HEADS, GDN_DK),
                                     gv.reshape(b_, s_, GDN_HEADS, GDN_DV), log_decay, beta)
    o_gdn = rms_norm(o_gdn, gdn_norm_w) * jax.nn.silu(gz.reshape(b_, s_, GDN_HEADS, GDN_DV).astype(F32))
    o_gdn = o_gdn.reshape(b_, s_, GDN_V_W).astype(h.dtype)
    dq = partial_rotary(dq.reshape(b_, s_, 2 * DIFF_HEADS, DIFF_D), cos, sin).reshape(b_, s_, DIFF_HEADS, 2, DIFF_D)
    dk = partial_rotary(dk.reshape(b_, s_, 2 * DIFF_HEADS, DIFF_D), cos, sin).reshape(b_, s_, DIFF_HEADS, 2, DIFF_D)
    lam = (jnp.exp(jnp.sum(lam_q1.astype(F32) * lam_k1.astype(F32)))
           - jnp.exp(jnp.sum(lam_q2.astype(F32) * lam_k2.astype(F32))) + lam_init)
    o_diff = diff_attention(dq, dk, dv.reshape(b_, s_, DIFF_HEADS, DIFF_DV), lam)
    o_diff = (rms_norm(o_diff, diff_norm_w) * (1.0 - lam_init)).reshape(b_, s_, DIFF_V_W)
    return jnp.concatenate([o_gdn, o_diff.astype(h.dtype)], -1) @ w_out


def moe_ffn(x2d, router_w, router_b, w_gu, b_gu, w_down, b_down):
    n_tok = x2d.shape[0]
    nk = n_tok * TOP_K
    m_pad = nk + N_EXPERTS * MOE_BLOCK
    n_blocks = m_pad // MOE_BLOCK
    logits = (x2d @ router_w + router_b).astype(F32)
    top_vals, top_idx = lax.top_k(logits, TOP_K)
    gates = jax.nn.softmax(top_vals, axis=-1)
    flat_e = top_idx.reshape(-1)
    flat_tok = jnp.repeat(jnp.arange(n_tok, dtype=jnp.int32), TOP_K)
    flat_gate = gates.reshape(-1)
    order = jnp.argsort(flat_e)
    sorted_e = flat_e[order]
    counts = jnp.bincount(flat_e, length=N_EXPERTS)
    padded = ((counts + MOE_BLOCK - 1) // MOE_BLOCK) * MOE_BLOCK
    pad_end = jnp.cumsum(padded)
    pad_start = pad_end - padded
    grp_start = jnp.cumsum(counts) - counts
    rank = jnp.arange(nk) - grp_start[sorted_e]
    dest = pad_start[sorted_e] + rank
    slot_tok = jnp.zeros((m_pad,), jnp.int32).at[dest].set(flat_tok[order])
    slot_gate = jnp.zeros((m_pad,), F32).at[dest].set(flat_gate[order])
    block_start = jnp.arange(n_blocks) * MOE_BLOCK
    block_expert = jnp.clip(jnp.searchsorted(pad_end, block_start, side='right'), 0, N_EXPERTS - 1)
    xs = x2d[slot_tok].reshape(n_blocks, MOE_BLOCK, x2d.shape[-1])

    def expert_block(args):
        xb, e = args
        hgu = xb @ w_gu[e] + b_gu[e]
        gate = jnp.minimum(hgu[:, :D_FF], SWIGLU_LIMIT)
        up = jnp.clip(hgu[:, D_FF:], -SWIGLU_LIMIT, SWIGLU_LIMIT)
        act = (up + 1.0) * gate * jax.nn.sigmoid(SWIGLU_ALPHA * gate)
        return act @ w_down[e] + b_down[e]

    ys = lax.map(expert_block, (xs, block_expert)).reshape(m_pad, -1)
    contrib = (ys.astype(F32) * slot_gate[:, None]).astype(x2d.dtype)
    return jnp.zeros_like(x2d).at[slot_tok].add(contrib)


def setup_inputs(seed: int = 0) -> dict:
    key = jax.random.key(seed)
    ks = jax.random.split(key, 32)
    L = DEPTH
    nrm = lambda k, shape, s: jax.random.normal(k, shape, F32) * s
    x = jax.random.normal(ks[0], (BATCH, SEQ, D_MODEL), F32)
    p = jax.random.normal(ks[1], (DEPTH, BATCH, SEQ, PLE_DIM), F32)
    positions = (jax.random.randint(ks[2], (BATCH, 1), 0, 1024) + jnp.arange(SEQ)[None, :]).astype(jnp.int32)
    col_scale = np.ones((IN_W,), np.float32)
    col_scale[IN_OFFSETS[1]:IN_OFFSETS[2]] = DN_BETA
    col_scale[IN_OFFSETS[7]:] = DN_BETA
    w_in = nrm(ks[3], (L, D_MODEL, IN_W), D_MODEL ** -0.5) * jnp.asarray(col_scale)
    conv_w = nrm(ks[4], (L, CONV_WIDTH, CONV_CH), CONV_WIDTH ** -0.5)
    a_log = jnp.log(jax.random.uniform(ks[5], (L, GDN_HEADS), F32, 1.0, 16.0))
    dt = jnp.exp(jax.random.uniform(ks[6], (L, GDN_HEADS), F32, math.log(1e-3), math.log(1e-1)))
    dt_bias = dt + jnp.log(-jnp.expm1(-dt))
    gdn_norm_w = 1.0 + nrm(ks[7], (L, GDN_DV), 0.02)
    lam_q1 = nrm(ks[8], (L, DIFF_D), 0.1)
    lam_k1 = nrm(ks[9], (L, DIFF_D), 0.1)
    lam_q2 = nrm(ks[10], (L, DIFF_D), 0.1)
    lam_k2 = nrm(ks[11], (L, DIFF_D), 0.1)
    diff_norm_w = 1.0 + nrm(ks[12], (L, DIFF_DV), 0.02)
    w_out = nrm(ks[13], (L, MIX_W, D_MODEL), MIX_W ** -0.5 * DN_BETA)
    ln1_g = 1.0 + nrm(ks[14], (L, D_MODEL), 0.02)
    ln1_b = nrm(ks[15], (L, D_MODEL), 0.02)
    router_w = nrm(ks[16], (L, D_MODEL, N_EXPERTS), D_MODEL ** -0.5)
    router_b = nrm(ks[17], (L, N_EXPERTS), 0.01)
    w_gu = nrm(ks[18], (L, N_EXPERTS, D_MODEL, 2 * D_FF), D_MODEL ** -0.5)
    b_gu = nrm(ks[19], (L, N_EXPERTS, 2 * D_FF), 0.01)
    w_down = nrm(ks[20], (L, N_EXPERTS, D_FF, D_MODEL), D_FF ** -0.5 * DN_BETA)
    b_down = nrm(ks[21], (L, N_EXPERTS, D_MODEL), 0.01)
    ln2_g = 1.0 + nrm(ks[22], (L, D_MODEL), 0.02)
    ln2_b = nrm(ks[23], (L, D_MODEL), 0.02)
    ple_w = nrm(ks[24], (L, PLE_DIM, D_MODEL), PLE_DIM ** -0.5 * DN_BETA)
    ple_gate_w = nrm(ks[25], (L, D_MODEL, D_MODEL), D_MODEL ** -0.5)
    ple_gate_b = nrm(ks[26], (L, D_MODEL), 0.01)
    ln3_g = 1.0 + nrm(ks[27], (L, D_MODEL), 0.02)
    ln3_b = nrm(ks[28], (L, D_MODEL), 0.02)
    return {'x': x, 'p': p, 'positions': positions, 'w_in': w_in, 'conv_w': conv_w,
            'a_log': a_log, 'dt_bias': dt_bias, 'gdn_norm_w': gdn_norm_w,
            'lam_q1': lam_q1, 'lam_k1': lam_k1, 'lam_q2': lam_q2, 'lam_k2': lam_k2,
            'diff_norm_w': diff_norm_w, 'w_out': w_out, 'ln1_g': ln1_g, 'ln1_b': ln1_b,
            'router_w': router_w, 'router_b': router_b, 'w_gu': w_gu, 'b_gu': b_gu,
            'w_down': w_down, 'b_down': b_down, 'ln2_g': ln2_g, 'ln2_b': ln2_b,
            'ple_w': ple_w, 'ple_gate_w': ple_gate_w, 'ple_gate_b': ple_gate_b,
            'ln3_g': ln3_g, 'ln3_b': ln3_b}


def reference(x, p, positions, w_in, conv_w, a_log, dt_bias, gdn_norm_w,
              lam_q1, lam_k1, lam_q2, lam_k2, diff_norm_w, w_out, ln1_g, ln1_b,
              router_w, router_b, w_gu, b_gu, w_down, b_down, ln2_g, ln2_b,
              ple_w, ple_gate_w, ple_gate_b, ln3_g, ln3_b):
    b_, s_, d_ = x.shape
    inv_freq = ROPE_THETA ** (-jnp.arange(0, ROPE_DIM, 2, dtype=F32) / ROPE_DIM)
    ang = positions.astype(F32)[..., None] * inv_freq
    ang = jnp.concatenate([ang, ang], -1)[:, :, None, :]
    cos = jnp.cos(ang).astype(x.dtype)
    sin = jnp.sin(ang).astype(x.dtype)
    h = x
    for i in range(DEPTH):
        lam_init = 0.8 - 0.6 * math.exp(-0.3 * i)
        mix = hybrid_mixer(h, cos, sin, w_in[i], conv_w[i], a_log[i], dt_bias[i], gdn_norm_w[i],
                           lam_q1[i], lam_k1[i], lam_q2[i], lam_k2[i], diff_norm_w[i], w_out[i], lam_init)
        h = layer_norm(DN_ALPHA * h + mix, ln1_g[i], ln1_b[i])
        ffn = moe_ffn(h.reshape(b_ * s_, d_), router_w[i], router_b[i], w_gu[i], b_gu[i],
                      w_down[i], b_down[i]).reshape(b_, s_, d_)
        h = layer_norm(DN_ALPHA * h + ffn, ln2_g[i], ln2_b[i])
        gate = jax.nn.sigmoid(h @ ple_gate_w[i] + ple_gate_b[i])
        ple = gate * (p[i].astype(h.dtype) @ ple_w[i])
        h = layer_norm(DN_ALPHA * h + ple, ln3_g[i], ln3_b[i])
    return h
```

```python
import contextlib
import math
import numpy as np
import concourse.bass as bass
import concourse.mybir as mybir
from concourse.bass_utils import run_bass_kernel_spmd

F32 = mybir.dt.float32
BF16 = mybir.dt.bfloat16
I32 = mybir.dt.int32
AF = mybir.ActivationFunctionType
ALU = mybir.AluOpType
AX = mybir.AxisListType

SEQ = 4096
NBLK = 8
DN_ALPHA = 2.0 ** 0.25
LAM_INIT = 0.2
NCONST = 1032
BIG = 30000.0


class Sched:
    STREAMS = ("pe", "act", "dve", "pool", "sp")

    def __init__(self, nc):
        self.nc = nc
        self.ops = {s: [] for s in self.STREAMS}
        self.last_write = {}
        self.readers = {}
        self.dma_slots = {}
        self.sync_same = {"act", "dve", "pool"}

    def _deps_for(self, reads, writes):
        deps = []
        for k in reads:
            t = self.last_write.get(k)
            if t is not None:
                deps.append(t)
        for k in writes:
            t = self.last_write.get(k)
            if t is not None:
                deps.append(t)
            deps.extend(self.readers.get(k, ()))
        return [(d[0], d[1], self.dma_slots[d[1]]) if d[0] == "dma" else d for d in deps]

    def _commit(self, tok, reads, writes):
        for k in reads:
            self.readers.setdefault(k, []).append(tok)
        for k in writes:
            self.last_write[k] = tok
            self.readers[k] = []

    enabled = True

    def fence(self, fn):
        en = self.enabled
        self.enabled = True
        self.op("dve", fn, r=(), w=("__fence__",), _nofence=True)
        self.enabled = en

    def op(self, stream, fn, r=(), w=(), _nofence=False):
        if not self.enabled:
            return
        if not _nofence:
            r = tuple(r) + ("__fence__",)
        w = tuple(w) + tuple(k for k in r if len(k) == 2 and k[0] == "P" and k[1].isdigit())
        deps = self._deps_for(r, w)
        idx = len(self.ops[stream])
        self.ops[stream].append(dict(fn=fn, deps=deps, dma=None))
        self._commit(("eng", stream, idx), r, w)

    def dma(self, stream, fn, slot, r=(), w=()):
        if not self.enabled:
            return
        r = tuple(r) + ("__fence__",)
        deps = self._deps_for(r, w)
        n = self.dma_slots.get(slot, 0) + 1
        self.dma_slots[slot] = n
        self.ops[stream].append(dict(fn=fn, deps=deps, dma=(slot, n)))
        self._commit(("dma", slot, n), r, w)

    def emit(self, final_wait_slots=()):
        nc = self.nc
        milestone = {s: set() for s in self.STREAMS}
        for s in self.STREAMS:
            for o in self.ops[s]:
                for d in o["deps"]:
                    if d[0] == "eng":
                        if d[1] == s and s not in self.sync_same:
                            continue
                        milestone[d[1]].add(d[2])
        mcount = {}
        for s in self.STREAMS:
            c = 0
            arr = []
            for i in range(len(self.ops[s])):
                if i in milestone[s]:
                    c += 1
                arr.append(c)
            mcount[s] = arr
        with contextlib.ExitStack() as es:
            esem = {s: es.enter_context(nc.semaphore("s_" + s)) for s in self.STREAMS}
            dsem = {sl: es.enter_context(nc.semaphore("d_" + str(sl))) for sl in self.dma_slots}
            block = es.enter_context(nc.Block())

            def run_stream(s, eng):
                waited = {}
                for i, o in enumerate(self.ops[s]):
                    need = {}
                    for d in o["deps"]:
                        if d[0] == "eng":
                            if d[1] == s and s not in self.sync_same:
                                continue
                            key = ("e", d[1])
                            val = mcount[d[1]][d[2]]
                        else:
                            key = ("d", d[1])
                            val = 16 * d[2]
                        if val > need.get(key, 0):
                            need[key] = val
                    for key, val in need.items():
                        if waited.get(key, 0) >= val:
                            continue
                        waited[key] = val
                        sem = esem[key[1]] if key[0] == "e" else dsem[key[1]]
                        eng.wait_ge(sem, val)
                    ins = o["fn"](eng)
                    if o["dma"] is not None:
                        ins.then_inc(dsem[o["dma"][0]], 16)
                    elif i in milestone[s]:
                        ins.then_inc(esem[s], 1)
                if s == "sp":
                    for sl in final_wait_slots:
                        eng.wait_ge(dsem[sl], 16 * self.dma_slots[sl])

            block.sync(lambda e: run_stream("sp", e))
            block.tensor(lambda e: run_stream("pe", e))
            block.scalar(lambda e: run_stream("act", e))
            block.vector(lambda e: run_stream("dve", e))
            block.gpsimd(lambda e: run_stream("pool", e))


def build_program(debug=False, phases="12CDE", nblk=NBLK, stop=None):
    nc = bass.Bass("TRN2", target_bir_lowering=False)
    S = Sched(nc)

    def mark(level):
        if stop is not None and level > stop:
            S.enabled = False

    def din(name, shape, dt=F32):
        return nc.dram_tensor(name, list(shape), dt, kind="ExternalInput").ap()

    xT = din("xT", [8, 128, SEQ])
    posb = din("posb", [128, SEQ], I32)
    wfm_d = din("wfm", [8, 128, 2560])
    wtm_d = din("wtm", [8, 128, 1032])
    convw_d = din("convw", [128, 48])
    hs_d = din("hs", [128, 8])
    gnw_d = din("gnw4", [128, 512])
    dnw_d = din("dnw4", [128, 512])
    lamv_d = din("lamv", [128, 256])
    wout_d = din("wout", [8, 128, 1024])
    const_d = din("consts", [128, NCONST])
    msk4_d = din("msk4", [128, 1024])
    m01_d = din("m01", [128, 2])
    xown_d = din("xown", [16, 128, 1024])
    ptown_d = din("ptown", [2, 128, 2048])
    lnp_d = din("lnp", [6, 128, 1024])
    rw_d = din("rw", [8, 128, 32])
    rbb_d = din("rbb", [128, 32])
    wgu_d = din("wgu", [32, 8, 128, 2048]) if "D" in phases else None
    bgu_d = din("bgu", [128, 512])
    wdn_d = din("wdn", [32, 8, 128, 1024]) if "D" in phases else None
    bdn_d = din("bdn", [128, 1024])
    wpg_d = din("wpg", [8, 128, 1024])
    pgb_d = din("pgb", [128, 1024])
    plew_d = din("plew", [2, 128, 1024])
    out_d = nc.dram_tensor("out", [16, 128, 1024], F32, kind="ExternalOutput").ap()
    ogs_d = nc.dram_tensor("ogs", [32, 128, 512], BF16, kind="Internal").ap()
    mixs_d = nc.dram_tensor("mixs", [32, 128, 1024], F32, kind="Internal").ap()
    dbg = {}
    if debug:
        dbg["mix"] = nc.dram_tensor("dbg_mix", [32, 128, 1024], F32, kind="ExternalOutput").ap()
        dbg["og"] = nc.dram_tensor("dbg_og", [32, 128, 512], F32, kind="ExternalOutput").ap()
        dbg["h1"] = nc.dram_tensor("dbg_h1", [16, 128, 1024], F32, kind="ExternalOutput").ap()
        dbg["acc"] = nc.dram_tensor("dbg_acc", [16, 128, 1024], F32, kind="ExternalOutput").ap()

    with contextlib.ExitStack() as top:
        P = [top.enter_context(nc.psum_tensor("P%d" % i, [128, 512], F32)) for i in range(8)]

        def pk(i):
            return "P%d" % i

        def sbt(es, name, shape, dt=F32):
            return es.enter_context(nc.sbuf_tensor("sb_" + name, list(shape), dt))

        C = sbt(top, "C", [128, NCONST])
        ident = C[:, 0:128]
        tri = C[:, 128:256]
        blk = C[:, 256:384]
        mstrict = C[:, 384:512]
        minclT = C[:, 512:640]
        ind = C[:, 1024:1026]
        freq = C[:, 1026:1027]
        c0 = C[:, 1027:1028]
        c1 = C[:, 1028:1029]
        fz = sbt(top, "fz", [128, 8])

        def fence():
            S.fence(lambda e: e.memset(fz[:], 0.0))
        Cb = sbt(top, "Cb", [128, 768], BF16)
        ident_b = Cb[:, 0:128]
        causal_b = Cb[:, 128:256]
        ones_b = Cb[:, 256:384]
        perm_b = Cb[:, 384:512]
        tri_b = Cb[:, 512:640]
        blk_b = Cb[:, 640:768]
        S.dma("sp", lambda e: e.dma_start(out=C[:], in_=const_d[:, :]), "c0", w=["C"])
        S.op("dve", lambda e: e.tensor_copy(out=Cb[:, 0:128], in_=C[:, 0:128]), r=["C"], w=["Cb"])
        S.op("dve", lambda e: e.tensor_copy(out=Cb[:, 128:384], in_=C[:, 640:896]), r=["C"], w=["Cb"])
        S.op("dve", lambda e: e.tensor_copy(out=Cb[:, 384:512], in_=C[:, 896:1024]), r=["C"], w=["Cb"])
        S.op("dve", lambda e: e.tensor_copy(out=Cb[:, 512:768], in_=C[:, 128:384]), r=["C"], w=["Cb"])

        S.enabled = "1" in phases
        with contextlib.ExitStack() as es:
            wg1 = sbt(es, "wg1", [128, 8, 1536], BF16)
            wt1 = sbt(es, "wt1", [128, 8, 520], BF16)
            xTb = sbt(es, "xTb", [128, 8, 512], BF16)
            cin = sbt(es, "cin", [128, 12, 515], BF16)
            cw = sbt(es, "cw", [128, 48])
            hsb = sbt(es, "hsb", [128, 8])
            nega = sbt(es, "nega", [128, 4])
            gnw = sbt(es, "gnw", [128, 512])
            ycv = sbt(es, "ycv", [128, 512])
            gfm = sbt(es, "gfm", [128, 12, 512], BF16)
            zs = sbt(es, "zs", [128, 4, 512], BF16)
            beta = sbt(es, "beta", [128, 4, 4])
            ldt = sbt(es, "ldt", [128, 4, 4])
            sm = sbt(es, "sm", [128, 64])
            sq = sbt(es, "sq", [128, 8, 128], BF16)
            tA = sbt(es, "tA", [128, 512])
            tB = sbt(es, "tB", [128, 512])
            qn = sbt(es, "qn", [128, 4, 128], BF16)
            kn = sbt(es, "kn", [128, 4, 128], BF16)
            Rr = sbt(es, "Rr", [128, 4, 256])
            Rb = sbt(es, "Rb", [128, 4, 256], BF16)
            kd0 = sbt(es, "kd0", [128, 4, 128], BF16)
            kd1 = sbt(es, "kd1", [128, 4, 128], BF16)
            ldB = sbt(es, "ldB", [128, 8, 128], BF16)
            ldhl = sbt(es, "ldhl", [128, 8], BF16)
            msk4 = sbt(es, "msk4", [128, 1024])
            mstrict4 = msk4[:, 0:512]
            minclT4 = msk4[:, 512:1024]
            S.dma("sp", lambda e: e.dma_start(out=msk4[:], in_=msk4_d[:, :]), "c1", w=["msk4"])
            Ga = sbt(es, "Ga", [128, 4, 128])
            Gb = sbt(es, "Gb", [128, 4, 128])
            egr = sbt(es, "egr", [128, 4, 128])
            Em = sbt(es, "Em", [128, 4, 128])
            ETm = sbt(es, "ETm", [128, 4, 128])
            Nb = sbt(es, "Nb", [128, 4, 128], BF16)
            Mb = sbt(es, "Mb", [128, 4, 128], BF16)
            qkT = sbt(es, "qkT", [128, 4, 128], BF16)
            wb = sbt(es, "wb", [128, 4, 128], BF16)
            wT = sbt(es, "wT", [128, 4, 128], BF16)
            qd = sbt(es, "qd", [128, 4, 128], BF16)
            St = sbt(es, "St", [128, 4, 128])
            Sb = sbt(es, "Sb", [128, 4, 128], BF16)
            vnb = sbt(es, "vnb", [128, 4, 128], BF16)
            og = sbt(es, "og", [128, 4, 128])
            ogb = sbt(es, "ogb", [128, 512], BF16)
            PT = P[1][:].bitcast(BF16)

            for k in range(8):
                S.dma("pool", (lambda k: lambda e: e.dma_start(out=wg1[:, k, :], in_=wfm_d[k, :, 0:1536]))(k), "w1", w=["wg1"])
                S.dma("pool", (lambda k: lambda e: e.dma_start(out=wt1[:, k, 0:512], in_=wtm_d[k, :, 0:512]))(k), "w1", w=["wt1"])
                S.dma("pool", (lambda k: lambda e: e.dma_start(out=wt1[:, k, 512:520], in_=wtm_d[k, :, 1024:1032]))(k), "w1", w=["wt1"])
            S.dma("sp", lambda e: e.dma_start(out=cw[:], in_=convw_d[:, :]), "c1", w=["cw"])
            S.dma("sp", lambda e: e.dma_start(out=hsb[:], in_=hs_d[:, :]), "c1", w=["hsb"])
            S.dma("sp", lambda e: e.dma_start(out=gnw[:], in_=gnw_d[:, :]), "c1", w=["gnw"])
            S.op("act", lambda e: e.activation(out=nega[:], in_=hsb[:, 0:4], func=AF.Exp), r=["hsb"], w=["nega"])
            S.op("dve", lambda e: e.tensor_scalar(out=nega[:], in0=nega[:], scalar1=-1.0, scalar2=None, op0=ALU.mult), r=["nega"], w=["nega"])
            S.op("dve", lambda e: e.memset(cin[:].rearrange("p a b -> p (a b)"), 0.0), w=["cin"] + ["cin%d" % i for i in range(12)])
            S.op("dve", lambda e: e.memset(St[:].rearrange("p a b -> p (a b)"), 0.0), w=["St"])
            S.op("dve", lambda e: e.memset(Sb[:].rearrange("p a b -> p (a b)"), 0.0), w=["Sb"])
            S.op("dve", lambda e: e.memset(vnb[:].rearrange("p a b -> p (a b)"), 0.0), w=["vnb"])

            for b in range(nblk):
                t0 = b * 512
                mark(2)
                S.dma("pool", (lambda t0: lambda e: e.dma_start(out=xTb[:], in_=xT[:, :, t0:t0 + 512].rearrange("k p t -> p k t")))(t0), "x1", w=["xTb"])
                for ch in range(12):
                    def mm(e, ch=ch):
                        for k in range(8):
                            ins = e.matmul(P[0][:], lhsT=wg1[:, k, ch * 128:(ch + 1) * 128], rhs=xTb[:, k, :], start=(k == 0), stop=(k == 7))
                        return ins
                    S.op("pe", mm, r=["wg1", "xTb"], w=[pk(0)])
                    S.op("act", (lambda ch: lambda e: e.activation(out=cin[:, ch, 3:515], in_=P[0][:], func=AF.Copy))(ch), r=[pk(0)], w=["cin%d" % ch, "cin"])
                    S.op("dve", (lambda ch: lambda e: e.tensor_scalar(out=ycv[:], in0=cin[:, ch, 0:512], scalar1=cw[:, ch * 4:ch * 4 + 1], scalar2=None, op0=ALU.mult))(ch),
                         r=["cin%d" % ch, "cw"], w=["ycv"])
                    for wi in range(1, 4):
                        S.op("dve", (lambda ch, wi: lambda e: e.scalar_tensor_tensor(out=ycv[:], in0=cin[:, ch, wi:wi + 512], scalar=cw[:, ch * 4 + wi:ch * 4 + wi + 1],
                                                                                     in1=ycv[:], op0=ALU.mult, op1=ALU.add))(ch, wi), r=["cin%d" % ch, "cw", "ycv"], w=["ycv"])
                    S.op("act", (lambda ch: lambda e: e.activation(out=gfm[:, ch, :], in_=ycv[:], func=AF.Silu))(ch), r=["ycv"], w=["gfm"])
                    S.op("pool", (lambda ch: lambda e: e.tensor_copy(out=cin[:, ch, 0:3], in_=cin[:, ch, 512:515]))(ch), r=["cin%d" % ch], w=["cin%d" % ch])
                mark(3)
                for t in range(4):
                    c0_ = t * 128

                    def mmz(e, c0_=c0_):
                        for k in range(8):
                            ins = e.matmul(P[0][:], lhsT=xTb[:, k, c0_:c0_ + 128], rhs=wt1[:, k, 0:512], start=(k == 0), stop=(k == 7))
                        return ins
                    S.op("pe", mmz, r=["wt1", "xTb"], w=[pk(0)])
                    S.op("act", (lambda t: lambda e: e.activation(out=zs[:, t, :], in_=P[0][:], func=AF.Silu))(t), r=[pk(0)], w=["zs"])

                    def mmb(e, c0_=c0_):
                        for k in range(8):
                            ins = e.matmul(P[2][:, 0:8], lhsT=xTb[:, k, c0_:c0_ + 128], rhs=wt1[:, k, 512:520], start=(k == 0), stop=(k == 7))
                        return ins
                    S.op("pe", mmb, r=["wt1", "xTb"], w=[pk(2)])
                    S.op("act", lambda e: e.activation(out=sm[:, 0:4], in_=P[2][:, 0:4], func=AF.Exp, scale=-1.0), r=[pk(2)], w=["sm"])
                    S.op("dve", lambda e: e.tensor_scalar(out=sm[:, 0:4], in0=sm[:, 0:4], scalar1=1.0, scalar2=None, op0=ALU.add), r=["sm"], w=["sm"])
                    S.op("dve", (lambda t: lambda e: e.reciprocal(out=beta[:, t, :], in_=sm[:, 0:4]))(t), r=["sm"], w=["beta"])
                    S.op("dve", lambda e: e.tensor_tensor(out=sm[:, 4:8], in0=P[2][:, 4:8], in1=hsb[:, 4:8], op=ALU.add), r=[pk(2), "hsb"], w=["sm"])
                    S.op("dve", lambda e: e.tensor_scalar(out=sm[:, 8:12], in0=sm[:, 4:8], scalar1=-1.0, scalar2=None, op0=ALU.mult), r=["sm"], w=["sm"])
                    S.op("dve", lambda e: e.tensor_tensor(out=sm[:, 8:12], in0=sm[:, 8:12], in1=sm[:, 4:8], op=ALU.max), r=["sm"], w=["sm"])
                    S.op("act", lambda e: e.activation(out=sm[:, 8:12], in_=sm[:, 8:12], func=AF.Exp, scale=-1.0), r=["sm"], w=["sm"])
                    S.op("act", lambda e: e.activation(out=sm[:, 8:12], in_=sm[:, 8:12], func=AF.Ln, bias=1.0), r=["sm"], w=["sm"])
                    S.op("dve", lambda e: e.tensor_scalar(out=sm[:, 4:8], in0=sm[:, 4:8], scalar1=0.0, scalar2=None, op0=ALU.max), r=["sm"], w=["sm"])
                    S.op("dve", lambda e: e.tensor_tensor(out=sm[:, 4:8], in0=sm[:, 4:8], in1=sm[:, 8:12], op=ALU.add), r=["sm"], w=["sm"])
                    S.op("dve", (lambda t: lambda e: e.tensor_tensor(out=ldt[:, t, :], in0=sm[:, 4:8], in1=nega[:], op=ALU.mult))(t), r=["sm", "nega"], w=["ldt"])

                for t in range(4):
                    cs = slice(t * 128, (t + 1) * 128)
                    bet = beta[:, t, :]
                    ld = ldt[:, t, :]
                    mark(4)
                    S.op("pool", (lambda cs: lambda e: e.tensor_tensor(out=sq[:], in0=gfm[:, 0:8, cs], in1=gfm[:, 0:8, cs], op=ALU.mult))(cs), r=["gfm"], w=["sq"])
                    sqf = sq[:].rearrange("p h d -> p (h d)")
                    S.op("pe", lambda e: e.matmul(P[2][:], lhsT=ones_b, rhs=sqf[:, 0:512], start=True, stop=True), r=["sq", "Cb"], w=[pk(2)])
                    S.op("pe", lambda e: e.matmul(P[3][:], lhsT=ones_b, rhs=sqf[:, 512:1024], start=True, stop=True), r=["sq", "Cb"], w=[pk(3)])
                    S.op("act", lambda e: e.activation(out=tA[:], in_=P[2][:], func=AF.Ln, bias=1e-6), r=[pk(2)], w=["tA"])
                    S.op("act", lambda e: e.activation(out=tA[:], in_=tA[:], func=AF.Exp, scale=-0.5), r=["tA"], w=["tA"])
                    S.op("act", lambda e: e.activation(out=tB[:], in_=P[3][:], func=AF.Ln, bias=1e-6), r=[pk(3)], w=["tB"])
                    S.op("act", lambda e: e.activation(out=tB[:], in_=tB[:], func=AF.Exp, scale=-0.5), r=["tB"], w=["tB"])
                    S.op("dve", (lambda cs: lambda e: e.scalar_tensor_tensor(out=qn[:], in0=gfm[:, 0:4, cs], scalar=float(128 ** -0.5),
                                                                             in1=tA[:].rearrange("p (h d) -> p h d", h=4), op0=ALU.mult, op1=ALU.mult))(cs), r=["gfm", "tA"], w=["qn"])
                    S.op("dve", (lambda cs: lambda e: e.tensor_tensor(out=kn[:], in0=gfm[:, 4:8, cs], in1=tB[:].rearrange("p (h d) -> p h d", h=4), op=ALU.mult))(cs),
                         r=["gfm", "tB"], w=["kn"])

                    mark(4.2)
                    def trkv(e, cs=cs):
                        for h in range(4):
                            e.transpose(PT[:, h * 128:(h + 1) * 128], kn[:, h, :], ident_b)
                        for h in range(4):
                            ins = e.transpose(PT[:, 512 + h * 128:512 + (h + 1) * 128], gfm[:, 8 + h, cs], ident_b)
                        return ins
                    S.op("pe", trkv, r=["kn", "gfm", "Cb"], w=[pk(1)])
                    mark(4.4)
                    S.op("act", (lambda ld: lambda e: e.activation(out=ldhl[:, 0:4], in_=ld, func=AF.Copy))(ld), r=["ldt"], w=["ldhl"])
                    S.op("dve", (lambda ld: lambda e: e.tensor_tensor(out=ldhl[:, 4:8], in0=ld, in1=ldhl[:, 0:4], op=ALU.subtract))(ld), r=["ldt", "ldhl"], w=["ldhl"])

                    def mmg2(e):
                        e.matmul(P[5][:, 0:4], lhsT=tri_b, rhs=ldhl[:, 0:4], start=True, stop=False)
                        e.matmul(P[5][:, 0:4], lhsT=tri_b, rhs=ldhl[:, 4:8], start=False, stop=True)
                        e.matmul(P[5][:, 4:8], lhsT=blk_b, rhs=ldhl[:, 0:4], start=True, stop=False)
                        return e.matmul(P[5][:, 4:8], lhsT=blk_b, rhs=ldhl[:, 4:8], start=False, stop=True)
                    S.op("pe", mmg2, r=["Cb", "ldhl"], w=[pk(5)])
                    S.op("act", lambda e: e.activation(out=sm[:, 16:20], in_=P[5][:, 0:4], func=AF.Copy), r=[pk(5)], w=["sm2"])
                    S.op("act", lambda e: e.activation(out=sm[:, 24:28], in_=P[5][:, 0:4], func=AF.Exp), r=[pk(5)], w=["sm2"])
                    S.op("dve", lambda e: e.tensor_scalar(out=sm[:, 20:24], in0=sm[:, 16:20], scalar1=-1.0, scalar2=None, op0=ALU.mult), r=["sm2"], w=["sm2"])
                    S.op("dve", lambda e: e.tensor_tensor(out=sm[:, 28:32], in0=P[5][:, 4:8], in1=sm[:, 16:20], op=ALU.subtract), r=[pk(5), "sm2"], w=["sm2"])
                    S.op("act", lambda e: e.activation(out=sm[:, 28:32], in_=sm[:, 28:32], func=AF.Exp), r=["sm2"], w=["sm2"])
                    S.op("dve", (lambda bet: lambda e: e.tensor_tensor(out=sm[:, 32:36], in0=sm[:, 24:28], in1=bet, op=ALU.mult))(bet), r=["sm2", "beta"], w=["sm2"])
                    S.op("dve", lambda e: e.tensor_scalar(out=sm[:, 36:40], in0=sm[:, 28:32], scalar1=ind[:, 0:1], scalar2=None, op0=ALU.mult), r=["sm2", "C"], w=["sm2"])
                    S.op("dve", lambda e: e.tensor_scalar(out=sm[:, 40:44], in0=sm[:, 28:32], scalar1=ind[:, 1:2], scalar2=None, op0=ALU.mult), r=["sm2", "C"], w=["sm2"])
                    S.op("dve", (lambda bet: lambda e: e.tensor_scalar(out=sm[:, 44:48], in0=bet, scalar1=-1.0, scalar2=None, op0=ALU.mult))(bet), r=["beta"], w=["sm2"])

                    mark(4.6)

                    def bc(col):
                        return sm[:, col:col + 4].unsqueeze(2).to_broadcast([128, 4, 128])
                    PTk = PT[:, 0:512].rearrange("p (h d) -> p h d", h=4)
                    PTv = PT[:, 512:1024].rearrange("p (h d) -> p h d", h=4)
                    S.op("dve", (lambda bet: lambda e: e.tensor_tensor(out=Rr[:, :, 0:128], in0=PTv, in1=bet.unsqueeze(2).to_broadcast([128, 4, 128]), op=ALU.mult))(bet),
                         r=[pk(1), "beta"], w=["Rr"])
                    S.op("dve", lambda e: e.tensor_tensor(out=Rr[:, :, 128:256], in0=PTk, in1=bc(32), op=ALU.mult), r=[pk(1), "sm2"], w=["Rr"])
                    S.op("dve", lambda e: e.tensor_tensor(out=kd0[:], in0=PTk, in1=bc(36), op=ALU.mult), r=[pk(1), "sm2"], w=["kd0"])
                    S.op("dve", lambda e: e.tensor_tensor(out=kd1[:], in0=PTk, in1=bc(40), op=ALU.mult), r=[pk(1), "sm2"], w=["kd1"])
                    mark(5)
                    S.op("dve", lambda e: e.tensor_copy(out=ldB[:], in_=ldhl[:].unsqueeze(2).to_broadcast([128, 8, 128])), r=["ldhl"], w=["ldB"])

                    def mmG(e):
                        for h in range(4):
                            e.matmul(P[4][:, h * 128:(h + 1) * 128], lhsT=ldB[:, h, :], rhs=tri_b, start=True, stop=False)
                            ins = e.matmul(P[4][:, h * 128:(h + 1) * 128], lhsT=ldB[:, 4 + h, :], rhs=tri_b, start=False, stop=True)
                        return ins
                    S.op("pe", mmG, r=["ldB", "Cb"], w=[pk(4)])
                    P4v = P[4][:].rearrange("p (h d) -> p h d", h=4)
                    mark(5.2)
                    S.op("dve", lambda e: e.tensor_tensor(out=Ga[:], in0=P4v, in1=mstrict4.rearrange("p (h d) -> p h d", h=4), op=ALU.add), r=[pk(4), "msk4"], w=["Ga"])
                    S.op("dve", lambda e: e.tensor_tensor(out=Gb[:], in0=P4v, in1=minclT4.rearrange("p (h d) -> p h d", h=4), op=ALU.subtract), r=[pk(4), "msk4"], w=["Gb"])
                    S.op("act", lambda e: e.activation(out=egr[:], in_=P4v, func=AF.Exp), r=[pk(4)], w=["egr"])
                    mark(5.4)
                    for h in range(4):
                        S.op("act", (lambda h: lambda e: e.activation(out=Em[:, h, :], in_=Ga[:, h, :], func=AF.Exp, scale=-1.0, bias=sm[:, 16 + h:17 + h]))(h),
                             r=["Ga", "sm2"], w=["Em"])
                        S.op("act", (lambda h: lambda e: e.activation(out=ETm[:, h, :], in_=Gb[:, h, :], func=AF.Exp, bias=sm[:, 20 + h:21 + h]))(h),
                             r=["Gb", "sm2"], w=["ETm"])

                    mark(5.6)
                    def mmKK(e):
                        for h in range(4):
                            e.matmul(P[2][:, h * 128:(h + 1) * 128], lhsT=kn[:, h, :], rhs=kn[:, h, :], start=True, stop=True)
                        for h in range(4):
                            ins = e.matmul(P[3][:, h * 128:(h + 1) * 128], lhsT=kn[:, h, :], rhs=qn[:, h, :], start=True, stop=True)
                        return ins
                    S.op("pe", mmKK, r=["kn", "qn"], w=[pk(2), pk(3)])
                    P2v = P[2][:].rearrange("p (h d) -> p h d", h=4)
                    P3v = P[3][:].rearrange("p (h d) -> p h d", h=4)
                    tAv = tA[:].rearrange("p (h d) -> p h d", h=4)
                    S.op("dve", lambda e: e.tensor_tensor(out=tAv, in0=P2v, in1=bc(44), op=ALU.mult), r=[pk(2), "sm2"], w=["tA"])
                    S.op("dve", lambda e: e.tensor_tensor(out=Nb[:], in0=tAv, in1=Em[:], op=ALU.mult), r=["tA", "Em"], w=["Nb"])
                    S.op("dve", lambda e: e.tensor_tensor(out=qkT[:], in0=P3v, in1=ETm[:], op=ALU.mult), r=[pk(3), "ETm"], w=["qkT"])

                    def trN(e):
                        for h in range(4):
                            ins = e.transpose(PT[:, h * 128:(h + 1) * 128], Nb[:, h, :], ident_b)
                        return ins
                    S.op("pe", trN, r=["Nb", "Cb"], w=[pk(1)])
                    S.op("act", lambda e: e.activation(out=Mb[:], in_=PTk, func=AF.Copy), r=[pk(1)], w=["Mb"])
                    mark(6)
                    for p in range(6):
                        S.op("act", lambda e: e.activation(out=Rb[:], in_=Rr[:], func=AF.Copy), r=["Rr"], w=["Rb"])

                        def mmR(e):
                            for h in range(4):
                                ins = e.matmul(P[6 + h // 2][:, (h % 2) * 256:(h % 2) * 256 + 256], lhsT=Mb[:, h, :], rhs=Rb[:, h, :], start=True, stop=True)
                            return ins
                        S.op("pe", mmR, r=["Mb", "Rb"], w=[pk(6), pk(7)])
                        S.op("dve", lambda e: e.tensor_tensor(out=Rr[:, 0:2, :], in0=Rr[:, 0:2, :], in1=P[6][:].rearrange("p (h d) -> p h d", h=2), op=ALU.add),
                             r=["Rr", pk(6)], w=["Rr"])
                        S.op("dve", lambda e: e.tensor_tensor(out=Rr[:, 2:4, :], in0=Rr[:, 2:4, :], in1=P[7][:].rearrange("p (h d) -> p h d", h=2), op=ALU.add),
                             r=["Rr", pk(7)], w=["Rr"])
                        if p < 5:
                            def mmSq(e):
                                for h in range(4):
                                    e.matmul(P[4][:, h * 128:(h + 1) * 128], lhsT=Mb[:, h, :], rhs=Nb[:, h, :], start=True, stop=True)
                                for h in range(4):
                                    ins = e.matmul(P[5][:, h * 128:(h + 1) * 128], lhsT=Nb[:, h, :], rhs=Mb[:, h, :], start=True, stop=True)
                                return ins
                            S.op("pe", mmSq, r=["Mb", "Nb"], w=[pk(4), pk(5)])
                            S.op("act", lambda e: e.activation(out=Nb[:], in_=P4v, func=AF.Copy), r=[pk(4)], w=["Nb"])
                            S.op("dve", lambda e: e.tensor_copy(out=Mb[:], in_=P[5][:].rearrange("p (h d) -> p h d", h=4)), r=[pk(5)], w=["Mb"])
                    mark(7)
                    S.op("act", lambda e: e.activation(out=wb[:], in_=Rr[:, :, 128:256], func=AF.Copy), r=["Rr"], w=["wb"])

                    def trW(e):
                        for h in range(4):
                            ins = e.transpose(PT[:, h * 128:(h + 1) * 128], wb[:, h, :], ident_b)
                        return ins
                    S.op("pe", trW, r=["wb", "Cb"], w=[pk(1)])
                    S.op("act", lambda e: e.activation(out=wT[:], in_=PTk, func=AF.Copy), r=[pk(1)], w=["wT"])
                    S.op("dve", lambda e: e.tensor_tensor(out=qd[:], in0=qn[:], in1=egr[:], op=ALU.mult), r=["qn", "egr"], w=["qd"])
                    for ch in range(2):
                        kd = kd0 if ch == 0 else kd1
                        kdk = "kd0" if ch == 0 else "kd1"
                        last = 63 if ch == 0 else 127

                        def mmV(e):
                            for h in range(4):
                                ins = e.matmul(P[2][:, h * 128:(h + 1) * 128], lhsT=wT[:, h, :], rhs=Sb[:, h, :], start=True, stop=True)
                            return ins
                        S.op("pe", mmV, r=["wT", "Sb"], w=[pk(2)])
                        S.op("dve", lambda e: e.tensor_tensor(out=vnb[:], in0=Rr[:, :, 0:128], in1=P2v, op=ALU.subtract), r=["Rr", pk(2)], w=["vnb"])

                        def mmO(e, kd=kd):
                            for h in range(4):
                                e.matmul(P[3][:, h * 128:(h + 1) * 128], lhsT=qd[:, h, :], rhs=Sb[:, h, :], start=True, stop=False)
                                e.matmul(P[3][:, h * 128:(h + 1) * 128], lhsT=qkT[:, h, :], rhs=vnb[:, h, :], start=False, stop=True)
                            for h in range(4):
                                ins = e.matmul(P[4][:, h * 128:(h + 1) * 128], lhsT=kd[:, h, :], rhs=vnb[:, h, :], start=True, stop=True)
                            return ins
                        S.op("pe", mmO, r=["qd", "Sb", "qkT", "vnb", kdk], w=[pk(3), pk(4)])
                        if ch == 0:
                            S.op("dve", lambda e: e.tensor_scalar(out=og[:], in0=P3v, scalar1=ind[:, 0:1], scalar2=None, op0=ALU.mult), r=[pk(3), "C"], w=["og"])
                        else:
                            S.op("dve", lambda e: e.scalar_tensor_tensor(out=og[:], in0=P3v, scalar=ind[:, 1:2], in1=og[:], op0=ALU.mult, op1=ALU.add),
                                 r=[pk(3), "C", "og"], w=["og"])
                        for h in range(4):
                            S.op("dve", (lambda h, last: lambda e: e.scalar_tensor_tensor(out=St[:, h, :], in0=St[:, h, :], scalar=egr[:, h, last:last + 1],
                                                                                          in1=P[4][:, h * 128:(h + 1) * 128], op0=ALU.mult, op1=ALU.add))(h, last),
                                 r=["St", "egr", pk(4)], w=["St"])
                        S.op("act", lambda e: e.activation(out=Sb[:], in_=St[:], func=AF.Copy), r=["St"], w=["Sb"])
                    mark(8)
                    ogf = og[:].rearrange("p h d -> p (h d)")
                    S.op("pool", lambda e: e.tensor_tensor(out=tB[:], in0=ogf, in1=ogf, op=ALU.mult), r=["og"], w=["tB"])
                    S.op("dve", lambda e: e.tensor_reduce(out=sm[:, 48:52], in_=tB[:].rearrange("p (h d) -> p h d", h=4), axis=AX.X, op=ALU.add), r=["tB"], w=["sm3"])
                    S.op("act", lambda e: e.activation(out=sm[:, 48:52], in_=sm[:, 48:52], func=AF.Ln, scale=1.0 / 128.0, bias=1e-6), r=["sm3"], w=["sm3"])
                    S.op("act", lambda e: e.activation(out=sm[:, 48:52], in_=sm[:, 48:52], func=AF.Exp, scale=-0.5), r=["sm3"], w=["sm3"])
                    S.op("dve", lambda e: e.tensor_tensor(out=tAv, in0=og[:], in1=bc(48), op=ALU.mult), r=["og", "sm3"], w=["tA"])
                    S.op("dve", lambda e: e.tensor_tensor(out=tA[:], in0=tA[:], in1=gnw[:], op=ALU.mult), r=["tA", "gnw"], w=["tA"])
                    S.op("dve", (lambda t: lambda e: e.tensor_tensor(out=ogb[:], in0=tA[:], in1=zs[:, t, :], op=ALU.mult))(t), r=["tA", "zs"], w=["ogb"])
                    ti = b * 4 + t
                    S.dma("sp", (lambda ti: lambda e: e.dma_start(out=ogs_d[ti, :, :], in_=ogb[:]))(ti), "ogw", r=["ogb"], w=["ogs%d" % ti])
                    if debug:
                        S.dma("sp", (lambda ti: lambda e: e.dma_start(out=dbg["og"][ti, :, :], in_=og[:].rearrange("p h d -> p (h d)")))(ti), "dbg", r=["og"])

        fence()
        S.enabled = "2" in phases
        with contextlib.ExitStack() as es:
            wq2 = sbt(es, "wq2", [128, 8, 1024], BF16)
            wv2 = sbt(es, "wv2", [128, 8, 512], BF16)
            wo = sbt(es, "wo", [128, 8, 1024], BF16)
            xT2 = sbt(es, "xTb2", [128, 8, 512], BF16)
            KT = sbt(es, "KT", [128, 4, SEQ], BF16)
            Vs = sbt(es, "Vs", [128, 32, 4, 132], BF16)
            pi_ = sbt(es, "pi_", [128, 512], I32)
            pf = sbt(es, "pf", [128, 512])
            ka = sbt(es, "ka", [128, 512])
            ki = sbt(es, "ki", [128, 512], I32)
            cosF = sbt(es, "cosF", [128, 512])
            sinF = sbt(es, "sinF", [128, 512])
            mb = sbt(es, "mb", [128, 512], BF16)
            r1 = sbt(es, "r1", [128, 512])
            r2 = sbt(es, "r2", [128, 512])
            Q0 = sbt(es, "Q0", [128, 4, 512], BF16)
            Q1 = sbt(es, "Q1", [128, 4, 512], BF16)
            pT = [sbt(es, "pT%d" % i, [128, 512], BF16) for i in range(2)]
            den = sbt(es, "den", [128, 16])
            od = sbt(es, "od", [128, 4, 4, 128])
            ot = sbt(es, "ot", [128, 4, 1024], BF16)
            oT = sbt(es, "oT", [128, 8, 128], BF16)
            mixt = sbt(es, "mixt", [128, 1024])
            dnw = sbt(es, "dnw", [128, 512])
            lamv = sbt(es, "lamv", [128, 256])
            lam = sbt(es, "lam", [128, 8])
            PT2 = P[6][:].bitcast(BF16)

            for k in range(8):
                S.dma("pool", (lambda k: lambda e: e.dma_start(out=wq2[:, k, :], in_=wfm_d[k, :, 1536:2560]))(k), "w2", w=["wq2"])
                S.dma("pool", (lambda k: lambda e: e.dma_start(out=wv2[:, k, :], in_=wtm_d[k, :, 512:1024]))(k), "w2", w=["wv2"])
                S.dma("pool", (lambda k: lambda e: e.dma_start(out=wo[:, k, :], in_=wout_d[k, :, :]))(k), "w2", w=["wo"])
            S.dma("sp", lambda e: e.dma_start(out=dnw[:], in_=dnw_d[:, :]), "c2", w=["dnw"])
            S.dma("sp", lambda e: e.dma_start(out=lamv[:], in_=lamv_d[:, :]), "c2", w=["lamv"])
            S.op("dve", lambda e: e.memset(Vs[:].rearrange("p a b c -> p (a b c)"), 1.0), w=["Vs"])
            S.op("dve", lambda e: e.tensor_tensor(out=lamv[:, 0:64], in0=lamv[:, 0:64], in1=lamv[:, 64:128], op=ALU.mult), r=["lamv"], w=["lamv"])
            S.op("dve", lambda e: e.tensor_tensor(out=lamv[:, 128:192], in0=lamv[:, 128:192], in1=lamv[:, 192:256], op=ALU.mult), r=["lamv"], w=["lamv"])
            S.op("dve", lambda e: e.tensor_reduce(out=lam[:, 0:1], in_=lamv[:, 0:64], axis=AX.X, op=ALU.add), r=["lamv"], w=["lam"])
            S.op("dve", lambda e: e.tensor_reduce(out=lam[:, 1:2], in_=lamv[:, 128:192], axis=AX.X, op=ALU.add), r=["lamv"], w=["lam"])
            S.op("act", lambda e: e.activation(out=lam[:, 0:2], in_=lam[:, 0:2], func=AF.Exp), r=["lam"], w=["lam"])
            S.op("dve", lambda e: e.tensor_tensor(out=lam[:, 2:3], in0=lam[:, 1:2], in1=lam[:, 0:1], op=ALU.subtract), r=["lam"], w=["lam"])
            S.op("dve", lambda e: e.tensor_scalar(out=lam[:, 2:3], in0=lam[:, 2:3], scalar1=-LAM_INIT, scalar2=None, op0=ALU.add), r=["lam"], w=["lam"])

            def range_reduce_sin(dst, src_key):
                S.op("dve", lambda e: e.tensor_scalar(out=ki[:], in0=ka[:], scalar1=float(1.0 / (2 * math.pi)), scalar2=None, op0=ALU.mult), r=["ka"], w=["ki"])
                S.op("dve", lambda e: e.tensor_copy(out=r1[:], in_=ki[:]), r=["ki"], w=["r1"])
                S.op("dve", lambda e: e.scalar_tensor_tensor(out=ka[:], in0=r1[:], scalar=float(-2 * math.pi), in1=ka[:], op0=ALU.mult, op1=ALU.add), r=["r1", "ka"], w=["ka"])
                S.op("dve", lambda e: e.tensor_scalar(out=r1[:], in0=ka[:], scalar1=float(math.pi), scalar2=float(-2 * math.pi), op0=ALU.is_gt, op1=ALU.mult), r=["ka"], w=["r1"])
                S.op("dve", lambda e: e.tensor_tensor(out=ka[:], in0=ka[:], in1=r1[:], op=ALU.add), r=["ka", "r1"], w=["ka"])
                S.op("act", lambda e: e.activation(out=dst[:], in_=ka[:], func=AF.Sin), r=["ka"], w=[src_key])

            for b in range(nblk):
                t0 = b * 512
                S.dma("pool", (lambda t0: lambda e: e.dma_start(out=xT2[:], in_=xT[:, :, t0:t0 + 512].rearrange("k p t -> p k t")))(t0), "x2", w=["xTb2"])
                S.dma("sp", (lambda t0: lambda e: e.dma_start(out=pi_[:], in_=posb[:, t0:t0 + 512]))(t0), "pos", w=["pi_"])
                S.op("dve", lambda e: e.tensor_copy(out=pf[:], in_=pi_[:]), r=["pi_"], w=["pf"])
                S.op("dve", lambda e: e.tensor_scalar(out=pf[:], in0=pf[:], scalar1=freq, scalar2=None, op0=ALU.mult), r=["pf", "C"], w=["pf"])
                S.op("dve", lambda e: e.tensor_copy(out=ka[:], in_=pf[:]), r=["pf"], w=["ka"])
                range_reduce_sin(sinF, "sinF")
                S.op("dve", lambda e: e.tensor_scalar(out=ka[:], in0=pf[:], scalar1=float(math.pi / 2), scalar2=None, op0=ALU.add), r=["pf"], w=["ka"])
                range_reduce_sin(cosF, "cosF")
                for which in range(2):
                    for h in range(4):
                        col = which * 512 + h * 128

                        def mm(e, col=col):
                            for k in range(8):
                                ins = e.matmul(P[0][:], lhsT=wq2[:, k, col:col + 128], rhs=xT2[:, k, :], start=(k == 0), stop=(k == 7))
                            return ins
                        S.op("pe", mm, r=["wq2", "xTb2"], w=[pk(0)])
                        S.op("act", lambda e: e.activation(out=mb[:], in_=P[0][:], func=AF.Copy), r=[pk(0)], w=["mb"])
                        S.op("pe", lambda e: e.matmul(P[1][:], lhsT=perm_b, rhs=mb[:], start=True, stop=True), r=["mb", "Cb"], w=[pk(1)])
                        S.op("dve", lambda e: e.tensor_tensor(out=r1[:], in0=mb[:], in1=cosF[:], op=ALU.mult), r=["mb", "cosF"], w=["r1"])
                        S.op("dve", lambda e: e.tensor_tensor(out=r2[:], in0=P[1][:], in1=sinF[:], op=ALU.mult), r=[pk(1), "sinF"], w=["r2"])
                        if which == 0:
                            S.op("dve", lambda e: e.tensor_tensor(out=r1[:], in0=r1[:], in1=r2[:], op=ALU.add), r=["r1", "r2"], w=["r1"])
                            S.op("dve", (lambda h: lambda e: e.tensor_scalar(out=Q0[:, h, :], in0=r1[:], scalar1=c0, scalar2=None, op0=ALU.mult))(h), r=["r1", "C"], w=["Q0"])
                            S.op("dve", (lambda h: lambda e: e.tensor_scalar(out=Q1[:, h, :], in0=r1[:], scalar1=c1, scalar2=None, op0=ALU.mult))(h), r=["r1", "C"], w=["Q1"])
                        else:
                            S.op("dve", (lambda h, t0: lambda e: e.tensor_tensor(out=KT[:, h, t0:t0 + 512], in0=r1[:], in1=r2[:], op=ALU.add))(h, t0), r=["r1", "r2"], w=["KT"])
                for t in range(4):
                    c0_ = t * 128
                    ti = b * 4 + t

                    def mmv(e, c0_=c0_):
                        for k in range(8):
                            ins = e.matmul(P[0][:], lhsT=xT2[:, k, c0_:c0_ + 128], rhs=wv2[:, k, :], start=(k == 0), stop=(k == 7))
                        return ins
                    S.op("pe", mmv, r=["wv2", "xTb2"], w=[pk(0)])
                    S.op("act", (lambda ti: lambda e: e.activation(out=Vs[:, ti, :, 0:128], in_=P[0][:].rearrange("p (h d) -> p h d", h=4), func=AF.Copy))(ti),
                         r=[pk(0)], w=["Vs"])
                nkt = 4 * b + 4
                it = 0
                for h in range(4):
                    for c in range(2):
                        Qc = Q0 if c == 0 else Q1
                        qk_ = "Q0" if c == 0 else "Q1"
                        accb = (2, 3) if c == 0 else (4, 5)
                        for kt in range(nkt):
                            j = kt - 4 * b
                            q0 = 128 * j if j >= 0 else 0
                            sp_ = it % 2
                            it += 1
                            S.op("pe", (lambda sp_, h, kt, q0, Qc: lambda e: e.matmul(P[sp_][:, q0:512], lhsT=KT[:, h, kt * 128:(kt + 1) * 128], rhs=Qc[:, h, q0:512],
                                                                                      start=True, stop=True))(sp_, h, kt, q0, Qc), r=["KT", qk_], w=[pk(sp_)])
                            S.op("act", (lambda sp_, q0: lambda e: e.activation(out=pT[sp_][:, q0:512], in_=P[sp_][:, q0:512], func=AF.Exp, scale=0.125))(sp_, q0),
                                 r=[pk(sp_)], w=["pT%d" % sp_])
                            if j >= 0:
                                S.op("pool", (lambda sp_, q0: lambda e: e.tensor_tensor(out=pT[sp_][:, q0:q0 + 128], in0=pT[sp_][:, q0:q0 + 128], in1=causal_b, op=ALU.mult))(sp_, q0),
                                     r=["pT%d" % sp_, "Cb"], w=["pT%d" % sp_])

                            def mmpv(e, sp_=sp_, h=h, kt=kt, j=j, accb=accb, b=b):
                                ins = None
                                for qi in range(max(j, 0), 4):
                                    bank = P[accb[qi // 2]]
                                    o0 = (qi % 2) * 256
                                    ins = e.matmul(bank[:, o0:o0 + 129], lhsT=pT[sp_][:, qi * 128:(qi + 1) * 128], rhs=Vs[:, kt, h, 0:129],
                                                   start=(kt == 0 and qi % 2 == 0), stop=(kt == 4 * b + qi), skip_group_check=True)
                                return ins
                            S.op("pe", mmpv, r=["pT%d" % sp_, "Vs"], w=[pk(accb[0]), pk(accb[1])])
                    for qi in range(4):
                        o0 = (qi % 2) * 256
                        bA = P[2 + qi // 2]
                        bB = P[4 + qi // 2]
                        S.op("dve", (lambda qi, bA, o0: lambda e: e.tensor_copy(out=den[:, qi:qi + 1], in_=bA[:, o0 + 128:o0 + 129]))(qi, bA, o0), r=[pk(2 + qi // 2)], w=["den"])
                        S.op("dve", (lambda qi, bB, o0: lambda e: e.tensor_copy(out=den[:, 4 + qi:5 + qi], in_=bB[:, o0 + 128:o0 + 129]))(qi, bB, o0), r=[pk(4 + qi // 2)], w=["den"])
                    S.op("dve", lambda e: e.reciprocal(out=den[:, 8:16], in_=den[:, 0:8]), r=["den"], w=["den"])
                    S.op("dve", lambda e: e.tensor_scalar(out=den[:, 12:16], in0=den[:, 12:16], scalar1=lam[:, 2:3], scalar2=None, op0=ALU.mult), r=["den", "lam"], w=["den"])
                    for qi in range(4):
                        o0 = (qi % 2) * 256
                        bA = P[2 + qi // 2]
                        bB = P[4 + qi // 2]
                        S.op("dve", (lambda qi, bA, o0, h: lambda e: e.tensor_scalar(out=od[:, qi, h, :], in0=bA[:, o0:o0 + 128], scalar1=den[:, 8 + qi:9 + qi], scalar2=None,
                                                                                      op0=ALU.mult))(qi, bA, o0, h), r=[pk(2 + qi // 2), "den"], w=["od"])
                        S.op("dve", (lambda qi, bB, o0, h: lambda e: e.scalar_tensor_tensor(out=od[:, qi, h, :], in0=bB[:, o0:o0 + 128], scalar=den[:, 12 + qi:13 + qi],
                                                                                             in1=od[:, qi, h, :], op0=ALU.mult, op1=ALU.add))(qi, bB, o0, h),
                             r=[pk(4 + qi // 2), "den", "od"], w=["od"])
                for qi in range(4):
                    ti = b * 4 + qi
                    odf = od[:, qi, :, :].rearrange("p h d -> p (h d)")
                    S.op("pool", (lambda odf: lambda e: e.tensor_tensor(out=r1[:], in0=odf, in1=odf, op=ALU.mult))(odf), r=["od"], w=["r1"])
                    S.op("dve", lambda e: e.tensor_reduce(out=lam[:, 4:8], in_=r1[:].rearrange("p (h d) -> p h d", h=4), axis=AX.X, op=ALU.add), r=["r1"], w=["lam2"])
                    S.op("act", lambda e: e.activation(out=lam[:, 4:8], in_=lam[:, 4:8], func=AF.Ln, scale=1.0 / 128.0, bias=1e-6), r=["lam2"], w=["lam2"])
                    S.op("act", lambda e: e.activation(out=lam[:, 4:8], in_=lam[:, 4:8], func=AF.Exp, scale=-0.5), r=["lam2"], w=["lam2"])
                    S.op("dve", (lambda qi: lambda e: e.tensor_tensor(out=r1[:].rearrange("p (h d) -> p h d", h=4), in0=od[:, qi, :, :],
                                                                      in1=lam[:, 4:8].unsqueeze(2).to_broadcast([128, 4, 128]), op=ALU.mult))(qi), r=["od", "lam2"], w=["r1"])
                    S.op("dve", (lambda qi: lambda e: e.scalar_tensor_tensor(out=ot[:, qi, 512:1024], in0=r1[:], scalar=float(1.0 - LAM_INIT), in1=dnw[:],
                                                                             op0=ALU.mult, op1=ALU.mult))(qi), r=["r1", "dnw"], w=["ot%d" % qi])
                    S.dma("sp", (lambda qi, ti: lambda e: e.dma_start(out=ot[:, qi, 0:512], in_=ogs_d[ti, :, :]))(qi, ti), "ogr", r=["ogs%d" % ti], w=["ot%d" % qi])

                    def trO(e, qi=qi):
                        for k in range(8):
                            ins = e.transpose(PT2[:, k * 128:(k + 1) * 128], ot[:, qi, k * 128:(k + 1) * 128], ident_b)
                        return ins
                    S.op("pe", trO, r=["ot%d" % qi, "Cb"], w=[pk(6)])
                    S.op("act", lambda e: e.activation(out=oT[:], in_=PT2[:, :].rearrange("p (k t) -> p k t", k=8), func=AF.Copy), r=[pk(6)], w=["oT"])
                    for half in range(2):
                        def mmo(e, half=half):
                            for k in range(8):
                                ins = e.matmul(P[6 + half][:], lhsT=oT[:, k, :], rhs=wo[:, k, half * 512:(half + 1) * 512], start=(k == 0), stop=(k == 7))
                            return ins
                        S.op("pe", mmo, r=["oT", "wo"], w=[pk(6 + half)])
                        S.op("act", (lambda half: lambda e: e.activation(out=mixt[:, half * 512:(half + 1) * 512], in_=P[6 + half][:], func=AF.Copy))(half),
                             r=[pk(6 + half)], w=["mixt"])
                    S.dma("sp", (lambda ti: lambda e: e.dma_start(out=mixs_d[ti, :, :], in_=mixt[:]))(ti), "mixw", r=["mixt"], w=["mixs%d" % ti])
                    if debug:
                        S.dma("sp", (lambda ti: lambda e: e.dma_start(out=dbg["mix"][ti, :, :], in_=mixt[:]))(ti), "dbg", r=["mixt"])

        fence()
        S.enabled = "C" in phases
        with contextlib.ExitStack() as es:
            acc = sbt(es, "acc", [128, 16, 1024])
            h1T = sbt(es, "h1T", [128, 8, 2048], BF16)
            gates = sbt(es, "gates", [128, 16, 32])
            m01 = sbt(es, "m01", [128, 2])
            rbb = sbt(es, "rbb", [128, 32])
            bgu = sbt(es, "bgu", [128, 512])
            st = sbt(es, "st", [128, 32])
            S.dma("sp", lambda e: e.dma_start(out=m01[:], in_=m01_d[:, :]), "c3", w=["m01"])
            S.dma("sp", lambda e: e.dma_start(out=rbb[:], in_=rbb_d[:, :]), "c3", w=["rbb"])
            S.dma("sp", lambda e: e.dma_start(out=bgu[:], in_=bgu_d[:, :]), "c3", w=["bgu"])

            def layer_norm(src, srck, dst, dstk, gam, bet, gk, scr, scrk):
                S.op("dve", lambda e: e.tensor_reduce(out=st[:, 0:1], in_=src, axis=AX.X, op=ALU.add), r=[srck], w=["st"])
                S.op("act", lambda e: e.activation(out=scr, in_=src, func=AF.Square), r=[srck], w=[scrk])
                S.op("dve", lambda e: e.tensor_reduce(out=st[:, 1:2], in_=scr, axis=AX.X, op=ALU.add), r=[scrk], w=["st"])
                S.op("dve", lambda e: e.tensor_scalar(out=st[:, 0:2], in0=st[:, 0:2], scalar1=1.0 / 1024.0, scalar2=None, op0=ALU.mult), r=["st"], w=["st"])
                S.op("dve", lambda e: e.tensor_tensor(out=st[:, 2:3], in0=st[:, 0:1], in1=st[:, 0:1], op=ALU.mult), r=["st"], w=["st"])
                S.op("dve", lambda e: e.tensor_tensor(out=st[:, 2:3], in0=st[:, 1:2], in1=st[:, 2:3], op=ALU.subtract), r=["st"], w=["st"])
                S.op("act", lambda e: e.activation(out=st[:, 2:3], in_=st[:, 2:3], func=AF.Ln, bias=1e-5), r=["st"], w=["st"])
                S.op("act", lambda e: e.activation(out=st[:, 2:3], in_=st[:, 2:3], func=AF.Exp, scale=-0.5), r=["st"], w=["st"])
                S.op("dve", lambda e: e.tensor_scalar(out=dst, in0=src, scalar1=st[:, 0:1], scalar2=st[:, 2:3], op0=ALU.subtract, op1=ALU.mult), r=[srck, "st"], w=[dstk])
                S.op("dve", lambda e: e.tensor_tensor(out=dst, in0=dst, in1=gam, op=ALU.mult), r=[dstk, gk], w=[dstk])
                S.op("dve", lambda e: e.tensor_tensor(out=dst, in0=dst, in1=bet, op=ALU.add), r=[dstk, gk], w=[dstk])

            with contextlib.ExitStack() as es2:
                lng = sbt(es2, "lng", [128, 1024])
                lnb = sbt(es2, "lnb", [128, 1024])
                rw = sbt(es2, "rw", [128, 8, 32])
                rwh = sbt(es2, "rwh", [128, 8, 32], BF16)
                rwl = sbt(es2, "rwl", [128, 8, 32], BF16)
                bdn = sbt(es2, "bdn", [128, 1024])
                bdh = sbt(es2, "bdh", [128, 1024], BF16)
                bdl = sbt(es2, "bdl", [128, 1024], BF16)
                h1hl = sbt(es2, "h1hl", [128, 2, 1024], BF16)
                hTl = sbt(es2, "hTl", [128, 8, 128], BF16)
                gpad = sbt(es2, "gpad", [128, 2, 128], BF16)
                xo = sbt(es2, "xo", [128, 1024])
                mA = sbt(es2, "mA", [128, 1024])
                mB = sbt(es2, "mB", [128, 1024])
                h1 = sbt(es2, "h1", [128, 1024])
                scr = sbt(es2, "scr", [128, 1024])
                gT = sbt(es2, "gT", [128, 2, 128], BF16)
                lg = sbt(es2, "lg", [128, 32])
                S.dma("sp", lambda e: e.dma_start(out=lng[:], in_=lnp_d[0, :, :]), "c3", w=["lng"])
                S.dma("sp", lambda e: e.dma_start(out=lnb[:], in_=lnp_d[1, :, :]), "c3", w=["lng"])
                S.dma("sp", lambda e: e.dma_start(out=rw[:], in_=rw_d[:, :, :].rearrange("k p n -> p k n")), "c3", w=["rw"])
                S.dma("sp", lambda e: e.dma_start(out=bdn[:], in_=bdn_d[:, :]), "c3", w=["bdn"])
                S.op("act", lambda e: e.activation(out=rwh[:], in_=rw[:], func=AF.Copy), r=["rw"], w=["rwh"])
                S.op("dve", lambda e: e.tensor_tensor(out=rwl[:], in0=rw[:], in1=rwh[:], op=ALU.subtract), r=["rw", "rwh"], w=["rwl"])
                S.op("act", lambda e: e.activation(out=bdh[:], in_=bdn[:], func=AF.Copy), r=["bdn"], w=["bdh"])
                S.op("dve", lambda e: e.tensor_tensor(out=bdl[:], in0=bdn[:], in1=bdh[:], op=ALU.subtract), r=["bdn", "bdh"], w=["bdl"])
                S.op("dve", lambda e: e.memset(gpad[:].rearrange("p a b -> p (a b)"), 0.0), w=["gpad"])
                for j in range(16):
                    S.dma("sp", (lambda j: lambda e: e.dma_start(out=xo[:], in_=xown_d[j, :, :]))(j), "ldx", w=["xo"])
                    S.dma("sp", (lambda j: lambda e: e.dma_start(out=mA[:], in_=mixs_d[j, :, :]))(j), "ldx", r=["mixs%d" % j], w=["mA"])
                    S.dma("sp", (lambda j: lambda e: e.dma_start(out=mB[:], in_=mixs_d[j + 16, :, :]))(j), "ldx", r=["mixs%d" % (j + 16)], w=["mB"])
                    S.op("dve", lambda e: e.tensor_scalar(out=xo[:], in0=xo[:], scalar1=float(DN_ALPHA), scalar2=None, op0=ALU.mult), r=["xo"], w=["xo"])
                    S.op("dve", lambda e: e.scalar_tensor_tensor(out=xo[:], in0=mA[:], scalar=m01[:, 0:1], in1=xo[:], op0=ALU.mult, op1=ALU.add), r=["mA", "m01", "xo"], w=["xo"])
                    S.op("dve", lambda e: e.scalar_tensor_tensor(out=xo[:], in0=mB[:], scalar=m01[:, 1:2], in1=xo[:], op0=ALU.mult, op1=ALU.add), r=["mB", "m01", "xo"], w=["xo"])
                    layer_norm(xo[:], "xo", h1[:], "h1", lng[:], lnb[:], "lng", scr[:], "scr")
                    if debug:
                        S.dma("sp", (lambda j: lambda e: e.dma_start(out=dbg["h1"][j, :, :], in_=h1[:]))(j), "dbg", r=["h1"])

                    S.op("act", lambda e: e.activation(out=h1hl[:, 0, :], in_=h1[:], func=AF.Copy), r=["h1"], w=["h1hl"])
                    S.op("dve", lambda e: e.tensor_tensor(out=h1hl[:, 1, :], in0=h1[:], in1=h1hl[:, 0, :], op=ALU.subtract), r=["h1", "h1hl"], w=["h1hl"])
                    P0T = P[0][:].bitcast(BF16)
                    P1T = P[1][:].bitcast(BF16)

                    def trh(e):
                        for k in range(8):
                            e.transpose(P0T[:, k * 128:(k + 1) * 128], h1hl[:, 0, k * 128:(k + 1) * 128], ident_b)
                        for k in range(8):
                            ins = e.transpose(P1T[:, k * 128:(k + 1) * 128], h1hl[:, 1, k * 128:(k + 1) * 128], ident_b)
                        return ins
                    S.op("pe", trh, r=["h1hl", "Cb"], w=[pk(0), pk(1)])
                    S.op("act", (lambda j: lambda e: e.activation(out=h1T[:, :, j * 128:(j + 1) * 128], in_=P0T[:, :].rearrange("p (k t) -> p k t", k=8), func=AF.Copy))(j),
                         r=[pk(0)], w=["h1T"])
                    S.op("dve", lambda e: e.tensor_copy(out=hTl[:], in_=P1T[:, :].rearrange("p (k t) -> p k t", k=8)), r=[pk(1)], w=["hTl"])

                    def mmr(e, j=j):
                        n = 0
                        for k in range(8):
                            for (a, bb) in ((h1T[:, k, j * 128:(j + 1) * 128], rwh[:, k, :]), (hTl[:, k, :], rwh[:, k, :]), (h1T[:, k, j * 128:(j + 1) * 128], rwl[:, k, :])):
                                ins = e.matmul(P[2][:, 0:32], lhsT=a, rhs=bb, start=(n == 0), stop=(n == 23))
                                n += 1
                        return ins
                    S.op("pe", mmr, r=["h1T", "hTl", "rwh", "rwl"], w=[pk(2)])
                    S.op("dve", lambda e: e.tensor_tensor(out=lg[:], in0=P[2][:, 0:32], in1=rbb[:], op=ALU.add), r=[pk(2), "rbb"], w=["lg"])
                    S.op("dve", lambda e: e.max(out=st[:, 8:16], in_=lg[:]), r=["lg"], w=["st2"])
                    S.op("dve", lambda e: e.tensor_scalar(out=st[:, 16:17], in0=st[:, 8:9], scalar1=-1.0, scalar2=None, op0=ALU.mult), r=["st2"], w=["st2"])
                    S.op("act", lambda e: e.activation(out=scr[:, 0:32], in_=lg[:], func=AF.Exp, bias=st[:, 16:17]), r=["lg", "st2"], w=["scr"])
                    S.op("dve", lambda e: e.tensor_scalar(out=lg[:], in0=lg[:], scalar1=st[:, 11:12], scalar2=None, op0=ALU.is_ge), r=["lg", "st2"], w=["lg"])
                    S.op("dve", lambda e: e.tensor_tensor(out=lg[:], in0=lg[:], in1=scr[:, 0:32], op=ALU.mult), r=["lg", "scr"], w=["lg"])
                    S.op("dve", lambda e: e.tensor_reduce(out=st[:, 17:18], in_=lg[:], axis=AX.X, op=ALU.add), r=["lg"], w=["st2"])
                    S.op("dve", lambda e: e.reciprocal(out=st[:, 17:18], in_=st[:, 17:18]), r=["st2"], w=["st2"])
                    S.op("dve", (lambda j: lambda e: e.tensor_scalar(out=gates[:, j, :], in0=lg[:], scalar1=st[:, 17:18], scalar2=None, op0=ALU.mult))(j), r=["lg", "st2"], w=["gates"])
                    S.op("act", (lambda j: lambda e: e.activation(out=gpad[:, 0, 0:32], in_=gates[:, j, :], func=AF.Copy))(j), r=["gates"], w=["gpad"])
                    S.op("dve", (lambda j: lambda e: e.tensor_tensor(out=gpad[:, 1, 0:32], in0=gates[:, j, :], in1=gpad[:, 0, 0:32], op=ALU.subtract))(j), r=["gates", "gpad"], w=["gpad"])
                    P3T = P[3][:].bitcast(BF16)

                    def trg(e):
                        e.transpose(P3T[:, 0:128], gpad[:, 0, :], ident_b)
                        return e.transpose(P3T[:, 128:256], gpad[:, 1, :], ident_b)
                    S.op("pe", trg, r=["gpad", "Cb"], w=[pk(3)])
                    S.op("act", lambda e: e.activation(out=gT[:], in_=P3T[:, 0:256].rearrange("p (a t) -> p a t", a=2), func=AF.Copy), r=[pk(3)], w=["gT"])
                    for half in range(2):
                        def mmbd(e, half=half):
                            hs_ = slice(half * 512, (half + 1) * 512)
                            e.matmul(P[4 + half][:], lhsT=gT[:, 0, :], rhs=bdh[:, hs_], start=True, stop=False)
                            e.matmul(P[4 + half][:], lhsT=gT[:, 1, :], rhs=bdh[:, hs_], start=False, stop=False)
                            return e.matmul(P[4 + half][:], lhsT=gT[:, 0, :], rhs=bdl[:, hs_], start=False, stop=True)
                        S.op("pe", mmbd, r=["gT", "bdh", "bdl"], w=[pk(4 + half)])
                        S.op("dve", (lambda half, j: lambda e: e.scalar_tensor_tensor(out=acc[:, j, half * 512:(half + 1) * 512], in0=h1[:, half * 512:(half + 1) * 512],
                                                                                        scalar=float(DN_ALPHA), in1=P[4 + half][:], op0=ALU.mult, op1=ALU.add))(half, j),
                             r=["h1", pk(4 + half)], w=["acc%d" % j])

            fence()
            S.enabled = "D" in phases
            with contextlib.ExitStack() as es2:
                wg = [sbt(es2, "ewg%d" % i, [128, 8, 1024], BF16) for i in range(2)]
                wu = [sbt(es2, "ewu%d" % i, [128, 8, 1024], BF16) for i in range(2)]
                wd = sbt(es2, "wd", [128, 8, 1024], BF16)
                aT = [sbt(es2, "aT%d" % i, [128, 8, 512], BF16) for i in range(2)]
                gsb = sbt(es2, "gsb", [128, 512])
                usb = sbt(es2, "usb", [128, 512], BF16)
                sgs = sbt(es2, "sgs", [128, 512], BF16)
                ucl = sbt(es2, "ucl", [128, 512], BF16)
                it = 0
                def load_gu(ex):
                    wb_ = ex % 2
                    for k in range(8):
                        S.dma("pool", (lambda ex, k, wb_: lambda e: e.dma_start(out=wg[wb_][:, k, :], in_=wgu_d[ex, k, :, 0:1024]))(ex, k, wb_), "wg%d" % wb_, w=["wg%d" % wb_])
                        S.dma("pool", (lambda ex, k, wb_: lambda e: e.dma_start(out=wu[wb_][:, k, :], in_=wgu_d[ex, k, :, 1024:2048]))(ex, k, wb_), "wu%d" % wb_, w=["wu%d" % wb_])

                def load_d(ex):
                    for k in range(8):
                        S.dma("pool", (lambda ex, k: lambda e: e.dma_start(out=wd[:, k, :], in_=wdn_d[ex, k, :, :]))(ex, k), "wd", w=["wd"])
                load_gu(0)
                load_d(0)
                for ex in range(32):
                    wb_ = ex % 2
                    if ex + 1 < 32:
                        load_gu(ex + 1)
                    for tb in range(4):
                        ab = it % 2
                        it += 1
                        for fc in range(8):
                            pg_, pu_ = (0, 1) if fc % 2 == 0 else (2, 3)

                            def mmg(e, fc=fc, tb=tb, wb_=wb_, pg_=pg_, pu_=pu_):
                                for k in range(8):
                                    e.matmul(P[pg_][:], lhsT=wg[wb_][:, k, fc * 128:(fc + 1) * 128], rhs=h1T[:, k, tb * 512:(tb + 1) * 512], start=(k == 0), stop=(k == 7))
                                for k in range(8):
                                    ins = e.matmul(P[pu_][:], lhsT=wu[wb_][:, k, fc * 128:(fc + 1) * 128], rhs=h1T[:, k, tb * 512:(tb + 1) * 512], start=(k == 0), stop=(k == 7))
                                return ins
                            S.op("pe", mmg, r=["wg%d" % wb_, "wu%d" % wb_, "h1T"], w=[pk(pg_), pk(pu_)])
                            bgc = bgu[:, ex * 16 + fc:ex * 16 + fc + 1]
                            buc = bgu[:, ex * 16 + 8 + fc:ex * 16 + 8 + fc + 1]
                            S.op("dve", (lambda pg_, bgc: lambda e: e.tensor_scalar(out=gsb[:], in0=P[pg_][:], scalar1=bgc, scalar2=7.0, op0=ALU.add, op1=ALU.min))(pg_, bgc),
                                 r=[pk(pg_), "bgu"], w=["gsb"])
                            S.op("act", (lambda pu_, buc: lambda e: e.activation(out=usb[:], in_=P[pu_][:], func=AF.Identity, bias=buc))(pu_, buc), r=[pk(pu_), "bgu"], w=["usb"])
                            S.op("act", lambda e: e.activation(out=sgs[:], in_=gsb[:], func=AF.Sigmoid, scale=1.702), r=["gsb"], w=["sgs"])
                            S.op("pool", lambda e: e.tensor_scalar(out=ucl[:], in0=usb[:], scalar1=7.0, scalar2=-7.0, op0=ALU.min, op1=ALU.max), r=["usb"], w=["ucl"])
                            S.op("dve", lambda e: e.tensor_tensor(out=gsb[:], in0=gsb[:], in1=sgs[:], op=ALU.mult), r=["gsb", "sgs"], w=["gsb"])
                            S.op("dve", (lambda ab, fc: lambda e: e.scalar_tensor_tensor(out=aT[ab][:, fc, :], in0=ucl[:], scalar=1.0, in1=gsb[:], op0=ALU.add, op1=ALU.mult))(ab, fc),
                                 r=["ucl", "gsb"], w=["aT%d" % ab])
                        for tt in range(4):
                            j = tb * 4 + tt
                            for half in range(2):
                                pb = 4 + half + 2 * (tt % 2)

                                def mmd(e, ab=ab, tt=tt, half=half, pb=pb):
                                    for fc in range(8):
                                        ins = e.matmul(P[pb][:], lhsT=aT[ab][:, fc, tt * 128:(tt + 1) * 128], rhs=wd[:, fc, half * 512:(half + 1) * 512], start=(fc == 0), stop=(fc == 7))
                                    return ins
                                S.op("pe", mmd, r=["aT%d" % ab, "wd"], w=[pk(pb)])
                                S.op("dve", (lambda j, half, pb, ex: lambda e: e.scalar_tensor_tensor(out=acc[:, j, half * 512:(half + 1) * 512], in0=P[pb][:],
                                                                                                       scalar=gates[:, j, ex:ex + 1], in1=acc[:, j, half * 512:(half + 1) * 512],
                                                                                                       op0=ALU.mult, op1=ALU.add))(j, half, pb, ex),
                                     r=[pk(pb), "gates", "acc%d" % j], w=["acc%d" % j])
                    if ex + 1 < 32:
                        load_d(ex + 1)

            fence()
            S.enabled = "E" in phases
            with contextlib.ExitStack() as es2:
                l2g = sbt(es2, "l2g", [128, 1024])
                l2b = sbt(es2, "l2b", [128, 1024])
                l3g = sbt(es2, "l3g", [128, 1024])
                l3b = sbt(es2, "l3b", [128, 1024])
                pgb = sbt(es2, "pgb", [128, 1024])
                wpg = sbt(es2, "wpg", [128, 8, 1024], BF16)
                plw = sbt(es2, "plw", [128, 2, 1024], BF16)
                ptn = sbt(es2, "ptn", [128, 2, 2048], BF16)
                h2 = sbt(es2, "h2", [128, 1024])
                h2b = sbt(es2, "h2b", [128, 1024], BF16)
                h2T = sbt(es2, "h2T", [128, 8, 128], BF16)
                scrE = sbt(es2, "scr2", [128, 1024])
                gt = sbt(es2, "gt", [128, 1024])
                y3 = sbt(es2, "y3", [128, 1024])
                PT3 = P[0][:].bitcast(BF16)
                S.dma("sp", lambda e: e.dma_start(out=l2g[:], in_=lnp_d[2, :, :]), "c4", w=["l2"])
                S.dma("sp", lambda e: e.dma_start(out=l2b[:], in_=lnp_d[3, :, :]), "c4", w=["l2"])
                S.dma("sp", lambda e: e.dma_start(out=l3g[:], in_=lnp_d[4, :, :]), "c4", w=["l3"])
                S.dma("sp", lambda e: e.dma_start(out=l3b[:], in_=lnp_d[5, :, :]), "c4", w=["l3"])
                S.dma("sp", lambda e: e.dma_start(out=pgb[:], in_=pgb_d[:, :]), "c4", w=["pgb"])
                for k in range(8):
                    S.dma("pool", (lambda k: lambda e: e.dma_start(out=wpg[:, k, :], in_=wpg_d[k, :, :]))(k), "w4", w=["wpg"])
                for k in range(2):
                    S.dma("pool", (lambda k: lambda e: e.dma_start(out=plw[:, k, :], in_=plew_d[k, :, :]))(k), "w4", w=["plw"])
                    S.dma("pool", (lambda k: lambda e: e.dma_start(out=ptn[:, k, :], in_=ptown_d[k, :, :]))(k), "w4", w=["ptn"])
                for j in range(16):
                    if debug:
                        S.dma("sp", (lambda j: lambda e: e.dma_start(out=dbg["acc"][j, :, :], in_=acc[:, j, :]))(j), "dbg", r=["acc%d" % j])
                    layer_norm(acc[:, j, :], "acc%d" % j, h2[:], "h2", l2g[:], l2b[:], "l2", scrE[:], "scr2")
                    S.op("act", lambda e: e.activation(out=h2b[:], in_=h2[:], func=AF.Copy), r=["h2"], w=["h2b"])

                    def trh2(e):
                        for k in range(8):
                            ins = e.transpose(PT3[:, k * 128:(k + 1) * 128], h2b[:, k * 128:(k + 1) * 128], ident_b)
                        return ins
                    S.op("pe", trh2, r=["h2b", "Cb"], w=[pk(0)])
                    S.op("act", lambda e: e.activation(out=h2T[:], in_=PT3[:, :].rearrange("p (k t) -> p k t", k=8), func=AF.Copy), r=[pk(0)], w=["h2T"])
                    for half in range(2):
                        hs_ = slice(half * 512, (half + 1) * 512)

                        def mmgt(e, hs_=hs_, half=half):
                            for k in range(8):
                                ins = e.matmul(P[2 + half][:], lhsT=h2T[:, k, :], rhs=wpg[:, k, hs_], start=(k == 0), stop=(k == 7))
                            return ins
                        S.op("pe", mmgt, r=["h2T", "wpg"], w=[pk(2 + half)])

                        def mmpl(e, hs_=hs_, half=half, j=j):
                            for k in range(2):
                                ins = e.matmul(P[4 + half][:], lhsT=ptn[:, k, j * 128:(j + 1) * 128], rhs=plw[:, k, hs_], start=(k == 0), stop=(k == 1))
                            return ins
                        S.op("pe", mmpl, r=["ptn", "plw"], w=[pk(4 + half)])
                        S.op("dve", (lambda hs_, half: lambda e: e.tensor_tensor(out=gt[:, hs_], in0=P[2 + half][:], in1=pgb[:, hs_], op=ALU.add))(hs_, half),
                             r=[pk(2 + half), "pgb"], w=["gt"])
                        S.op("act", (lambda hs_: lambda e: e.activation(out=gt[:, hs_], in_=gt[:, hs_], func=AF.Sigmoid))(hs_), r=["gt"], w=["gt"])
                        S.op("dve", (lambda hs_, half: lambda e: e.tensor_tensor(out=gt[:, hs_], in0=P[4 + half][:], in1=gt[:, hs_], op=ALU.mult))(hs_, half),
                             r=[pk(4 + half), "gt"], w=["gt"])
                    S.op("dve", lambda e: e.scalar_tensor_tensor(out=y3[:], in0=h2[:], scalar=float(DN_ALPHA), in1=gt[:], op0=ALU.mult, op1=ALU.add), r=["h2", "gt"], w=["y3"])
                    layer_norm(y3[:], "y3", y3[:], "y3", l3g[:], l3b[:], "l3", scrE[:], "scr2")
                    S.dma("sp", (lambda j: lambda e: e.dma_start(out=out_d[j, :, :], in_=y3[:]))(j), "outw", r=["y3"])

        S.emit(final_wait_slots=[x for x in ["outw", "dbg", "ogw", "mixw"] if x in S.dma_slots])
    return nc


def _consts():
    c = np.zeros((128, NCONST), np.float32)
    i = np.arange(128)
    same = (i[:, None] // 64) == (i[None, :] // 64)
    c[:, 0:128] = np.eye(128)
    c[:, 128:256] = ((i[:, None] <= i[None, :]) & same)
    c[:, 256:384] = same
    c[:, 384:512] = np.where((i[None, :] < i[:, None]) & same, 0.0, BIG)
    c[:, 512:640] = np.where((i[:, None] <= i[None, :]) & same, 0.0, BIG)
    c[:, 640:768] = (i[None, :] >= i[:, None])
    c[:, 768:896] = 1.0
    pm = np.zeros((128, 128), np.float32)
    for m in range(128):
        d = m % 64
        if d < 8:
            pm[m + 8, m] = -1.0
        elif d < 16:
            pm[m - 8, m] = 1.0
    c[:, 896:1024] = pm
    c[:, 1024] = (i < 64)
    c[:, 1025] = (i >= 64)
    inv_freq = (500000.0 ** (-np.arange(0, 16, 2, dtype=np.float32) / np.float32(16))).astype(np.float32)
    d = i % 64
    c[:, 1026] = np.where(d < 16, inv_freq[d % 8], 0.0)
    c[:, 1027] = (i < 64)
    c[:, 1028] = (i >= 64)
    return c


def _bc(v, n=128):
    return np.ascontiguousarray(np.broadcast_to(np.asarray(v, np.float32).reshape(1, -1), (n, np.asarray(v).size)))


def _prep(inp):
    f = lambda a: np.ascontiguousarray(np.asarray(a, np.float32))
    x = f(inp["x"])
    w_in = f(inp["w_in"][0])
    O = [0, 512, 1024, 1536, 2048, 2052, 2056, 2568, 3080, 3592]
    gqkv = w_in[:, 0:1536]
    gz = w_in[:, O[3]:O[4]]
    gba = w_in[:, O[4]:O[6]]
    dq = w_in[:, O[6]:O[7]]
    dk = w_in[:, O[7]:O[8]]
    dv = w_in[:, O[8]:O[9]]
    wfm = np.concatenate([gqkv, dq, dk], 1).reshape(8, 128, 2560)
    wtm = np.concatenate([gz, dv, gba], 1).reshape(8, 128, 1032)
    convw = f(inp["conv_w"][0]).T.reshape(12, 128, 4).transpose(1, 0, 2).reshape(128, 48)
    hs = np.concatenate([_bc(inp["a_log"][0]), _bc(inp["dt_bias"][0])], 1)
    gnw4 = _bc(np.tile(f(inp["gdn_norm_w"][0]), 4))
    dnw4 = _bc(np.tile(f(inp["diff_norm_w"][0]), 4))
    lamv = np.concatenate([_bc(inp["lam_q1"][0]), _bc(inp["lam_k1"][0]), _bc(inp["lam_q2"][0]), _bc(inp["lam_k2"][0])], 1)
    shared = dict(
        wfm=np.ascontiguousarray(wfm), wtm=np.ascontiguousarray(wtm), convw=np.ascontiguousarray(convw), hs=np.ascontiguousarray(hs),
        gnw4=gnw4, dnw4=dnw4, lamv=np.ascontiguousarray(lamv),
        wout=f(inp["w_out"][0]).reshape(8, 128, 1024), consts=_consts(),
        msk4=np.ascontiguousarray(np.concatenate([np.tile(_consts()[:, 384:512], (1, 4)), np.tile(_consts()[:, 512:640], (1, 4))], 1)),
        lnp=np.ascontiguousarray(np.stack([_bc(inp[k][0]) for k in ("ln1_g", "ln1_b", "ln2_g", "ln2_b", "ln3_g", "ln3_b")], 0)),
        rw=f(inp["router_w"][0]).reshape(8, 128, 32), rbb=_bc(inp["router_b"][0]),
        wgu=f(inp["w_gu"][0]).reshape(32, 8, 128, 2048),
        bgu=np.ascontiguousarray(f(inp["b_gu"][0]).reshape(32, 16, 128).transpose(2, 0, 1).reshape(128, 512)),
        wdn=f(inp["w_down"][0]).reshape(32, 8, 128, 1024), bdn=np.concatenate([f(inp["b_down"][0]), np.zeros((96, 1024), np.float32)], 0),
        wpg=f(inp["ple_gate_w"][0]).reshape(8, 128, 1024), pgb=_bc(inp["ple_gate_b"][0]),
        plew=f(inp["ple_w"][0]).reshape(2, 128, 1024),
    )
    pos = np.asarray(inp["positions"], np.int32)
    p = f(inp["p"][0])
    maps = []
    for c in range(8):
        b, half = c // 2, c % 2
        m = dict(shared)
        m["xT"] = np.ascontiguousarray(x[b].T).reshape(8, 128, SEQ)
        m["posb"] = np.ascontiguousarray(np.broadcast_to(pos[b][None, :], (128, SEQ)))
        m01 = np.zeros((128, 2), np.float32)
        m01[:, half] = 1.0
        m["m01"] = m01
        m["xown"] = np.ascontiguousarray(x[b, half * 2048:(half + 1) * 2048]).reshape(16, 128, 1024)
        m["ptown"] = np.ascontiguousarray(p[b, half * 2048:(half + 1) * 2048].T).reshape(2, 128, 2048)
        maps.append(m)
    return maps


def _run(inputs, debug=False, phases="12CDE", nblk=NBLK):
    maps = _prep(inputs)
    if "D" not in phases:
        for m in maps:
            m.pop("wgu")
            m.pop("wdn")
    nc = build_program(debug=debug, phases=phases, nblk=nblk)
    res = run_bass_kernel_spmd(nc, maps, core_ids=list(range(8)))
    return res.results


def kernel(**inputs):
    results = _run(inputs, debug=False)
    out = np.zeros((4, SEQ, 1024), np.float32)
    for c in range(8):
        b, half = c // 2, c % 2
        out[b, half * 2048:(half + 1) * 2048] = np.asarray(results[c]["out"], np.float32).reshape(2048, 1024)
    return out
```

```python
import contextlib
import math
import numpy as np
import concourse.bass as bass
import concourse.mybir as mybir
from concourse.bass_utils import run_bass_kernel_spmd

F32 = mybir.dt.float32
BF16 = mybir.dt.bfloat16
I32 = mybir.dt.int32
AF = mybir.ActivationFunctionType
ALU = mybir.AluOpType
AX = mybir.AxisListType

SEQ = 4096
NBLK = 8
DN_ALPHA = 2.0 ** 0.25
LAM_INIT = 0.2
NCONST = 1032
BIG = 30000.0


class Sched:
    STREAMS = ("pe", "act", "dve", "pool", "sp")

    def __init__(self, nc):
        self.nc = nc
        self.ops = {s: [] for s in self.STREAMS}
        self.last_write = {}
        self.readers = {}
        self.dma_slots = {}
        self.sync_same = {"act", "dve", "pool"}

    def _deps_for(self, reads, writes):
        deps = []
        for k in reads:
            t = self.last_write.get(k)
            if t is not None:
                deps.append(t)
        for k in writes:
            t = self.last_write.get(k)
            if t is not None:
                deps.append(t)
            deps.extend(self.readers.get(k, ()))
        return [(d[0], d[1], self.dma_slots[d[1]]) if d[0] == "dma" else d for d in deps]

    def _commit(self, tok, reads, writes):
        for k in reads:
            self.readers.setdefault(k, []).append(tok)
        for k in writes:
            self.last_write[k] = tok
            self.readers[k] = []

    enabled = True

    def fence(self, fn):
        en = self.enabled
        self.enabled = True
        self.op("dve", fn, r=(), w=("__fence__",), _nofence=True)
        self.enabled = en

    def op(self, stream, fn, r=(), w=(), _nofence=False):
        if not self.enabled:
            return
        if not _nofence:
            r = tuple(r) + ("__fence__",)
        w = tuple(w) + tuple(k for k in r if len(k) == 2 and k[0] == "P" and k[1].isdigit())
        deps = self._deps_for(r, w)
        idx = len(self.ops[stream])
        self.ops[stream].append(dict(fn=fn, deps=deps, dma=None))
        self._commit(("eng", stream, idx), r, w)

    def dma(self, stream, fn, slot, r=(), w=()):
        if not self.enabled:
            return
        r = tuple(r) + ("__fence__",)
        deps = self._deps_for(r, w)
        n = self.dma_slots.get(slot, 0) + 1
        self.dma_slots[slot] = n
        self.ops[stream].append(dict(fn=fn, deps=deps, dma=(slot, n)))
        self._commit(("dma", slot, n), r, w)

    def emit(self, final_wait_slots=()):
        nc = self.nc
        milestone = {s: set() for s in self.STREAMS}
        for s in self.STREAMS:
            for o in self.ops[s]:
                for d in o["deps"]:
                    if d[0] == "eng":
                        if d[1] == s and s not in self.sync_same:
                            continue
                        milestone[d[1]].add(d[2])
        mcount = {}
        for s in self.STREAMS:
            c = 0
            arr = []
            for i in range(len(self.ops[s])):
                if i in milestone[s]:
                    c += 1
                arr.append(c)
            mcount[s] = arr
        with contextlib.ExitStack() as es:
            esem = {s: es.enter_context(nc.semaphore("s_" + s)) for s in self.STREAMS}
            dsem = {sl: es.enter_context(nc.semaphore("d_" + str(sl))) for sl in self.dma_slots}
            block = es.enter_context(nc.Block())

            def run_stream(s, eng):
                waited = {}
                for i, o in enumerate(self.ops[s]):
                    need = {}
                    for d in o["deps"]:
                        if d[0] == "eng":
                            if d[1] == s and s not in self.sync_same:
                                continue
                            key = ("e", d[1])
                            val = mcount[d[1]][d[2]]
                        else:
                            key = ("d", d[1])
                            val = 16 * d[2]
                        if val > need.get(key, 0):
                            need[key] = val
                    for key, val in need.items():
                        if waited.get(key, 0) >= val:
                            continue
                        waited[key] = val
                        sem = esem[key[1]] if key[0] == "e" else dsem[key[1]]
                        eng.wait_ge(sem, val)
                    ins = o["fn"](eng)
                    if o["dma"] is not None:
                        ins.then_inc(dsem[o["dma"][0]], 16)
                    elif i in milestone[s]:
                        ins.then_inc(esem[s], 1)
                if s == "sp":
                    for sl in final_wait_slots:
                        eng.wait_ge(dsem[sl], 16 * self.dma_slots[sl])

            block.sync(lambda e: run_stream("sp", e))
            block.tensor(lambda e: run_stream("pe", e))
            block.scalar(lambda e: run_stream("act", e))
            block.vector(lambda e: run_stream("dve", e))
            block.gpsimd(lambda e: run_stream("pool", e))


def build_program(debug=False, phases="12CDE", nblk=NBLK, stop=None):
    nc = bass.Bass("TRN2", target_bir_lowering=False)
    S = Sched(nc)

    def mark(level):
        if stop is not None and level > stop:
            S.enabled = False

    def din(name, shape, dt=F32):
        return nc.dram_tensor(name, list(shape), dt, kind="ExternalInput").ap()

    xT = din("xT", [8, 128, SEQ])
    posb = din("posb", [128, SEQ], I32)
    wfm_d = din("wfm", [8, 128, 2560])
    wtm_d = din("wtm", [8, 128, 1032])
    convw_d = din("convw", [128, 48])
    hs_d = din("hs", [128, 8])
    gnw_d = din("gnw4", [128, 512])
    dnw_d = din("dnw4", [128, 512])
    lamv_d = din("lamv", [128, 256])
    wout_d = din("wout", [8, 128, 1024])
    const_d = din("consts", [128, NCONST])
    msk4_d = din("msk4", [128, 1024])
    m01_d = din("m01", [128, 2])
    xown_d = din("xown", [16, 128, 1024])
    ptown_d = din("ptown", [2, 128, 2048])
    lnp_d = din("lnp", [6, 128, 1024])
    rw_d = din("rw", [8, 128, 32])
    rbb_d = din("rbb", [128, 32])
    wgu_d = din("wgu", [32, 8, 128, 2048]) if "D" in phases else None
    bgu_d = din("bgu", [128, 512])
    wdn_d = din("wdn", [32, 8, 128, 1024]) if "D" in phases else None
    bdn_d = din("bdn", [128, 1024])
    wpg_d = din("wpg", [8, 128, 1024])
    pgb_d = din("pgb", [128, 1024])
    plew_d = din("plew", [2, 128, 1024])
    out_d = nc.dram_tensor("out", [16, 128, 1024], F32, kind="ExternalOutput").ap()
    ogs_d = nc.dram_tensor("ogs", [32, 128, 512], BF16, kind="Internal").ap()
    mixs_d = nc.dram_tensor("mixs", [32, 128, 1024], F32, kind="Internal").ap()
    dbg = {}
    if debug:
        dbg["mix"] = nc.dram_tensor("dbg_mix", [32, 128, 1024], F32, kind="ExternalOutput").ap()
        dbg["og"] = nc.dram_tensor("dbg_og", [32, 128, 512], F32, kind="ExternalOutput").ap()
        dbg["h1"] = nc.dram_tensor("dbg_h1", [16, 128, 1024], F32, kind="ExternalOutput").ap()
        dbg["acc"] = nc.dram_tensor("dbg_acc", [16, 128, 1024], F32, kind="ExternalOutput").ap()

    with contextlib.ExitStack() as top:
        P = [top.enter_context(nc.psum_tensor("P%d" % i, [128, 512], F32)) for i in range(8)]

        def pk(i):
            return "P%d" % i

        def sbt(es, name, shape, dt=F32):
            return es.enter_context(nc.sbuf_tensor("sb_" + name, list(shape), dt))

        C = sbt(top, "C", [128, NCONST])
        ident = C[:, 0:128]
        tri = C[:, 128:256]
        blk = C[:, 256:384]
        mstrict = C[:, 384:512]
        minclT = C[:, 512:640]
        ind = C[:, 1024:1026]
        freq = C[:, 1026:1027]
        c0 = C[:, 1027:1028]
        c1 = C[:, 1028:1029]
        fz = sbt(top, "fz", [128, 8])

        def fence():
            S.fence(lambda e: e.memset(fz[:], 0.0))
        Cb = sbt(top, "Cb", [128, 768], BF16)
        ident_b = Cb[:, 0:128]
        causal_b = Cb[:, 128:256]
        ones_b = Cb[:, 256:384]
        perm_b = Cb[:, 384:512]
        tri_b = Cb[:, 512:640]
        blk_b = Cb[:, 640:768]
        S.dma("sp", lambda e: e.dma_start(out=C[:], in_=const_d[:, :]), "c0", w=["C"])
        S.op("dve", lambda e: e.tensor_copy(out=Cb[:, 0:128], in_=C[:, 0:128]), r=["C"], w=["Cb"])
        S.op("dve", lambda e: e.tensor_copy(out=Cb[:, 128:384], in_=C[:, 640:896]), r=["C"], w=["Cb"])
        S.op("dve", lambda e: e.tensor_copy(out=Cb[:, 384:512], in_=C[:, 896:1024]), r=["C"], w=["Cb"])
        S.op("dve", lambda e: e.tensor_copy(out=Cb[:, 512:768], in_=C[:, 128:384]), r=["C"], w=["Cb"])

        S.enabled = "1" in phases
        with contextlib.ExitStack() as es:
            wg1 = sbt(es, "wg1", [128, 8, 1536], BF16)
            wt1 = sbt(es, "wt1", [128, 8, 520], BF16)
            xTb = sbt(es, "xTb", [128, 8, 512], BF16)
            cin = sbt(es, "cin", [128, 12, 515], BF16)
            cw = sbt(es, "cw", [128, 48])
            hsb = sbt(es, "hsb", [128, 8])
            nega = sbt(es, "nega", [128, 4])
            gnw = sbt(es, "gnw", [128, 512])
            ycv = sbt(es, "ycv", [128, 512])
            gfm = sbt(es, "gfm", [128, 12, 512], BF16)
            zs = sbt(es, "zs", [128, 4, 512], BF16)
            beta = sbt(es, "beta", [128, 4, 4])
            ldt = sbt(es, "ldt", [128, 4, 4])
            sm = sbt(es, "sm", [128, 64])
            sq = sbt(es, "sq", [128, 8, 128], BF16)
            tA = sbt(es, "tA", [128, 512])
            tB = sbt(es, "tB", [128, 512])
            qn = sbt(es, "qn", [128, 4, 128], BF16)
            kn = sbt(es, "kn", [128, 4, 128], BF16)
            Rr = sbt(es, "Rr", [128, 4, 256])
            Rb = sbt(es, "Rb", [128, 4, 256], BF16)
            kd0 = sbt(es, "kd0", [128, 4, 128], BF16)
            kd1 = sbt(es, "kd1", [128, 4, 128], BF16)
            ldB = sbt(es, "ldB", [128, 8, 128], BF16)
            ldhl = sbt(es, "ldhl", [128, 8], BF16)
            msk4 = sbt(es, "msk4", [128, 1024])
            mstrict4 = msk4[:, 0:512]
            minclT4 = msk4[:, 512:1024]
            S.dma("sp", lambda e: e.dma_start(out=msk4[:], in_=msk4_d[:, :]), "c1", w=["msk4"])
            Ga = sbt(es, "Ga", [128, 4, 128])
            Gb = sbt(es, "Gb", [128, 4, 128])
            egr = sbt(es, "egr", [128, 4, 128])
            Em = sbt(es, "Em", [128, 4, 128])
            ETm = sbt(es, "ETm", [128, 4, 128])
            Nb = sbt(es, "Nb", [128, 4, 128], BF16)
            Mb = sbt(es, "Mb", [128, 4, 128], BF16)
            qkT = sbt(es, "qkT", [128, 4, 128], BF16)
            wb = sbt(es, "wb", [128, 4, 128], BF16)
            wT = sbt(es, "wT", [128, 4, 128], BF16)
            qd = sbt(es, "qd", [128, 4, 128], BF16)
            St = sbt(es, "St", [128, 4, 128])
            Sb = sbt(es, "Sb", [128, 4, 128], BF16)
            vnb = sbt(es, "vnb", [128, 4, 128], BF16)
            og = sbt(es, "og", [128, 4, 128])
            ogb = sbt(es, "ogb", [128, 512], BF16)
            PT = P[1][:].bitcast(BF16)

            for k in range(8):
                S.dma("pool", (lambda k: lambda e: e.dma_start(out=wg1[:, k, :], in_=wfm_d[k, :, 0:1536]))(k), "w1", w=["wg1"])
                S.dma("pool", (lambda k: lambda e: e.dma_start(out=wt1[:, k, 0:512], in_=wtm_d[k, :, 0:512]))(k), "w1", w=["wt1"])
                S.dma("pool", (lambda k: lambda e: e.dma_start(out=wt1[:, k, 512:520], in_=wtm_d[k, :, 1024:1032]))(k), "w1", w=["wt1"])
            S.dma("sp", lambda e: e.dma_start(out=cw[:], in_=convw_d[:, :]), "c1", w=["cw"])
            S.dma("sp", lambda e: e.dma_start(out=hsb[:], in_=hs_d[:, :]), "c1", w=["hsb"])
            S.dma("sp", lambda e: e.dma_start(out=gnw[:], in_=gnw_d[:, :]), "c1", w=["gnw"])
            S.op("act", lambda e: e.activation(out=nega[:], in_=hsb[:, 0:4], func=AF.Exp), r=["hsb"], w=["nega"])
            S.op("dve", lambda e: e.tensor_scalar(out=nega[:], in0=nega[:], scalar1=-1.0, scalar2=None, op0=ALU.mult), r=["nega"], w=["nega"])
            S.op("dve", lambda e: e.memset(cin[:].rearrange("p a b -> p (a b)"), 0.0), w=["cin"] + ["cin%d" % i for i in range(12)])
            S.op("dve", lambda e: e.memset(St[:].rearrange("p a b -> p (a b)"), 0.0), w=["St"])
            S.op("dve", lambda e: e.memset(Sb[:].rearrange("p a b -> p (a b)"), 0.0), w=["Sb"])
            S.op("dve", lambda e: e.memset(vnb[:].rearrange("p a b -> p (a b)"), 0.0), w=["vnb"])

            for b in range(nblk):
                t0 = b * 512
                mark(2)
                S.dma("pool", (lambda t0: lambda e: e.dma_start(out=xTb[:], in_=xT[:, :, t0:t0 + 512].rearrange("k p t -> p k t")))(t0), "x1", w=["xTb"])
                for ch in range(12):
                    def mm(e, ch=ch):
                        for k in range(8):
                            ins = e.matmul(P[0][:], lhsT=wg1[:, k, ch * 128:(ch + 1) * 128], rhs=xTb[:, k, :], start=(k == 0), stop=(k == 7))
                        return ins
                    S.op("pe", mm, r=["wg1", "xTb"], w=[pk(0)])
                    S.op("act", (lambda ch: lambda e: e.activation(out=cin[:, ch, 3:515], in_=P[0][:], func=AF.Copy))(ch), r=[pk(0)], w=["cin%d" % ch, "cin"])
                    S.op("dve", (lambda ch: lambda e: e.tensor_scalar(out=ycv[:], in0=cin[:, ch, 0:512], scalar1=cw[:, ch * 4:ch * 4 + 1], scalar2=None, op0=ALU.mult))(ch),
                         r=["cin%d" % ch, "cw"], w=["ycv"])
                    for wi in range(1, 4):
                        S.op("dve", (lambda ch, wi: lambda e: e.scalar_tensor_tensor(out=ycv[:], in0=cin[:, ch, wi:wi + 512], scalar=cw[:, ch * 4 + wi:ch * 4 + wi + 1],
                                                                                     in1=ycv[:], op0=ALU.mult, op1=ALU.add))(ch, wi), r=["cin%d" % ch, "cw", "ycv"], w=["ycv"])
                    S.op("act", (lambda ch: lambda e: e.activation(out=gfm[:, ch, :], in_=ycv[:], func=AF.Silu))(ch), r=["ycv"], w=["gfm"])
                    S.op("pool", (lambda ch: lambda e: e.tensor_copy(out=cin[:, ch, 0:3], in_=cin[:, ch, 512:515]))(ch), r=["cin%d" % ch], w=["cin%d" % ch])
                mark(3)
                for t in range(4):
                    c0_ = t * 128

                    def mmz(e, c0_=c0_):
                        for k in range(8):
                            ins = e.matmul(P[0][:], lhsT=xTb[:, k, c0_:c0_ + 128], rhs=wt1[:, k, 0:512], start=(k == 0), stop=(k == 7))
                        return ins
                    S.op("pe", mmz, r=["wt1", "xTb"], w=[pk(0)])
                    S.op("act", (lambda t: lambda e: e.activation(out=zs[:, t, :], in_=P[0][:], func=AF.Silu))(t), r=[pk(0)], w=["zs"])

                    def mmb(e, c0_=c0_):
                        for k in range(8):
                            ins = e.matmul(P[2][:, 0:8], lhsT=xTb[:, k, c0_:c0_ + 128], rhs=wt1[:, k, 512:520], start=(k == 0), stop=(k == 7))
                        return ins
                    S.op("pe", mmb, r=["wt1", "xTb"], w=[pk(2)])
                    S.op("act", lambda e: e.activation(out=sm[:, 0:4], in_=P[2][:, 0:4], func=AF.Exp, scale=-1.0), r=[pk(2)], w=["sm"])
                    S.op("dve", lambda e: e.tensor_scalar(out=sm[:, 0:4], in0=sm[:, 0:4], scalar1=1.0, scalar2=None, op0=ALU.add), r=["sm"], w=["sm"])
                    S.op("dve", (lambda t: lambda e: e.reciprocal(out=beta[:, t, :], in_=sm[:, 0:4]))(t), r=["sm"], w=["beta"])
                    S.op("dve", lambda e: e.tensor_tensor(out=sm[:, 4:8], in0=P[2][:, 4:8], in1=hsb[:, 4:8], op=ALU.add), r=[pk(2), "hsb"], w=["sm"])
                    S.op("dve", lambda e: e.tensor_scalar(out=sm[:, 8:12], in0=sm[:, 4:8], scalar1=-1.0, scalar2=None, op0=ALU.mult), r=["sm"], w=["sm"])
                    S.op("dve", lambda e: e.tensor_tensor(out=sm[:, 8:12], in0=sm[:, 8:12], in1=sm[:, 4:8], op=ALU.max), r=["sm"], w=["sm"])
                    S.op("act", lambda e: e.activation(out=sm[:, 8:12], in_=sm[:, 8:12], func=AF.Exp, scale=-1.0), r=["sm"], w=["sm"])
                    S.op("act", lambda e: e.activation(out=sm[:, 8:12], in_=sm[:, 8:12], func=AF.Ln, bias=1.0), r=["sm"], w=["sm"])
                    S.op("dve", lambda e: e.tensor_scalar(out=sm[:, 4:8], in0=sm[:, 4:8], scalar1=0.0, scalar2=None, op0=ALU.max), r=["sm"], w=["sm"])
                    S.op("dve", lambda e: e.tensor_tensor(out=sm[:, 4:8], in0=sm[:, 4:8], in1=sm[:, 8:12], op=ALU.add), r=["sm"], w=["sm"])
                    S.op("dve", (lambda t: lambda e: e.tensor_tensor(out=ldt[:, t, :], in0=sm[:, 4:8], in1=nega[:], op=ALU.mult))(t), r=["sm", "nega"], w=["ldt"])

                for t in range(4):
                    cs = slice(t * 128, (t + 1) * 128)
                    bet = beta[:, t, :]
                    ld = ldt[:, t, :]
                    mark(4)
                    S.op("pool", (lambda cs: lambda e: e.tensor_tensor(out=sq[:], in0=gfm[:, 0:8, cs], in1=gfm[:, 0:8, cs], op=ALU.mult))(cs), r=["gfm"], w=["sq"])
                    sqf = sq[:].rearrange("p h d -> p (h d)")
                    S.op("pe", lambda e: e.matmul(P[2][:], lhsT=ones_b, rhs=sqf[:, 0:512], start=True, stop=True), r=["sq", "Cb"], w=[pk(2)])
                    S.op("pe", lambda e: e.matmul(P[3][:], lhsT=ones_b, rhs=sqf[:, 512:1024], start=True, stop=True), r=["sq", "Cb"], w=[pk(3)])
                    S.op("act", lambda e: e.activation(out=tA[:], in_=P[2][:], func=AF.Ln, bias=1e-6), r=[pk(2)], w=["tA"])
                    S.op("act", lambda e: e.activation(out=tA[:], in_=tA[:], func=AF.Exp, scale=-0.5), r=["tA"], w=["tA"])
                    S.op("act", lambda e: e.activation(out=tB[:], in_=P[3][:], func=AF.Ln, bias=1e-6), r=[pk(3)], w=["tB"])
                    S.op("act", lambda e: e.activation(out=tB[:], in_=tB[:], func=AF.Exp, scale=-0.5), r=["tB"], w=["tB"])
                    S.op("dve", (lambda cs: lambda e: e.scalar_tensor_tensor(out=qn[:], in0=gfm[:, 0:4, cs], scalar=float(128 ** -0.5),
                                                                             in1=tA[:].rearrange("p (h d) -> p h d", h=4), op0=ALU.mult, op1=ALU.mult))(cs), r=["gfm", "tA"], w=["qn"])
                    S.op("dve", (lambda cs: lambda e: e.tensor_tensor(out=kn[:], in0=gfm[:, 4:8, cs], in1=tB[:].rearrange("p (h d) -> p h d", h=4), op=ALU.mult))(cs),
                         r=["gfm", "tB"], w=["kn"])

                    mark(4.2)
                    def trkv(e, cs=cs):
                        for h in range(4):
                            e.transpose(PT[:, h * 128:(h + 1) * 128], kn[:, h, :], ident_b)
                        for h in range(4):
                            ins = e.transpose(PT[:, 512 + h * 128:512 + (h + 1) * 128], gfm[:, 8 + h, cs], ident_b)
                        return ins
                    S.op("pe", trkv, r=["kn", "gfm", "Cb"], w=[pk(1)])
                    mark(4.4)
                    S.op("act", (lambda ld: lambda e: e.activation(out=ldhl[:, 0:4], in_=ld, func=AF.Copy))(ld), r=["ldt"], w=["ldhl"])
                    S.op("dve", (lambda ld: lambda e: e.tensor_tensor(out=ldhl[:, 4:8], in0=ld, in1=ldhl[:, 0:4], op=ALU.subtract))(ld), r=["ldt", "ldhl"], w=["ldhl"])

                    def mmg2(e):
                        e.matmul(P[5][:, 0:4], lhsT=tri_b, rhs=ldhl[:, 0:4], start=True, stop=False)
                        e.matmul(P[5][:, 0:4], lhsT=tri_b, rhs=ldhl[:, 4:8], start=False, stop=True)
                        e.matmul(P[5][:, 4:8], lhsT=blk_b, rhs=ldhl[:, 0:4], start=True, stop=False)
                        return e.matmul(P[5][:, 4:8], lhsT=blk_b, rhs=ldhl[:, 4:8], start=False, stop=True)
                    S.op("pe", mmg2, r=["Cb", "ldhl"], w=[pk(5)])
                    S.op("act", lambda e: e.activation(out=sm[:, 16:20], in_=P[5][:, 0:4], func=AF.Copy), r=[pk(5)], w=["sm2"])
                    S.op("act", lambda e: e.activation(out=sm[:, 24:28], in_=P[5][:, 0:4], func=AF.Exp), r=[pk(5)], w=["sm2"])
                    S.op("dve", lambda e: e.tensor_scalar(out=sm[:, 20:24], in0=sm[:, 16:20], scalar1=-1.0, scalar2=None, op0=ALU.mult), r=["sm2"], w=["sm2"])
                    S.op("dve", lambda e: e.tensor_tensor(out=sm[:, 28:32], in0=P[5][:, 4:8], in1=sm[:, 16:20], op=ALU.subtract), r=[pk(5), "sm2"], w=["sm2"])
                    S.op("act", lambda e: e.activation(out=sm[:, 28:32], in_=sm[:, 28:32], func=AF.Exp), r=["sm2"], w=["sm2"])
                    S.op("dve", (lambda bet: lambda e: e.tensor_tensor(out=sm[:, 32:36], in0=sm[:, 24:28], in1=bet, op=ALU.mult))(bet), r=["sm2", "beta"], w=["sm2"])
                    S.op("dve", lambda e: e.tensor_scalar(out=sm[:, 36:40], in0=sm[:, 28:32], scalar1=ind[:, 0:1], scalar2=None, op0=ALU.mult), r=["sm2", "C"], w=["sm2"])
                    S.op("dve", lambda e: e.tensor_scalar(out=sm[:, 40:44], in0=sm[:, 28:32], scalar1=ind[:, 1:2], scalar2=None, op0=ALU.mult), r=["sm2", "C"], w=["sm2"])
                    S.op("dve", (lambda bet: lambda e: e.tensor_scalar(out=sm[:, 44:48], in0=bet, scalar1=-1.0, scalar2=None, op0=ALU.mult))(bet), r=["beta"], w=["sm2"])

                    mark(4.6)

                    def bc(col):
                        return sm[:, col:col + 4].unsqueeze(2).to_broadcast([128, 4, 128])
                    PTk = PT[:, 0:512].rearrange("p (h d) -> p h d", h=4)
                    PTv = PT[:, 512:1024].rearrange("p (h d) -> p h d", h=4)
                    S.op("dve", (lambda bet: lambda e: e.tensor_tensor(out=Rr[:, :, 0:128], in0=PTv, in1=bet.unsqueeze(2).to_broadcast([128, 4, 128]), op=ALU.mult))(bet),
                         r=[pk(1), "beta"], w=["Rr"])
                    S.op("dve", lambda e: e.tensor_tensor(out=Rr[:, :, 128:256], in0=PTk, in1=bc(32), op=ALU.mult), r=[pk(1), "sm2"], w=["Rr"])
                    S.op("dve", lambda e: e.tensor_tensor(out=kd0[:], in0=PTk, in1=bc(36), op=ALU.mult), r=[pk(1), "sm2"], w=["kd0"])
                    S.op("dve", lambda e: e.tensor_tensor(out=kd1[:], in0=PTk, in1=bc(40), op=ALU.mult), r=[pk(1), "sm2"], w=["kd1"])
                    mark(5)
                    S.op("dve", lambda e: e.tensor_copy(out=ldB[:], in_=ldhl[:].unsqueeze(2).to_broadcast([128, 8, 128])), r=["ldhl"], w=["ldB"])

                    def mmG(e):
                        for h in range(4):
                            e.matmul(P[4][:, h * 128:(h + 1) * 128], lhsT=ldB[:, h, :], rhs=tri_b, start=True, stop=False)
                            ins = e.matmul(P[4][:, h * 128:(h + 1) * 128], lhsT=ldB[:, 4 + h, :], rhs=tri_b, start=False, stop=True)
                        return ins
                    S.op("pe", mmG, r=["ldB", "Cb"], w=[pk(4)])
                    P4v = P[4][:].rearrange("p (h d) -> p h d", h=4)
                    mark(5.2)
                    S.op("dve", lambda e: e.tensor_tensor(out=Ga[:], in0=P4v, in1=mstrict4.rearrange("p (h d) -> p h d", h=4), op=ALU.add), r=[pk(4), "msk4"], w=["Ga"])
                    S.op("dve", lambda e: e.tensor_tensor(out=Gb[:], in0=P4v, in1=minclT4.rearrange("p (h d) -> p h d", h=4), op=ALU.subtract), r=[pk(4), "msk4"], w=["Gb"])
                    S.op("act", lambda e: e.activation(out=egr[:], in_=P4v, func=AF.Exp), r=[pk(4)], w=["egr"])
                    mark(5.4)
                    for h in range(4):
                        S.op("act", (lambda h: lambda e: e.activation(out=Em[:, h, :], in_=Ga[:, h, :], func=AF.Exp, scale=-1.0, bias=sm[:, 16 + h:17 + h]))(h),
                             r=["Ga", "sm2"], w=["Em"])
                        S.op("act", (lambda h: lambda e: e.activation(out=ETm[:, h, :], in_=Gb[:, h, :], func=AF.Exp, bias=sm[:, 20 + h:21 + h]))(h),
                             r=["Gb", "sm2"], w=["ETm"])

                    mark(5.6)
                    def mmKK(e):
                        for h in range(4):
                            e.matmul(P[2][:, h * 128:(h + 1) * 128], lhsT=kn[:, h, :], rhs=kn[:, h, :], start=True, stop=True)
                        for h in range(4):
                            ins = e.matmul(P[3][:, h * 128:(h + 1) * 128], lhsT=kn[:, h, :], rhs=qn[:, h, :], start=True, stop=True)
                        return ins
                    S.op("pe", mmKK, r=["kn", "qn"], w=[pk(2), pk(3)])
                    P2v = P[2][:].rearrange("p (h d) -> p h d", h=4)
                    P3v = P[3][:].rearrange("p (h d) -> p h d", h=4)
                    tAv = tA[:].rearrange("p (h d) -> p h d", h=4)
                    S.op("dve", lambda e: e.tensor_tensor(out=tAv, in0=P2v, in1=bc(44), op=ALU.mult), r=[pk(2), "sm2"], w=["tA"])
                    S.op("dve", lambda e: e.tensor_tensor(out=Nb[:], in0=tAv, in1=Em[:], op=ALU.mult), r=["tA", "Em"], w=["Nb"])
                    S.op("dve", lambda e: e.tensor_tensor(out=qkT[:], in0=P3v, in1=ETm[:], op=ALU.mult), r=[pk(3), "ETm"], w=["qkT"])

                    def trN(e):
                        for h in range(4):
                            ins = e.transpose(PT[:, h * 128:(h + 1) * 128], Nb[:, h, :], ident_b)
                        return ins
                    S.op("pe", trN, r=["Nb", "Cb"], w=[pk(1)])
                    S.op("act", lambda e: e.activation(out=Mb[:], in_=PTk, func=AF.Copy), r=[pk(1)], w=["Mb"])
                    mark(6)
                    for p in range(6):
                        S.op("act", lambda e: e.activation(out=Rb[:], in_=Rr[:], func=AF.Copy), r=["Rr"], w=["Rb"])

                        def mmR(e):
                            for h in range(4):
                                ins = e.matmul(P[6 + h // 2][:, (h % 2) * 256:(h % 2) * 256 + 256], lhsT=Mb[:, h, :], rhs=Rb[:, h, :], start=True, stop=True)
                            return ins
                        S.op("pe", mmR, r=["Mb", "Rb"], w=[pk(6), pk(7)])
                        S.op("dve", lambda e: e.tensor_tensor(out=Rr[:, 0:2, :], in0=Rr[:, 0:2, :], in1=P[6][:].rearrange("p (h d) -> p h d", h=2), op=ALU.add),
                             r=["Rr", pk(6)], w=["Rr"])
                        S.op("dve", lambda e: e.tensor_tensor(out=Rr[:, 2:4, :], in0=Rr[:, 2:4, :], in1=P[7][:].rearrange("p (h d) -> p h d", h=2), op=ALU.add),
                             r=["Rr", pk(7)], w=["Rr"])
                        if p < 5:
                            def mmSq(e):
                                for h in range(4):
                                    e.matmul(P[4][:, h * 128:(h + 1) * 128], lhsT=Mb[:, h, :], rhs=Nb[:, h, :], start=True, stop=True)
                                for h in range(4):
                                    ins = e.matmul(P[5][:, h * 128:(h + 1) * 128], lhsT=Nb[:, h, :], rhs=Mb[:, h, :], start=True, stop=True)
                                return ins
                            S.op("pe", mmSq, r=["Mb", "Nb"], w=[pk(4), pk(5)])
                            S.op("act", lambda e: e.activation(out=Nb[:], in_=P4v, func=AF.Copy), r=[pk(4)], w=["Nb"])
                            S.op("dve", lambda e: e.tensor_copy(out=Mb[:], in_=P[5][:].rearrange("p (h d) -> p h d", h=4)), r=[pk(5)], w=["Mb"])
                    mark(7)
                    S.op("act", lambda e: e.activation(out=wb[:], in_=Rr[:, :, 128:256], func=AF.Copy), r=["Rr"], w=["wb"])

                    def trW(e):
                        for h in range(4):
                            ins = e.transpose(PT[:, h * 128:(h + 1) * 128], wb[:, h, :], ident_b)
                        return ins
                    S.op("pe", trW, r=["wb", "Cb"], w=[pk(1)])
                    S.op("act", lambda e: e.activation(out=wT[:], in_=PTk, func=AF.Copy), r=[pk(1)], w=["wT"])
                    S.op("dve", lambda e: e.tensor_tensor(out=qd[:], in0=qn[:], in1=egr[:], op=ALU.mult), r=["qn", "egr"], w=["qd"])
                    for ch in range(2):
                        kd = kd0 if ch == 0 else kd1
                        kdk = "kd0" if ch == 0 else "kd1"
                        last = 63 if ch == 0 else 127

                        def mmV(e):
                            for h in range(4):
                                ins = e.matmul(P[2][:, h * 128:(h + 1) * 128], lhsT=wT[:, h, :], rhs=Sb[:, h, :], start=True, stop=True)
                            return ins
                        S.op("pe", mmV, r=["wT", "Sb"], w=[pk(2)])
                        S.op("dve", lambda e: e.tensor_tensor(out=vnb[:], in0=Rr[:, :, 0:128], in1=P2v, op=ALU.subtract), r=["Rr", pk(2)], w=["vnb"])

                        def mmO(e, kd=kd):
                            for h in range(4):
                                e.matmul(P[3][:, h * 128:(h + 1) * 128], lhsT=qd[:, h, :], rhs=Sb[:, h, :], start=True, stop=False)
                                e.matmul(P[3][:, h * 128:(h + 1) * 128], lhsT=qkT[:, h, :], rhs=vnb[:, h, :], start=False, stop=True)
                            for h in range(4):
                                ins = e.matmul(P[4][:, h * 128:(h + 1) * 128], lhsT=kd[:, h, :], rhs=vnb[:, h, :], start=True, stop=True)
                            return ins
                        S.op("pe", mmO, r=["qd", "Sb", "qkT", "vnb", kdk], w=[pk(3), pk(4)])
                        if ch == 0:
                            S.op("dve", lambda e: e.tensor_scalar(out=og[:], in0=P3v, scalar1=ind[:, 0:1], scalar2=None, op0=ALU.mult), r=[pk(3), "C"], w=["og"])
                        else:
                            S.op("dve", lambda e: e.scalar_tensor_tensor(out=og[:], in0=P3v, scalar=ind[:, 1:2], in1=og[:], op0=ALU.mult, op1=ALU.add),
                                 r=[pk(3), "C", "og"], w=["og"])
                        for h in range(4):
                            S.op("dve", (lambda h, last: lambda e: e.scalar_tensor_tensor(out=St[:, h, :], in0=St[:, h, :], scalar=egr[:, h, last:last + 1],
                                                                                          in1=P[4][:, h * 128:(h + 1) * 128], op0=ALU.mult, op1=ALU.add))(h, last),
                                 r=["St", "egr", pk(4)], w=["St"])
                        S.op("act", lambda e: e.activation(out=Sb[:], in_=St[:], func=AF.Copy), r=["St"], w=["Sb"])
                    mark(8)
                    ogf = og[:].rearrange("p h d -> p (h d)")
                    S.op("pool", lambda e: e.tensor_tensor(out=tB[:], in0=ogf, in1=ogf, op=ALU.mult), r=["og"], w=["tB"])
                    S.op("dve", lambda e: e.tensor_reduce(out=sm[:, 48:52], in_=tB[:].rearrange("p (h d) -> p h d", h=4), axis=AX.X, op=ALU.add), r=["tB"], w=["sm3"])
                    S.op("act", lambda e: e.activation(out=sm[:, 48:52], in_=sm[:, 48:52], func=AF.Ln, scale=1.0 / 128.0, bias=1e-6), r=["sm3"], w=["sm3"])
                    S.op("act", lambda e: e.activation(out=sm[:, 48:52], in_=sm[:, 48:52], func=AF.Exp, scale=-0.5), r=["sm3"], w=["sm3"])
                    S.op("dve", lambda e: e.tensor_tensor(out=tAv, in0=og[:], in1=bc(48), op=ALU.mult), r=["og", "sm3"], w=["tA"])
                    S.op("dve", lambda e: e.tensor_tensor(out=tA[:], in0=tA[:], in1=gnw[:], op=ALU.mult), r=["tA", "gnw"], w=["tA"])
                    S.op("dve", (lambda t: lambda e: e.tensor_tensor(out=ogb[:], in0=tA[:], in1=zs[:, t, :], op=ALU.mult))(t), r=["tA", "zs"], w=["ogb"])
                    ti = b * 4 + t
                    S.dma("sp", (lambda ti: lambda e: e.dma_start(out=ogs_d[ti, :, :], in_=ogb[:]))(ti), "ogw", r=["ogb"], w=["ogs%d" % ti])
                    if debug:
                        S.dma("sp", (lambda ti: lambda e: e.dma_start(out=dbg["og"][ti, :, :], in_=og[:].rearrange("p h d -> p (h d)")))(ti), "dbg", r=["og"])

        fence()
        S.enabled = "2" in phases
        with contextlib.ExitStack() as es:
            wq2 = sbt(es, "wq2", [128, 8, 1024], BF16)
            wv2 = sbt(es, "wv2", [128, 8, 512], BF16)
            wo = sbt(es, "wo", [128, 8, 1024], BF16)
            xT2 = sbt(es, "xTb2", [128, 8, 512], BF16)
            KT = sbt(es, "KT", [128, 4, SEQ], BF16)
            Vs = sbt(es, "Vs", [128, 32, 4, 132], BF16)
            pi_ = sbt(es, "pi_", [128, 512], I32)
            pf = sbt(es, "pf", [128, 512])
            ka = sbt(es, "ka", [128, 512])
            ki = sbt(es, "ki", [128, 512], I32)
            cosF = sbt(es, "cosF", [128, 512])
            sinF = sbt(es, "sinF", [128, 512])
            mb = sbt(es, "mb", [128, 512], BF16)
            r1 = sbt(es, "r1", [128, 512])
            r2 = sbt(es, "r2", [128, 512])
            Q0 = sbt(es, "Q0", [128, 4, 512], BF16)
            Q1 = sbt(es, "Q1", [128, 4, 512], BF16)
            pT = [sbt(es, "pT%d" % i, [128, 512], BF16) for i in range(2)]
            den = sbt(es, "den", [128, 16])
            od = sbt(es, "od", [128, 4, 4, 128])
            ot = sbt(es, "ot", [128, 4, 1024], BF16)
            oT = sbt(es, "oT", [128, 8, 128], BF16)
            mixt = sbt(es, "mixt", [128, 1024])
            dnw = sbt(es, "dnw", [128, 512])
            lamv = sbt(es, "lamv", [128, 256])
            lam = sbt(es, "lam", [128, 8])
            PT2 = P[6][:].bitcast(BF16)

            for k in range(8):
                S.dma("pool", (lambda k: lambda e: e.dma_start(out=wq2[:, k, :], in_=wfm_d[k, :, 1536:2560]))(k), "w2", w=["wq2"])
                S.dma("pool", (lambda k: lambda e: e.dma_start(out=wv2[:, k, :], in_=wtm_d[k, :, 512:1024]))(k), "w2", w=["wv2"])
                S.dma("pool", (lambda k: lambda e: e.dma_start(out=wo[:, k, :], in_=wout_d[k, :, :]))(k), "w2", w=["wo"])
            S.dma("sp", lambda e: e.dma_start(out=dnw[:], in_=dnw_d[:, :]), "c2", w=["dnw"])
            S.dma("sp", lambda e: e.dma_start(out=lamv[:], in_=lamv_d[:, :]), "c2", w=["lamv"])
            S.op("dve", lambda e: e.memset(Vs[:].rearrange("p a b c -> p (a b c)"), 1.0), w=["Vs"])
            S.op("dve", lambda e: e.tensor_tensor(out=lamv[:, 0:64], in0=lamv[:, 0:64], in1=lamv[:, 64:128], op=ALU.mult), r=["lamv"], w=["lamv"])
            S.op("dve", lambda e: e.tensor_tensor(out=lamv[:, 128:192], in0=lamv[:, 128:192], in1=lamv[:, 192:256], op=ALU.mult), r=["lamv"], w=["lamv"])
            S.op("dve", lambda e: e.tensor_reduce(out=lam[:, 0:1], in_=lamv[:, 0:64], axis=AX.X, op=ALU.add), r=["lamv"], w=["lam"])
            S.op("dve", lambda e: e.tensor_reduce(out=lam[:, 1:2], in_=lamv[:, 128:192], axis=AX.X, op=ALU.add), r=["lamv"], w=["lam"])
            S.op("act", lambda e: e.activation(out=lam[:, 0:2], in_=lam[:, 0:2], func=AF.Exp), r=["lam"], w=["lam"])
            S.op("dve", lambda e: e.tensor_tensor(out=lam[:, 2:3], in0=lam[:, 1:2], in1=lam[:, 0:1], op=ALU.subtract), r=["lam"], w=["lam"])
            S.op("dve", lambda e: e.tensor_scalar(out=lam[:, 2:3], in0=lam[:, 2:3], scalar1=-LAM_INIT, scalar2=None, op0=ALU.add), r=["lam"], w=["lam"])

            def range_reduce_sin(dst, src_key):
                S.op("dve", lambda e: e.tensor_scalar(out=ki[:], in0=ka[:], scalar1=float(1.0 / (2 * math.pi)), scalar2=None, op0=ALU.mult), r=["ka"], w=["ki"])
                S.op("dve", lambda e: e.tensor_copy(out=r1[:], in_=ki[:]), r=["ki"], w=["r1"])
                S.op("dve", lambda e: e.scalar_tensor_tensor(out=ka[:], in0=r1[:], scalar=float(-2 * math.pi), in1=ka[:], op0=ALU.mult, op1=ALU.add), r=["r1", "ka"], w=["ka"])
                S.op("dve", lambda e: e.tensor_scalar(out=r1[:], in0=ka[:], scalar1=float(math.pi), scalar2=float(-2 * math.pi), op0=ALU.is_gt, op1=ALU.mult), r=["ka"], w=["r1"])
                S.op("dve", lambda e: e.tensor_tensor(out=ka[:], in0=ka[:], in1=r1[:], op=ALU.add), r=["ka", "r1"], w=["ka"])
                S.op("act", lambda e: e.activation(out=dst[:], in_=ka[:], func=AF.Sin), r=["ka"], w=[src_key])

            for b in range(nblk):
                t0 = b * 512
                S.dma("pool", (lambda t0: lambda e: e.dma_start(out=xT2[:], in_=xT[:, :, t0:t0 + 512].rearrange("k p t -> p k t")))(t0), "x2", w=["xTb2"])
                S.dma("sp", (lambda t0: lambda e: e.dma_start(out=pi_[:], in_=posb[:, t0:t0 + 512]))(t0), "pos", w=["pi_"])
                S.op("dve", lambda e: e.tensor_copy(out=pf[:], in_=pi_[:]), r=["pi_"], w=["pf"])
                S.op("dve", lambda e: e.tensor_scalar(out=pf[:], in0=pf[:], scalar1=freq, scalar2=None, op0=ALU.mult), r=["pf", "C"], w=["pf"])
                S.op("dve", lambda e: e.tensor_copy(out=ka[:], in_=pf[:]), r=["pf"], w=["ka"])
                range_reduce_sin(sinF, "sinF")
                S.op("dve", lambda e: e.tensor_scalar(out=ka[:], in0=pf[:], scalar1=float(math.pi / 2), scalar2=None, op0=ALU.add), r=["pf"], w=["ka"])
                range_reduce_sin(cosF, "cosF")
                for which in range(2):
                    for h in range(4):
                        col = which * 512 + h * 128

                        def mm(e, col=col):
                            for k in range(8):
                                ins = e.matmul(P[0][:], lhsT=wq2[:, k, col:col + 128], rhs=xT2[:, k, :], start=(k == 0), stop=(k == 7))
                            return ins
                        S.op("pe", mm, r=["wq2", "xTb2"], w=[pk(0)])
                        S.op("act", lambda e: e.activation(out=mb[:], in_=P[0][:], func=AF.Copy), r=[pk(0)], w=["mb"])
                        S.op("pe", lambda e: e.matmul(P[1][:], lhsT=perm_b, rhs=mb[:], start=True, stop=True), r=["mb", "Cb"], w=[pk(1)])
                        S.op("dve", lambda e: e.tensor_tensor(out=r1[:], in0=mb[:], in1=cosF[:], op=ALU.mult), r=["mb", "cosF"], w=["r1"])
                        S.op("dve", lambda e: e.tensor_tensor(out=r2[:], in0=P[1][:], in1=sinF[:], op=ALU.mult), r=[pk(1), "sinF"], w=["r2"])
                        if which == 0:
                            S.op("dve", lambda e: e.tensor_tensor(out=r1[:], in0=r1[:], in1=r2[:], op=ALU.add), r=["r1", "r2"], w=["r1"])
                            S.op("dve", (lambda h: lambda e: e.tensor_scalar(out=Q0[:, h, :], in0=r1[:], scalar1=c0, scalar2=None, op0=ALU.mult))(h), r=["r1", "C"], w=["Q0"])
                            S.op("dve", (lambda h: lambda e: e.tensor_scalar(out=Q1[:, h, :], in0=r1[:], scalar1=c1, scalar2=None, op0=ALU.mult))(h), r=["r1", "C"], w=["Q1"])
                        else:
                            S.op("dve", (lambda h, t0: lambda e: e.tensor_tensor(out=KT[:, h, t0:t0 + 512], in0=r1[:], in1=r2[:], op=ALU.add))(h, t0), r=["r1", "r2"], w=["KT"])
                for t in range(4):
                    c0_ = t * 128
                    ti = b * 4 + t

                    def mmv(e, c0_=c0_):
                        for k in range(8):
                            ins = e.matmul(P[0][:], lhsT=xT2[:, k, c0_:c0_ + 128], rhs=wv2[:, k, :], start=(k == 0), stop=(k == 7))
                        return ins
                    S.op("pe", mmv, r=["wv2", "xTb2"], w=[pk(0)])
                    S.op("act", (lambda ti: lambda e: e.activation(out=Vs[:, ti, :, 0:128], in_=P[0][:].rearrange("p (h d) -> p h d", h=4), func=AF.Copy))(ti),
                         r=[pk(0)], w=["Vs"])
                nkt = 4 * b + 4
                it = 0
                for h in range(4):
                    for c in range(2):
                        Qc = Q0 if c == 0 else Q1
                        qk_ = "Q0" if c == 0 else "Q1"
                        accb = (2, 3) if c == 0 else (4, 5)
                        for kt in range(nkt):
                            j = kt - 4 * b
                            q0 = 128 * j if j >= 0 else 0
                            sp_ = it % 2
                            it += 1
                            S.op("pe", (lambda sp_, h, kt, q0, Qc: lambda e: e.matmul(P[sp_][:, q0:512], lhsT=KT[:, h, kt * 128:(kt + 1) * 128], rhs=Qc[:, h, q0:512],
                                                                                      start=True, stop=True))(sp_, h, kt, q0, Qc), r=["KT", qk_], w=[pk(sp_)])
                            S.op("act", (lambda sp_, q0: lambda e: e.activation(out=pT[sp_][:, q0:512], in_=P[sp_][:, q0:512], func=AF.Exp, scale=0.125))(sp_, q0),
                                 r=[pk(sp_)], w=["pT%d" % sp_])
                            if j >= 0:
                                S.op("pool", (lambda sp_, q0: lambda e: e.tensor_tensor(out=pT[sp_][:, q0:q0 + 128], in0=pT[sp_][:, q0:q0 + 128], in1=causal_b, op=ALU.mult))(sp_, q0),
                                     r=["pT%d" % sp_, "Cb"], w=["pT%d" % sp_])

                            def mmpv(e, sp_=sp_, h=h, kt=kt, j=j, accb=accb, b=b):
                                ins = None
                                for qi in range(max(j, 0), 4):
                                    bank = P[accb[qi // 2]]
                                    o0 = (qi % 2) * 256
                                    ins = e.matmul(bank[:, o0:o0 + 129], lhsT=pT[sp_][:, qi * 128:(qi + 1) * 128], rhs=Vs[:, kt, h, 0:129],
                                                   start=(kt == 0 and qi % 2 == 0), stop=(kt == 4 * b + qi), skip_group_check=True)
                                return ins
                            S.op("pe", mmpv, r=["pT%d" % sp_, "Vs"], w=[pk(accb[0]), pk(accb[1])])
                    for qi in range(4):
                        o0 = (qi % 2) * 256
                        bA = P[2 + qi // 2]
                        bB = P[4 + qi // 2]
                        S.op("dve", (lambda qi, bA, o0: lambda e: e.tensor_copy(out=den[:, qi:qi + 1], in_=bA[:, o0 + 128:o0 + 129]))(qi, bA, o0), r=[pk(2 + qi // 2)], w=["den"])
                        S.op("dve", (lambda qi, bB, o0: lambda e: e.tensor_copy(out=den[:, 4 + qi:5 + qi], in_=bB[:, o0 + 128:o0 + 129]))(qi, bB, o0), r=[pk(4 + qi // 2)], w=["den"])
                    S.op("dve", lambda e: e.reciprocal(out=den[:, 8:16], in_=den[:, 0:8]), r=["den"], w=["den"])
                    S.op("dve", lambda e: e.tensor_scalar(out=den[:, 12:16], in0=den[:, 12:16], scalar1=lam[:, 2:3], scalar2=None, op0=ALU.mult), r=["den", "lam"], w=["den"])
                    for qi in range(4):
                        o0 = (qi % 2) * 256
                        bA = P[2 + qi // 2]
                        bB = P[4 + qi // 2]
                        S.op("dve", (lambda qi, bA, o0, h: lambda e: e.tensor_scalar(out=od[:, qi, h, :], in0=bA[:, o0:o0 + 128], scalar1=den[:, 8 + qi:9 + qi], scalar2=None,
                                                                                      op0=ALU.mult))(qi, bA, o0, h), r=[pk(2 + qi // 2), "den"], w=["od"])
                        S.op("dve", (lambda qi, bB, o0, h: lambda e: e.scalar_tensor_tensor(out=od[:, qi, h, :], in0=bB[:, o0:o0 + 128], scalar=den[:, 12 + qi:13 + qi],
                                                                                             in1=od[:, qi, h, :], op0=ALU.mult, op1=ALU.add))(qi, bB, o0, h),
                             r=[pk(4 + qi // 2), "den", "od"], w=["od"])
                for qi in range(4):
                    ti = b * 4 + qi
                    odf = od[:, qi, :, :].rearrange("p h d -> p (h d)")
                    S.op("pool", (lambda odf: lambda e: e.tensor_tensor(out=r1[:], in0=odf, in1=odf, op=ALU.mult))(odf), r=["od"], w=["r1"])
                    S.op("dve", lambda e: e.tensor_reduce(out=lam[:, 4:8], in_=r1[:].rearrange("p (h d) -> p h d", h=4), axis=AX.X, op=ALU.add), r=["r1"], w=["lam2"])
                    S.op("act", lambda e: e.activation(out=lam[:, 4:8], in_=lam[:, 4:8], func=AF.Ln, scale=1.0 / 128.0, bias=1e-6), r=["lam2"], w=["lam2"])
                    S.op("act", lambda e: e.activation(out=lam[:, 4:8], in_=lam[:, 4:8], func=AF.Exp, scale=-0.5), r=["lam2"], w=["lam2"])
                    S.op("dve", (lambda qi: lambda e: e.tensor_tensor(out=r1[:].rearrange("p (h d) -> p h d", h=4), in0=od[:, qi, :, :],
                                                                      in1=lam[:, 4:8].unsqueeze(2).to_broadcast([128, 4, 128]), op=ALU.mult))(qi), r=["od", "lam2"], w=["r1"])
                    S.op("dve", (lambda qi: lambda e: e.scalar_tensor_tensor(out=ot[:, qi, 512:1024], in0=r1[:], scalar=float(1.0 - LAM_INIT), in1=dnw[:],
                                                                             op0=ALU.mult, op1=ALU.mult))(qi), r=["r1", "dnw"], w=["ot%d" % qi])
                    S.dma("sp", (lambda qi, ti: lambda e: e.dma_start(out=ot[:, qi, 0:512], in_=ogs_d[ti, :, :]))(qi, ti), "ogr", r=["ogs%d" % ti], w=["ot%d" % qi])

                    def trO(e, qi=qi):
                        for k in range(8):
                            ins = e.transpose(PT2[:, k * 128:(k + 1) * 128], ot[:, qi, k * 128:(k + 1) * 128], ident_b)
                        return ins
                    S.op("pe", trO, r=["ot%d" % qi, "Cb"], w=[pk(6)])
                    S.op("act", lambda e: e.activation(out=oT[:], in_=PT2[:, :].rearrange("p (k t) -> p k t", k=8), func=AF.Copy), r=[pk(6)], w=["oT"])
                    for half in range(2):
                        def mmo(e, half=half):
                            for k in range(8):
                                ins = e.matmul(P[6 + half][:], lhsT=oT[:, k, :], rhs=wo[:, k, half * 512:(half + 1) * 512], start=(k == 0), stop=(k == 7))
                            return ins
                        S.op("pe", mmo, r=["oT", "wo"], w=[pk(6 + half)])
                        S.op("act", (lambda half: lambda e: e.activation(out=mixt[:, half * 512:(half + 1) * 512], in_=P[6 + half][:], func=AF.Copy))(half),
                             r=[pk(6 + half)], w=["mixt"])
                    S.dma("sp", (lambda ti: lambda e: e.dma_start(out=mixs_d[ti, :, :], in_=mixt[:]))(ti), "mixw", r=["mixt"], w=["mixs%d" % ti])
                    if debug:
                        S.dma("sp", (lambda ti: lambda e: e.dma_start(out=dbg["mix"][ti, :, :], in_=mixt[:]))(ti), "dbg", r=["mixt"])

        fence()
        S.enabled = "C" in phases
        with contextlib.ExitStack() as es:
            acc = sbt(es, "acc", [128, 16, 1024])
            h1T = sbt(es, "h1T", [128, 8, 2048], BF16)
            gates = sbt(es, "gates", [128, 16, 32])
            m01 = sbt(es, "m01", [128, 2])
            rbb = sbt(es, "rbb", [128, 32])
            bgu = sbt(es, "bgu", [128, 512])
            st = sbt(es, "st", [128, 32])
            S.dma("sp", lambda e: e.dma_start(out=m01[:], in_=m01_d[:, :]), "c3", w=["m01"])
            S.dma("sp", lambda e: e.dma_start(out=rbb[:], in_=rbb_d[:, :]), "c3", w=["rbb"])
            S.dma("sp", lambda e: e.dma_start(out=bgu[:], in_=bgu_d[:, :]), "c3", w=["bgu"])

            def layer_norm(src, srck, dst, dstk, gam, bet, gk, scr, scrk):
                S.op("dve", lambda e: e.tensor_reduce(out=st[:, 0:1], in_=src, axis=AX.X, op=ALU.add), r=[srck], w=["st"])
                S.op("act", lambda e: e.activation(out=scr, in_=src, func=AF.Square), r=[srck], w=[scrk])
                S.op("dve", lambda e: e.tensor_reduce(out=st[:, 1:2], in_=scr, axis=AX.X, op=ALU.add), r=[scrk], w=["st"])
                S.op("dve", lambda e: e.tensor_scalar(out=st[:, 0:2], in0=st[:, 0:2], scalar1=1.0 / 1024.0, scalar2=None, op0=ALU.mult), r=["st"], w=["st"])
                S.op("dve", lambda e: e.tensor_tensor(out=st[:, 2:3], in0=st[:, 0:1], in1=st[:, 0:1], op=ALU.mult), r=["st"], w=["st"])
                S.op("dve", lambda e: e.tensor_tensor(out=st[:, 2:3], in0=st[:, 1:2], in1=st[:, 2:3], op=ALU.subtract), r=["st"], w=["st"])
                S.op("act", lambda e: e.activation(out=st[:, 2:3], in_=st[:, 2:3], func=AF.Ln, bias=1e-5), r=["st"], w=["st"])
                S.op("act", lambda e: e.activation(out=st[:, 2:3], in_=st[:, 2:3], func=AF.Exp, scale=-0.5), r=["st"], w=["st"])
                S.op("dve", lambda e: e.tensor_scalar(out=dst, in0=src, scalar1=st[:, 0:1], scalar2=st[:, 2:3], op0=ALU.subtract, op1=ALU.mult), r=[srck, "st"], w=[dstk])
                S.op("dve", lambda e: e.tensor_tensor(out=dst, in0=dst, in1=gam, op=ALU.mult), r=[dstk, gk], w=[dstk])
                S.op("dve", lambda e: e.tensor_tensor(out=dst, in0=dst, in1=bet, op=ALU.add), r=[dstk, gk], w=[dstk])

            with contextlib.ExitStack() as es2:
                lng = sbt(es2, "lng", [128, 1024])
                lnb = sbt(es2, "lnb", [128, 1024])
                rw = sbt(es2, "rw", [128, 8, 32])
                rwh = sbt(es2, "rwh", [128, 8, 32], BF16)
                rwl = sbt(es2, "rwl", [128, 8, 32], BF16)
                bdn = sbt(es2, "bdn", [128, 1024])
                bdh = sbt(es2, "bdh", [128, 1024], BF16)
                bdl = sbt(es2, "bdl", [128, 1024], BF16)
                h1hl = sbt(es2, "h1hl", [128, 2, 1024], BF16)
                hTl = sbt(es2, "hTl", [128, 8, 128], BF16)
                gpad = sbt(es2, "gpad", [128, 2, 128], BF16)
                xo = sbt(es2, "xo", [128, 1024])
                mA = sbt(es2, "mA", [128, 1024])
                mB = sbt(es2, "mB", [128, 1024])
                h1 = sbt(es2, "h1", [128, 1024])
                scr = sbt(es2, "scr", [128, 1024])
                gT = sbt(es2, "gT", [128, 2, 128], BF16)
                lg = sbt(es2, "lg", [128, 32])
                S.dma("sp", lambda e: e.dma_start(out=lng[:], in_=lnp_d[0, :, :]), "c3", w=["lng"])
                S.dma("sp", lambda e: e.dma_start(out=lnb[:], in_=lnp_d[1, :, :]), "c3", w=["lng"])
                S.dma("sp", lambda e: e.dma_start(out=rw[:], in_=rw_d[:, :, :].rearrange("k p n -> p k n")), "c3", w=["rw"])
                S.dma("sp", lambda e: e.dma_start(out=bdn[:], in_=bdn_d[:, :]), "c3", w=["bdn"])
                S.op("act", lambda e: e.activation(out=rwh[:], in_=rw[:], func=AF.Copy), r=["rw"], w=["rwh"])
                S.op("dve", lambda e: e.tensor_tensor(out=rwl[:], in0=rw[:], in1=rwh[:], op=ALU.subtract), r=["rw", "rwh"], w=["rwl"])
                S.op("act", lambda e: e.activation(out=bdh[:], in_=bdn[:], func=AF.Copy), r=["bdn"], w=["bdh"])
                S.op("dve", lambda e: e.tensor_tensor(out=bdl[:], in0=bdn[:], in1=bdh[:], op=ALU.subtract), r=["bdn", "bdh"], w=["bdl"])
                S.op("dve", lambda e: e.memset(gpad[:].rearrange("p a b -> p (a b)"), 0.0), w=["gpad"])
                for j in range(16):
                    S.dma("sp", (lambda j: lambda e: e.dma_start(out=xo[:], in_=xown_d[j, :, :]))(j), "ldx", w=["xo"])
                    S.dma("sp", (lambda j: lambda e: e.dma_start(out=mA[:], in_=mixs_d[j, :, :]))(j), "ldx", r=["mixs%d" % j], w=["mA"])
                    S.dma("sp", (lambda j: lambda e: e.dma_start(out=mB[:], in_=mixs_d[j + 16, :, :]))(j), "ldx", r=["mixs%d" % (j + 16)], w=["mB"])
                    S.op("dve", lambda e: e.tensor_scalar(out=xo[:], in0=xo[:], scalar1=float(DN_ALPHA), scalar2=None, op0=ALU.mult), r=["xo"], w=["xo"])
                    S.op("dve", lambda e: e.scalar_tensor_tensor(out=xo[:], in0=mA[:], scalar=m01[:, 0:1], in1=xo[:], op0=ALU.mult, op1=ALU.add), r=["mA", "m01", "xo"], w=["xo"])
                    S.op("dve", lambda e: e.scalar_tensor_tensor(out=xo[:], in0=mB[:], scalar=m01[:, 1:2], in1=xo[:], op0=ALU.mult, op1=ALU.add), r=["mB", "m01", "xo"], w=["xo"])
                    layer_norm(xo[:], "xo", h1[:], "h1", lng[:], lnb[:], "lng", scr[:], "scr")
                    if debug:
                        S.dma("sp", (lambda j: lambda e: e.dma_start(out=dbg["h1"][j, :, :], in_=h1[:]))(j), "dbg", r=["h1"])

                    S.op("act", lambda e: e.activation(out=h1hl[:, 0, :], in_=h1[:], func=AF.Copy), r=["h1"], w=["h1hl"])
                    S.op("dve", lambda e: e.tensor_tensor(out=h1hl[:, 1, :], in0=h1[:], in1=h1hl[:, 0, :], op=ALU.subtract), r=["h1", "h1hl"], w=["h1hl"])
                    P0T = P[0][:].bitcast(BF16)
                    P1T = P[1][:].bitcast(BF16)

                    def trh(e):
                        for k in range(8):
                            e.transpose(P0T[:, k * 128:(k + 1) * 128], h1hl[:, 0, k * 128:(k + 1) * 128], ident_b)
                        for k in range(8):
                            ins = e.transpose(P1T[:, k * 128:(k + 1) * 128], h1hl[:, 1, k * 128:(k + 1) * 128], ident_b)
                        return ins
                    S.op("pe", trh, r=["h1hl", "Cb"], w=[pk(0), pk(1)])
                    S.op("act", (lambda j: lambda e: e.activation(out=h1T[:, :, j * 128:(j + 1) * 128], in_=P0T[:, :].rearrange("p (k t) -> p k t", k=8), func=AF.Copy))(j),
                         r=[pk(0)], w=["h1T"])
                    S.op("dve", lambda e: e.tensor_copy(out=hTl[:], in_=P1T[:, :].rearrange("p (k t) -> p k t", k=8)), r=[pk(1)], w=["hTl"])

                    def mmr(e, j=j):
                        n = 0
                        for k in range(8):
                            for (a, bb) in ((h1T[:, k, j * 128:(j + 1) * 128], rwh[:, k, :]), (hTl[:, k, :], rwh[:, k, :]), (h1T[:, k, j * 128:(j + 1) * 128], rwl[:, k, :])):
                                ins = e.matmul(P[2][:, 0:32], lhsT=a, rhs=bb, start=(n == 0), stop=(n == 23))
                                n += 1
                        return ins
                    S.op("pe", mmr, r=["h1T", "hTl", "rwh", "rwl"], w=[pk(2)])
                    S.op("dve", lambda e: e.tensor_tensor(out=lg[:], in0=P[2][:, 0:32], in1=rbb[:], op=ALU.add), r=[pk(2), "rbb"], w=["lg"])
                    S.op("dve", lambda e: e.max(out=st[:, 8:16], in_=lg[:]), r=["lg"], w=["st2"])
                    S.op("dve", lambda e: e.tensor_scalar(out=st[:, 16:17], in0=st[:, 8:9], scalar1=-1.0, scalar2=None, op0=ALU.mult), r=["st2"], w=["st2"])
                    S.op("act", lambda e: e.activation(out=scr[:, 0:32], in_=lg[:], func=AF.Exp, bias=st[:, 16:17]), r=["lg", "st2"], w=["scr"])
                    S.op("dve", lambda e: e.tensor_scalar(out=lg[:], in0=lg[:], scalar1=st[:, 11:12], scalar2=None, op0=ALU.is_ge), r=["lg", "st2"], w=["lg"])
                    S.op("dve", lambda e: e.tensor_tensor(out=lg[:], in0=lg[:], in1=scr[:, 0:32], op=ALU.mult), r=["lg", "scr"], w=["lg"])
                    S.op("dve", lambda e: e.tensor_reduce(out=st[:, 17:18], in_=lg[:], axis=AX.X, op=ALU.add), r=["lg"], w=["st2"])
                    S.op("dve", lambda e: e.reciprocal(out=st[:, 17:18], in_=st[:, 17:18]), r=["st2"], w=["st2"])
                    S.op("dve", (lambda j: lambda e: e.tensor_scalar(out=gates[:, j, :], in0=lg[:], scalar1=st[:, 17:18], scalar2=None, op0=ALU.mult))(j), r=["lg", "st2"], w=["gates"])
                    S.op("act", (lambda j: lambda e: e.activation(out=gpad[:, 0, 0:32], in_=gates[:, j, :], func=AF.Copy))(j), r=["gates"], w=["gpad"])
                    S.op("dve", (lambda j: lambda e: e.tensor_tensor(out=gpad[:, 1, 0:32], in0=gates[:, j, :], in1=gpad[:, 0, 0:32], op=ALU.subtract))(j), r=["gates", "gpad"], w=["gpad"])
                    P3T = P[3][:].bitcast(BF16)

                    def trg(e):
                        e.transpose(P3T[:, 0:128], gpad[:, 0, :], ident_b)
                        return e.transpose(P3T[:, 128:256], gpad[:, 1, :], ident_b)
                    S.op("pe", trg, r=["gpad", "Cb"], w=[pk(3)])
                    S.op("act", lambda e: e.activation(out=gT[:], in_=P3T[:, 0:256].rearrange("p (a t) -> p a t", a=2), func=AF.Copy), r=[pk(3)], w=["gT"])
                    for half in range(2):
                        def mmbd(e, half=half):
                            hs_ = slice(half * 512, (half + 1) * 512)
                            e.matmul(P[4 + half][:], lhsT=gT[:, 0, :], rhs=bdh[:, hs_], start=True, stop=False)
                            e.matmul(P[4 + half][:], lhsT=gT[:, 1, :], rhs=bdh[:, hs_], start=False, stop=False)
                            return e.matmul(P[4 + half][:], lhsT=gT[:, 0, :], rhs=bdl[:, hs_], start=False, stop=True)
                        S.op("pe", mmbd, r=["gT", "bdh", "bdl"], w=[pk(4 + half)])
                        S.op("dve", (lambda half, j: lambda e: e.scalar_tensor_tensor(out=acc[:, j, half * 512:(half + 1) * 512], in0=h1[:, half * 512:(half + 1) * 512],
                                                                                        scalar=float(DN_ALPHA), in1=P[4 + half][:], op0=ALU.mult, op1=ALU.add))(half, j),
                             r=["h1", pk(4 + half)], w=["acc%d" % j])

            fence()
            S.enabled = "D" in phases
            with contextlib.ExitStack() as es2:
                wg = [sbt(es2, "ewg%d" % i, [128, 8, 1024], BF16) for i in range(2)]
                wu = [sbt(es2, "ewu%d" % i, [128, 8, 1024], BF16) for i in range(2)]
                wd = sbt(es2, "wd", [128, 8, 1024], BF16)
                aT = [sbt(es2, "aT%d" % i, [128, 8, 512], BF16) for i in range(2)]
                gsb = sbt(es2, "gsb", [128, 512])
                usb = sbt(es2, "usb", [128, 512], BF16)
                sgs = sbt(es2, "sgs", [128, 512], BF16)
                ucl = sbt(es2, "ucl", [128, 512], BF16)
                it = 0
                def load_gu(ex):
                    wb_ = ex % 2
                    for k in range(8):
                        S.dma("pool", (lambda ex, k, wb_: lambda e: e.dma_start(out=wg[wb_][:, k, :], in_=wgu_d[ex, k, :, 0:1024]))(ex, k, wb_), "wg%d" % wb_, w=["wg%d" % wb_])
                        S.dma("pool", (lambda ex, k, wb_: lambda e: e.dma_start(out=wu[wb_][:, k, :], in_=wgu_d[ex, k, :, 1024:2048]))(ex, k, wb_), "wu%d" % wb_, w=["wu%d" % wb_])

                def load_d(ex):
                    for k in range(8):
                        S.dma("pool", (lambda ex, k: lambda e: e.dma_start(out=wd[:, k, :], in_=wdn_d[ex, k, :, :]))(ex, k), "wd", w=["wd"])
                load_gu(0)
                load_d(0)
                for ex in range(32):
                    wb_ = ex % 2
                    if ex + 1 < 32:
                        load_gu(ex + 1)
                    for tb in range(4):
                        ab = it % 2
                        it += 1
                        for fc in range(8):
                            pg_, pu_ = (0, 1) if fc % 2 == 0 else (2, 3)

                            def mmg(e, fc=fc, tb=tb, wb_=wb_, pg_=pg_, pu_=pu_):
                                for k in range(8):
                                    e.matmul(P[pg_][:], lhsT=wg[wb_][:, k, fc * 128:(fc + 1) * 128], rhs=h1T[:, k, tb * 512:(tb + 1) * 512], start=(k == 0), stop=(k == 7))
                                for k in range(8):
                                    ins = e.matmul(P[pu_][:], lhsT=wu[wb_][:, k, fc * 128:(fc + 1) * 128], rhs=h1T[:, k, tb * 512:(tb + 1) * 512], start=(k == 0), stop=(k == 7))
                                return ins
                            S.op("pe", mmg, r=["wg%d" % wb_, "wu%d" % wb_, "h1T"], w=[pk(pg_), pk(pu_)])
                            bgc = bgu[:, ex * 16 + fc:ex * 16 + fc + 1]
                            buc = bgu[:, ex * 16 + 8 + fc:ex * 16 + 8 + fc + 1]
                            S.op("dve", (lambda pg_, bgc: lambda e: e.tensor_scalar(out=gsb[:], in0=P[pg_][:], scalar1=bgc, scalar2=7.0, op0=ALU.add, op1=ALU.min))(pg_, bgc),
                                 r=[pk(pg_), "bgu"], w=["gsb"])
                            S.op("act", (lambda pu_, buc: lambda e: e.activation(out=usb[:], in_=P[pu_][:], func=AF.Identity, bias=buc))(pu_, buc), r=[pk(pu_), "bgu"], w=["usb"])
                            S.op("act", lambda e: e.activation(out=sgs[:], in_=gsb[:], func=AF.Sigmoid, scale=1.702), r=["gsb"], w=["sgs"])
                            S.op("dve", lambda e: e.tensor_scalar(out=ucl[:], in0=usb[:], scalar1=7.0, scalar2=-7.0, op0=ALU.min, op1=ALU.max), r=["usb"], w=["ucl"])
                            S.op("dve", lambda e: e.tensor_tensor(out=gsb[:], in0=gsb[:], in1=sgs[:], op=ALU.mult), r=["gsb", "sgs"], w=["gsb"])
                            S.op("dve", (lambda ab, fc: lambda e: e.scalar_tensor_tensor(out=aT[ab][:, fc, :], in0=ucl[:], scalar=1.0, in1=gsb[:], op0=ALU.add, op1=ALU.mult))(ab, fc),
                                 r=["ucl", "gsb"], w=["aT%d" % ab])
                        for tt in range(4):
                            j = tb * 4 + tt
                            for half in range(2):
                                pb = 4 + half + 2 * (tt % 2)

                                def mmd(e, ab=ab, tt=tt, half=half, pb=pb):
                                    for fc in range(8):
                                        ins = e.matmul(P[pb][:], lhsT=aT[ab][:, fc, tt * 128:(tt + 1) * 128], rhs=wd[:, fc, half * 512:(half + 1) * 512], start=(fc == 0), stop=(fc == 7))
                                    return ins
                                S.op("pe", mmd, r=["aT%d" % ab, "wd"], w=[pk(pb)])
                                S.op("dve", (lambda j, half, pb, ex: lambda e: e.scalar_tensor_tensor(out=acc[:, j, half * 512:(half + 1) * 512], in0=P[pb][:],
                                                                                                       scalar=gates[:, j, ex:ex + 1], in1=acc[:, j, half * 512:(half + 1) * 512],
                                                                                                       op0=ALU.mult, op1=ALU.add))(j, half, pb, ex),
                                     r=[pk(pb), "gates", "acc%d" % j], w=["acc%d" % j])
                    if ex + 1 < 32:
                        load_d(ex + 1)

            fence()
            S.enabled = "E" in phases
            with contextlib.ExitStack() as es2:
                l2g = sbt(es2, "l2g", [128, 1024])
                l2b = sbt(es2, "l2b", [128, 1024])
                l3g = sbt(es2, "l3g", [128, 1024])
                l3b = sbt(es2, "l3b", [128, 1024])
                pgb = sbt(es2, "pgb", [128, 1024])
                wpg = sbt(es2, "wpg", [128, 8, 1024], BF16)
                plw = sbt(es2, "plw", [128, 2, 1024], BF16)
                ptn = sbt(es2, "ptn", [128, 2, 2048], BF16)
                h2 = sbt(es2, "h2", [128, 1024])
                h2b = sbt(es2, "h2b", [128, 1024], BF16)
                h2T = sbt(es2, "h2T", [128, 8, 128], BF16)
                scrE = sbt(es2, "scr2", [128, 1024])
                gt = sbt(es2, "gt", [128, 1024])
                y3 = sbt(es2, "y3", [128, 1024])
                PT3 = P[0][:].bitcast(BF16)
                S.dma("sp", lambda e: e.dma_start(out=l2g[:], in_=lnp_d[2, :, :]), "c4", w=["l2"])
                S.dma("sp", lambda e: e.dma_start(out=l2b[:], in_=lnp_d[3, :, :]), "c4", w=["l2"])
                S.dma("sp", lambda e: e.dma_start(out=l3g[:], in_=lnp_d[4, :, :]), "c4", w=["l3"])
                S.dma("sp", lambda e: e.dma_start(out=l3b[:], in_=lnp_d[5, :, :]), "c4", w=["l3"])
                S.dma("sp", lambda e: e.dma_start(out=pgb[:], in_=pgb_d[:, :]), "c4", w=["pgb"])
                for k in range(8):
                    S.dma("pool", (lambda k: lambda e: e.dma_start(out=wpg[:, k, :], in_=wpg_d[k, :, :]))(k), "w4", w=["wpg"])
                for k in range(2):
                    S.dma("pool", (lambda k: lambda e: e.dma_start(out=plw[:, k, :], in_=plew_d[k, :, :]))(k), "w4", w=["plw"])
                    S.dma("pool", (lambda k: lambda e: e.dma_start(out=ptn[:, k, :], in_=ptown_d[k, :, :]))(k), "w4", w=["ptn"])
                for j in range(16):
                    if debug:
                        S.dma("sp", (lambda j: lambda e: e.dma_start(out=dbg["acc"][j, :, :], in_=acc[:, j, :]))(j), "dbg", r=["acc%d" % j])
                    layer_norm(acc[:, j, :], "acc%d" % j, h2[:], "h2", l2g[:], l2b[:], "l2", scrE[:], "scr2")
                    S.op("act", lambda e: e.activation(out=h2b[:], in_=h2[:], func=AF.Copy), r=["h2"], w=["h2b"])

                    def trh2(e):
                        for k in range(8):
                            ins = e.transpose(PT3[:, k * 128:(k + 1) * 128], h2b[:, k * 128:(k + 1) * 128], ident_b)
                        return ins
                    S.op("pe", trh2, r=["h2b", "Cb"], w=[pk(0)])
                    S.op("act", lambda e: e.activation(out=h2T[:], in_=PT3[:, :].rearrange("p (k t) -> p k t", k=8), func=AF.Copy), r=[pk(0)], w=["h2T"])
                    for half in range(2):
                        hs_ = slice(half * 512, (half + 1) * 512)

                        def mmgt(e, hs_=hs_, half=half):
                            for k in range(8):
                                ins = e.matmul(P[2 + half][:], lhsT=h2T[:, k, :], rhs=wpg[:, k, hs_], start=(k == 0), stop=(k == 7))
                            return ins
                        S.op("pe", mmgt, r=["h2T", "wpg"], w=[pk(2 + half)])

                        def mmpl(e, hs_=hs_, half=half, j=j):
                            for k in range(2):
                                ins = e.matmul(P[4 + half][:], lhsT=ptn[:, k, j * 128:(j + 1) * 128], rhs=plw[:, k, hs_], start=(k == 0), stop=(k == 1))
                            return ins
                        S.op("pe", mmpl, r=["ptn", "plw"], w=[pk(4 + half)])
                        S.op("dve", (lambda hs_, half: lambda e: e.tensor_tensor(out=gt[:, hs_], in0=P[2 + half][:], in1=pgb[:, hs_], op=ALU.add))(hs_, half),
                             r=[pk(2 + half), "pgb"], w=["gt"])
                        S.op("act", (lambda hs_: lambda e: e.activation(out=gt[:, hs_], in_=gt[:, hs_], func=AF.Sigmoid))(hs_), r=["gt"], w=["gt"])
                        S.op("dve", (lambda hs_, half: lambda e: e.tensor_tensor(out=gt[:, hs_], in0=P[4 + half][:], in1=gt[:, hs_], op=ALU.mult))(hs_, half),
                             r=[pk(4 + half), "gt"], w=["gt"])
                    S.op("dve", lambda e: e.scalar_tensor_tensor(out=y3[:], in0=h2[:], scalar=float(DN_ALPHA), in1=gt[:], op0=ALU.mult, op1=ALU.add), r=["h2", "gt"], w=["y3"])
                    layer_norm(y3[:], "y3", y3[:], "y3", l3g[:], l3b[:], "l3", scrE[:], "scr2")
                    S.dma("sp", (lambda j: lambda e: e.dma_start(out=out_d[j, :, :], in_=y3[:]))(j), "outw", r=["y3"])

        S.emit(final_wait_slots=[x for x in ["outw", "dbg", "ogw", "mixw"] if x in S.dma_slots])
    return nc


def _consts():
    c = np.zeros((128, NCONST), np.float32)
    i = np.arange(128)
    same = (i[:, None] // 64) == (i[None, :] // 64)
    c[:, 0:128] = np.eye(128)
    c[:, 128:256] = ((i[:, None] <= i[None, :]) & same)
    c[:, 256:384] = same
    c[:, 384:512] = np.where((i[None, :] < i[:, None]) & same, 0.0, BIG)
    c[:, 512:640] = np.where((i[:, None] <= i[None, :]) & same, 0.0, BIG)
    c[:, 640:768] = (i[None, :] >= i[:, None])
    c[:, 768:896] = 1.0
    pm = np.zeros((128, 128), np.float32)
    for m in range(128):
        d = m % 64
        if d < 8:
            pm[m + 8, m] = -1.0
        elif d < 16:
            pm[m - 8, m] = 1.0
    c[:, 896:1024] = pm
    c[:, 1024] = (i < 64)
    c[:, 1025] = (i >= 64)
    inv_freq = (500000.0 ** (-np.arange(0, 16, 2, dtype=np.float32) / np.float32(16))).astype(np.float32)
    d = i % 64
    c[:, 1026] = np.where(d < 16, inv_freq[d % 8], 0.0)
    c[:, 1027] = (i < 64)
    c[:, 1028] = (i >= 64)
    return c


def _bc(v, n=128):
    return np.ascontiguousarray(np.broadcast_to(np.asarray(v, np.float32).reshape(1, -1), (n, np.asarray(v).size)))


def _prep(inp):
    f = lambda a: np.ascontiguousarray(np.asarray(a, np.float32))
    x = f(inp["x"])
    w_in = f(inp["w_in"][0])
    O = [0, 512, 1024, 1536, 2048, 2052, 2056, 2568, 3080, 3592]
    gqkv = w_in[:, 0:1536]
    gz = w_in[:, O[3]:O[4]]
    gba = w_in[:, O[4]:O[6]]
    dq = w_in[:, O[6]:O[7]]
    dk = w_in[:, O[7]:O[8]]
    dv = w_in[:, O[8]:O[9]]
    wfm = np.concatenate([gqkv, dq, dk], 1).reshape(8, 128, 2560)
    wtm = np.concatenate([gz, dv, gba], 1).reshape(8, 128, 1032)
    convw = f(inp["conv_w"][0]).T.reshape(12, 128, 4).transpose(1, 0, 2).reshape(128, 48)
    hs = np.concatenate([_bc(inp["a_log"][0]), _bc(inp["dt_bias"][0])], 1)
    gnw4 = _bc(np.tile(f(inp["gdn_norm_w"][0]), 4))
    dnw4 = _bc(np.tile(f(inp["diff_norm_w"][0]), 4))
    lamv = np.concatenate([_bc(inp["lam_q1"][0]), _bc(inp["lam_k1"][0]), _bc(inp["lam_q2"][0]), _bc(inp["lam_k2"][0])], 1)
    shared = dict(
        wfm=np.ascontiguousarray(wfm), wtm=np.ascontiguousarray(wtm), convw=np.ascontiguousarray(convw), hs=np.ascontiguousarray(hs),
        gnw4=gnw4, dnw4=dnw4, lamv=np.ascontiguousarray(lamv),
        wout=f(inp["w_out"][0]).reshape(8, 128, 1024), consts=_consts(),
        msk4=np.ascontiguousarray(np.concatenate([np.tile(_consts()[:, 384:512], (1, 4)), np.tile(_consts()[:, 512:640], (1, 4))], 1)),
        lnp=np.ascontiguousarray(np.stack([_bc(inp[k][0]) for k in ("ln1_g", "ln1_b", "ln2_g", "ln2_b", "ln3_g", "ln3_b")], 0)),
        rw=f(inp["router_w"][0]).reshape(8, 128, 32), rbb=_bc(inp["router_b"][0]),
        wgu=f(inp["w_gu"][0]).reshape(32, 8, 128, 2048),
        bgu=np.ascontiguousarray(f(inp["b_gu"][0]).reshape(32, 16, 128).transpose(2, 0, 1).reshape(128, 512)),
        wdn=f(inp["w_down"][0]).reshape(32, 8, 128, 1024), bdn=np.concatenate([f(inp["b_down"][0]), np.zeros((96, 1024), np.float32)], 0),
        wpg=f(inp["ple_gate_w"][0]).reshape(8, 128, 1024), pgb=_bc(inp["ple_gate_b"][0]),
        plew=f(inp["ple_w"][0]).reshape(2, 128, 1024),
    )
    pos = np.asarray(inp["positions"], np.int32)
    p = f(inp["p"][0])
    maps = []
    for c in range(8):
        b, half = c // 2, c % 2
        m = dict(shared)
        m["xT"] = np.ascontiguousarray(x[b].T).reshape(8, 128, SEQ)
        m["posb"] = np.ascontiguousarray(np.broadcast_to(pos[b][None, :], (128, SEQ)))
        m01 = np.zeros((128, 2), np.float32)
        m01[:, half] = 1.0
        m["m01"] = m01
        m["xown"] = np.ascontiguousarray(x[b, half * 2048:(half + 1) * 2048]).reshape(16, 128, 1024)
        m["ptown"] = np.ascontiguousarray(p[b, half * 2048:(half + 1) * 2048].T).reshape(2, 128, 2048)
        maps.append(m)
    return maps


def _run(inputs, debug=False, phases="12CDE", nblk=NBLK):
    maps = _prep(inputs)
    if "D" not in phases:
        for m in maps:
            m.pop("wgu")
            m.pop("wdn")
    nc = build_program(debug=debug, phases=phases, nblk=nblk)
    res = run_bass_kernel_spmd(nc, maps, core_ids=list(range(8)))
    return res.results


def kernel(**inputs):
    results = _run(inputs, debug=False)
    out = np.zeros((4, SEQ, 1024), np.float32)
    for c in range(8):
        b, half = c // 2, c % 2
        out[b, half * 2048:(half + 1) * 2048] = np.asarray(results[c]["out"], np.float32).reshape(2048, 1024)
    return out
```

```python
import contextlib
import math
import numpy as np
import concourse.bass as bass
import concourse.mybir as mybir
from concourse.bass_utils import run_bass_kernel_spmd

F32 = mybir.dt.float32
BF16 = mybir.dt.bfloat16
I32 = mybir.dt.int32
AF = mybir.ActivationFunctionType
ALU = mybir.AluOpType
AX = mybir.AxisListType

SEQ = 4096
NBLK = 8
DN_ALPHA = 2.0 ** 0.25
LAM_INIT = 0.2
NCONST = 1032
BIG = 30000.0


class Sched:
    STREAMS = ("pe", "act", "dve", "pool", "sp")

    def __init__(self, nc):
        self.nc = nc
        self.ops = {s: [] for s in self.STREAMS}
        self.last_write = {}
        self.readers = {}
        self.dma_slots = {}
        self.sync_same = {"act", "dve", "pool"}

    def _deps_for(self, reads, writes):
        deps = []
        for k in reads:
            t = self.last_write.get(k)
            if t is not None:
                deps.append(t)
        for k in writes:
            t = self.last_write.get(k)
            if t is not None:
                deps.append(t)
            deps.extend(self.readers.get(k, ()))
        return [(d[0], d[1], self.dma_slots[d[1]]) if d[0] == "dma" else d for d in deps]

    def _commit(self, tok, reads, writes):
        for k in reads:
            self.readers.setdefault(k, []).append(tok)
        for k in writes:
            self.last_write[k] = tok
            self.readers[k] = []

    enabled = True

    def fence(self, fn):
        en = self.enabled
        self.enabled = True
        self.op("dve", fn, r=(), w=("__fence__",), _nofence=True)
        self.enabled = en

    def op(self, stream, fn, r=(), w=(), _nofence=False):
        if not self.enabled:
            return
        if not _nofence:
            r = tuple(r) + ("__fence__",)
        w = tuple(w) + tuple(k for k in r if len(k) == 2 and k[0] == "P" and k[1].isdigit())
        deps = self._deps_for(r, w)
        idx = len(self.ops[stream])
        self.ops[stream].append(dict(fn=fn, deps=deps, dma=None))
        self._commit(("eng", stream, idx), r, w)

    def dma(self, stream, fn, slot, r=(), w=()):
        if not self.enabled:
            return
        r = tuple(r) + ("__fence__",)
        deps = self._deps_for(r, w)
        n = self.dma_slots.get(slot, 0) + 1
        self.dma_slots[slot] = n
        self.ops[stream].append(dict(fn=fn, deps=deps, dma=(slot, n)))
        self._commit(("dma", slot, n), r, w)

    def emit(self, final_wait_slots=()):
        nc = self.nc
        milestone = {s: set() for s in self.STREAMS}
        for s in self.STREAMS:
            for o in self.ops[s]:
                for d in o["deps"]:
                    if d[0] == "eng":
                        if d[1] == s and s not in self.sync_same:
                            continue
                        milestone[d[1]].add(d[2])
        mcount = {}
        for s in self.STREAMS:
            c = 0
            arr = []
            for i in range(len(self.ops[s])):
                if i in milestone[s]:
                    c += 1
                arr.append(c)
            mcount[s] = arr
        with contextlib.ExitStack() as es:
            esem = {s: es.enter_context(nc.semaphore("s_" + s)) for s in self.STREAMS}
            dsem = {sl: es.enter_context(nc.semaphore("d_" + str(sl))) for sl in self.dma_slots}
            block = es.enter_context(nc.Block())

            def run_stream(s, eng):
                waited = {}
                for i, o in enumerate(self.ops[s]):
                    need = {}
                    for d in o["deps"]:
                        if d[0] == "eng":
                            if d[1] == s and s not in self.sync_same:
                                continue
                            key = ("e", d[1])
                            val = mcount[d[1]][d[2]]
                        else:
                            key = ("d", d[1])
                            val = 16 * d[2]
                        if val > need.get(key, 0):
                            need[key] = val
                    for key, val in need.items():
                        if waited.get(key, 0) >= val:
                            continue
                        waited[key] = val
                        sem = esem[key[1]] if key[0] == "e" else dsem[key[1]]
                        eng.wait_ge(sem, val)
                    ins = o["fn"](eng)
                    if o["dma"] is not None:
                        ins.then_inc(dsem[o["dma"][0]], 16)
                    elif i in milestone[s]:
                        ins.then_inc(esem[s], 1)
                if s == "sp":
                    for sl in final_wait_slots:
                        eng.wait_ge(dsem[sl], 16 * self.dma_slots[sl])

            block.sync(lambda e: run_stream("sp", e))
            block.tensor(lambda e: run_stream("pe", e))
            block.scalar(lambda e: run_stream("act", e))
            block.vector(lambda e: run_stream("dve", e))
            block.gpsimd(lambda e: run_stream("pool", e))


def build_program(debug=False, phases="12CDE", nblk=NBLK, stop=None):
    nc = bass.Bass("TRN2", target_bir_lowering=False)
    S = Sched(nc)

    def mark(level):
        if stop is not None and level > stop:
            S.enabled = False

    def din(name, shape, dt=F32):
        return nc.dram_tensor(name, list(shape), dt, kind="ExternalInput").ap()

    xT = din("xT", [8, 128, SEQ])
    posb = din("posb", [128, SEQ], I32)
    wfm_d = din("wfm", [8, 128, 2560])
    wtm_d = din("wtm", [8, 128, 1032])
    convw_d = din("convw", [128, 48])
    hs_d = din("hs", [128, 8])
    gnw_d = din("gnw4", [128, 512])
    dnw_d = din("dnw4", [128, 512])
    lamv_d = din("lamv", [128, 256])
    wout_d = din("wout", [8, 128, 1024])
    const_d = din("consts", [128, NCONST])
    msk4_d = din("msk4", [128, 1024])
    m01_d = din("m01", [128, 2])
    xown_d = din("xown", [16, 128, 1024])
    ptown_d = din("ptown", [2, 128, 2048])
    lnp_d = din("lnp", [6, 128, 1024])
    rw_d = din("rw", [8, 128, 32])
    rbb_d = din("rbb", [128, 32])
    wgu_d = din("wgu", [32, 8, 128, 2048]) if "D" in phases else None
    bgu_d = din("bgu", [128, 512])
    wdn_d = din("wdn", [32, 8, 128, 1024]) if "D" in phases else None
    bdn_d = din("bdn", [128, 1024])
    wpg_d = din("wpg", [8, 128, 1024])
    pgb_d = din("pgb", [128, 1024])
    plew_d = din("plew", [2, 128, 1024])
    out_d = nc.dram_tensor("out", [16, 128, 1024], F32, kind="ExternalOutput").ap()
    ogs_d = nc.dram_tensor("ogs", [32, 128, 512], BF16, kind="Internal").ap()
    mixs_d = nc.dram_tensor("mixs", [32, 128, 1024], F32, kind="Internal").ap()
    dbg = {}
    if debug:
        dbg["mix"] = nc.dram_tensor("dbg_mix", [32, 128, 1024], F32, kind="ExternalOutput").ap()
        dbg["og"] = nc.dram_tensor("dbg_og", [32, 128, 512], F32, kind="ExternalOutput").ap()
        dbg["h1"] = nc.dram_tensor("dbg_h1", [16, 128, 1024], F32, kind="ExternalOutput").ap()
        dbg["acc"] = nc.dram_tensor("dbg_acc", [16, 128, 1024], F32, kind="ExternalOutput").ap()

    with contextlib.ExitStack() as top:
        P = [top.enter_context(nc.psum_tensor("P%d" % i, [128, 512], F32)) for i in range(8)]

        def pk(i):
            return "P%d" % i

        def sbt(es, name, shape, dt=F32):
            return es.enter_context(nc.sbuf_tensor("sb_" + name, list(shape), dt))

        C = sbt(top, "C", [128, NCONST])
        ident = C[:, 0:128]
        tri = C[:, 128:256]
        blk = C[:, 256:384]
        mstrict = C[:, 384:512]
        minclT = C[:, 512:640]
        ind = C[:, 1024:1026]
        freq = C[:, 1026:1027]
        c0 = C[:, 1027:1028]
        c1 = C[:, 1028:1029]
        fz = sbt(top, "fz", [128, 8])

        def fence():
            S.fence(lambda e: e.memset(fz[:], 0.0))
        Cb = sbt(top, "Cb", [128, 768], BF16)
        ident_b = Cb[:, 0:128]
        causal_b = Cb[:, 128:256]
        ones_b = Cb[:, 256:384]
        perm_b = Cb[:, 384:512]
        tri_b = Cb[:, 512:640]
        blk_b = Cb[:, 640:768]
        S.dma("sp", lambda e: e.dma_start(out=C[:], in_=const_d[:, :]), "c0", w=["C"])
        S.op("dve", lambda e: e.tensor_copy(out=Cb[:, 0:128], in_=C[:, 0:128]), r=["C"], w=["Cb"])
        S.op("dve", lambda e: e.tensor_copy(out=Cb[:, 128:384], in_=C[:, 640:896]), r=["C"], w=["Cb"])
        S.op("dve", lambda e: e.tensor_copy(out=Cb[:, 384:512], in_=C[:, 896:1024]), r=["C"], w=["Cb"])
        S.op("dve", lambda e: e.tensor_copy(out=Cb[:, 512:768], in_=C[:, 128:384]), r=["C"], w=["Cb"])

        S.enabled = "1" in phases
        with contextlib.ExitStack() as es:
            wg1 = sbt(es, "wg1", [128, 8, 1536], BF16)
            wt1 = sbt(es, "wt1", [128, 8, 520], BF16)
            xTbs = [sbt(es, "xTb%d" % i, [128, 8, 512], BF16) for i in range(2)]
            cin = sbt(es, "cin", [128, 12, 515], BF16)
            cw = sbt(es, "cw", [128, 48])
            hsb = sbt(es, "hsb", [128, 8])
            nega = sbt(es, "nega", [128, 4])
            gnw = sbt(es, "gnw", [128, 512])
            ycv = sbt(es, "ycv", [128, 512])
            gfm = sbt(es, "gfm", [128, 12, 512], BF16)
            zs = sbt(es, "zs", [128, 4, 512], BF16)
            beta = sbt(es, "beta", [128, 4, 4])
            ldt = sbt(es, "ldt", [128, 4, 4])
            sm = sbt(es, "sm", [128, 64])
            sq = sbt(es, "sq", [128, 8, 128], BF16)
            tA = sbt(es, "tA", [128, 512])
            tB = sbt(es, "tB", [128, 512])
            qn = sbt(es, "qn", [128, 4, 128], BF16)
            kn = sbt(es, "kn", [128, 4, 128], BF16)
            Rr = sbt(es, "Rr", [128, 4, 256])
            Rb = sbt(es, "Rb", [128, 4, 256], BF16)
            kd0 = sbt(es, "kd0", [128, 4, 128], BF16)
            kd1 = sbt(es, "kd1", [128, 4, 128], BF16)
            ldB = sbt(es, "ldB", [128, 8, 128], BF16)
            ldhl = sbt(es, "ldhl", [128, 8], BF16)
            msk4 = sbt(es, "msk4", [128, 1024])
            mstrict4 = msk4[:, 0:512]
            minclT4 = msk4[:, 512:1024]
            S.dma("sp", lambda e: e.dma_start(out=msk4[:], in_=msk4_d[:, :]), "c1", w=["msk4"])
            Ga = sbt(es, "Ga", [128, 4, 128])
            Gb = sbt(es, "Gb", [128, 4, 128])
            egr = sbt(es, "egr", [128, 4, 128])
            Em = sbt(es, "Em", [128, 4, 128])
            ETm = sbt(es, "ETm", [128, 4, 128])
            Nb = sbt(es, "Nb", [128, 4, 128], BF16)
            Mb = sbt(es, "Mb", [128, 4, 128], BF16)
            qkT = sbt(es, "qkT", [128, 4, 128], BF16)
            wb = sbt(es, "wb", [128, 4, 128], BF16)
            wT = sbt(es, "wT", [128, 4, 128], BF16)
            qd = sbt(es, "qd", [128, 4, 128], BF16)
            St = sbt(es, "St", [128, 4, 128])
            Sb = sbt(es, "Sb", [128, 4, 128], BF16)
            vnb = sbt(es, "vnb", [128, 4, 128], BF16)
            og = sbt(es, "og", [128, 4, 128])
            ogb = sbt(es, "ogb", [128, 512], BF16)
            PT = P[1][:].bitcast(BF16)

            for k in range(8):
                S.dma("pool", (lambda k: lambda e: e.dma_start(out=wg1[:, k, :], in_=wfm_d[k, :, 0:1536]))(k), "w1", w=["wg1"])
                S.dma("pool", (lambda k: lambda e: e.dma_start(out=wt1[:, k, 0:512], in_=wtm_d[k, :, 0:512]))(k), "w1", w=["wt1"])
                S.dma("pool", (lambda k: lambda e: e.dma_start(out=wt1[:, k, 512:520], in_=wtm_d[k, :, 1024:1032]))(k), "w1", w=["wt1"])
            S.dma("sp", lambda e: e.dma_start(out=cw[:], in_=convw_d[:, :]), "c1", w=["cw"])
            S.dma("sp", lambda e: e.dma_start(out=hsb[:], in_=hs_d[:, :]), "c1", w=["hsb"])
            S.dma("sp", lambda e: e.dma_start(out=gnw[:], in_=gnw_d[:, :]), "c1", w=["gnw"])
            S.op("act", lambda e: e.activation(out=nega[:], in_=hsb[:, 0:4], func=AF.Exp), r=["hsb"], w=["nega"])
            S.op("dve", lambda e: e.tensor_scalar(out=nega[:], in0=nega[:], scalar1=-1.0, scalar2=None, op0=ALU.mult), r=["nega"], w=["nega"])
            S.op("dve", lambda e: e.memset(cin[:].rearrange("p a b -> p (a b)"), 0.0), w=["cin"] + ["cin%d" % i for i in range(12)])
            S.op("dve", lambda e: e.memset(St[:].rearrange("p a b -> p (a b)"), 0.0), w=["St"])
            S.op("dve", lambda e: e.memset(Sb[:].rearrange("p a b -> p (a b)"), 0.0), w=["Sb"])
            S.op("dve", lambda e: e.memset(vnb[:].rearrange("p a b -> p (a b)"), 0.0), w=["vnb"])

            for b in range(nblk):
                t0 = b * 512
                mark(2)
                xTb = xTbs[b % 2]
                xk = "xTb%d" % (b % 2)
                for bb in ([0, 1] if b == 0 else [b + 1]):
                    if bb < nblk:
                        S.dma("pool", (lambda bb: lambda e: e.dma_start(out=xTbs[bb % 2][:], in_=xT[:, :, bb * 512:bb * 512 + 512].rearrange("k p t -> p k t")))(bb),
                              "x1_%d" % (bb % 2), w=["xTb%d" % (bb % 2)])
                for ch in range(12):
                    def mm(e, ch=ch, xTb=xTb):
                        for k in range(8):
                            ins = e.matmul(P[0][:], lhsT=wg1[:, k, ch * 128:(ch + 1) * 128], rhs=xTb[:, k, :], start=(k == 0), stop=(k == 7))
                        return ins
                    S.op("pe", mm, r=["wg1", xk], w=[pk(0)])
                    S.op("act", (lambda ch: lambda e: e.activation(out=cin[:, ch, 3:515], in_=P[0][:], func=AF.Copy))(ch), r=[pk(0)], w=["cin%d" % ch, "cin"])
                    S.op("dve", (lambda ch: lambda e: e.tensor_scalar(out=ycv[:], in0=cin[:, ch, 0:512], scalar1=cw[:, ch * 4:ch * 4 + 1], scalar2=None, op0=ALU.mult))(ch),
                         r=["cin%d" % ch, "cw"], w=["ycv"])
                    for wi in range(1, 4):
                        S.op("dve", (lambda ch, wi: lambda e: e.scalar_tensor_tensor(out=ycv[:], in0=cin[:, ch, wi:wi + 512], scalar=cw[:, ch * 4 + wi:ch * 4 + wi + 1],
                                                                                     in1=ycv[:], op0=ALU.mult, op1=ALU.add))(ch, wi), r=["cin%d" % ch, "cw", "ycv"], w=["ycv"])
                    S.op("act", (lambda ch: lambda e: e.activation(out=gfm[:, ch, :], in_=ycv[:], func=AF.Silu))(ch), r=["ycv"], w=["gfm"])
                    S.op("pool", (lambda ch: lambda e: e.tensor_copy(out=cin[:, ch, 0:3], in_=cin[:, ch, 512:515]))(ch), r=["cin%d" % ch], w=["cin%d" % ch])
                mark(3)
                for t in range(4):
                    c0_ = t * 128

                    def mmz(e, c0_=c0_, xTb=xTb):
                        for k in range(8):
                            ins = e.matmul(P[0][:], lhsT=xTb[:, k, c0_:c0_ + 128], rhs=wt1[:, k, 0:512], start=(k == 0), stop=(k == 7))
                        return ins
                    S.op("pe", mmz, r=["wt1", xk], w=[pk(0)])
                    S.op("act", (lambda t: lambda e: e.activation(out=zs[:, t, :], in_=P[0][:], func=AF.Silu))(t), r=[pk(0)], w=["zs"])

                    def mmb(e, c0_=c0_, xTb=xTb):
                        for k in range(8):
                            ins = e.matmul(P[2][:, 0:8], lhsT=xTb[:, k, c0_:c0_ + 128], rhs=wt1[:, k, 512:520], start=(k == 0), stop=(k == 7))
                        return ins
                    S.op("pe", mmb, r=["wt1", xk], w=[pk(2)])
                    S.op("act", lambda e: e.activation(out=sm[:, 0:4], in_=P[2][:, 0:4], func=AF.Exp, scale=-1.0), r=[pk(2)], w=["sm"])
                    S.op("dve", lambda e: e.tensor_scalar(out=sm[:, 0:4], in0=sm[:, 0:4], scalar1=1.0, scalar2=None, op0=ALU.add), r=["sm"], w=["sm"])
                    S.op("dve", (lambda t: lambda e: e.reciprocal(out=beta[:, t, :], in_=sm[:, 0:4]))(t), r=["sm"], w=["beta"])
                    S.op("dve", lambda e: e.tensor_tensor(out=sm[:, 4:8], in0=P[2][:, 4:8], in1=hsb[:, 4:8], op=ALU.add), r=[pk(2), "hsb"], w=["sm"])
                    S.op("dve", lambda e: e.tensor_scalar(out=sm[:, 8:12], in0=sm[:, 4:8], scalar1=-1.0, scalar2=None, op0=ALU.mult), r=["sm"], w=["sm"])
                    S.op("dve", lambda e: e.tensor_tensor(out=sm[:, 8:12], in0=sm[:, 8:12], in1=sm[:, 4:8], op=ALU.max), r=["sm"], w=["sm"])
                    S.op("act", lambda e: e.activation(out=sm[:, 8:12], in_=sm[:, 8:12], func=AF.Exp, scale=-1.0), r=["sm"], w=["sm"])
                    S.op("act", lambda e: e.activation(out=sm[:, 8:12], in_=sm[:, 8:12], func=AF.Ln, bias=1.0), r=["sm"], w=["sm"])
                    S.op("dve", lambda e: e.tensor_scalar(out=sm[:, 4:8], in0=sm[:, 4:8], scalar1=0.0, scalar2=None, op0=ALU.max), r=["sm"], w=["sm"])
                    S.op("dve", lambda e: e.tensor_tensor(out=sm[:, 4:8], in0=sm[:, 4:8], in1=sm[:, 8:12], op=ALU.add), r=["sm"], w=["sm"])
                    S.op("dve", (lambda t: lambda e: e.tensor_tensor(out=ldt[:, t, :], in0=sm[:, 4:8], in1=nega[:], op=ALU.mult))(t), r=["sm", "nega"], w=["ldt"])

                for t in range(4):
                    cs = slice(t * 128, (t + 1) * 128)
                    bet = beta[:, t, :]
                    ld = ldt[:, t, :]
                    mark(4)
                    S.op("pool", (lambda cs: lambda e: e.tensor_tensor(out=sq[:], in0=gfm[:, 0:8, cs], in1=gfm[:, 0:8, cs], op=ALU.mult))(cs), r=["gfm"], w=["sq"])
                    sqf = sq[:].rearrange("p h d -> p (h d)")
                    S.op("pe", lambda e: e.matmul(P[2][:], lhsT=ones_b, rhs=sqf[:, 0:512], start=True, stop=True), r=["sq", "Cb"], w=[pk(2)])
                    S.op("pe", lambda e: e.matmul(P[3][:], lhsT=ones_b, rhs=sqf[:, 512:1024], start=True, stop=True), r=["sq", "Cb"], w=[pk(3)])
                    S.op("act", lambda e: e.activation(out=tA[:], in_=P[2][:], func=AF.Ln, bias=1e-6), r=[pk(2)], w=["tA"])
                    S.op("act", lambda e: e.activation(out=tA[:], in_=tA[:], func=AF.Exp, scale=-0.5), r=["tA"], w=["tA"])
                    S.op("act", lambda e: e.activation(out=tB[:], in_=P[3][:], func=AF.Ln, bias=1e-6), r=[pk(3)], w=["tB"])
                    S.op("act", lambda e: e.activation(out=tB[:], in_=tB[:], func=AF.Exp, scale=-0.5), r=["tB"], w=["tB"])
                    S.op("dve", (lambda cs: lambda e: e.scalar_tensor_tensor(out=qn[:], in0=gfm[:, 0:4, cs], scalar=float(128 ** -0.5),
                                                                             in1=tA[:].rearrange("p (h d) -> p h d", h=4), op0=ALU.mult, op1=ALU.mult))(cs), r=["gfm", "tA"], w=["qn"])
                    S.op("dve", (lambda cs: lambda e: e.tensor_tensor(out=kn[:], in0=gfm[:, 4:8, cs], in1=tB[:].rearrange("p (h d) -> p h d", h=4), op=ALU.mult))(cs),
                         r=["gfm", "tB"], w=["kn"])

                    mark(4.2)
                    def trkv(e, cs=cs):
                        for h in range(4):
                            e.transpose(PT[:, h * 128:(h + 1) * 128], kn[:, h, :], ident_b)
                        for h in range(4):
                            ins = e.transpose(PT[:, 512 + h * 128:512 + (h + 1) * 128], gfm[:, 8 + h, cs], ident_b)
                        return ins
                    S.op("pe", trkv, r=["kn", "gfm", "Cb"], w=[pk(1)])
                    mark(4.4)
                    S.op("act", (lambda ld: lambda e: e.activation(out=ldhl[:, 0:4], in_=ld, func=AF.Copy))(ld), r=["ldt"], w=["ldhl"])
                    S.op("dve", (lambda ld: lambda e: e.tensor_tensor(out=ldhl[:, 4:8], in0=ld, in1=ldhl[:, 0:4], op=ALU.subtract))(ld), r=["ldt", "ldhl"], w=["ldhl"])

                    def mmg2(e):
                        e.matmul(P[5][:, 0:4], lhsT=tri_b, rhs=ldhl[:, 0:4], start=True, stop=False)
                        e.matmul(P[5][:, 0:4], lhsT=tri_b, rhs=ldhl[:, 4:8], start=False, stop=True)
                        e.matmul(P[5][:, 4:8], lhsT=blk_b, rhs=ldhl[:, 0:4], start=True, stop=False)
                        return e.matmul(P[5][:, 4:8], lhsT=blk_b, rhs=ldhl[:, 4:8], start=False, stop=True)
                    S.op("pe", mmg2, r=["Cb", "ldhl"], w=[pk(5)])
                    S.op("act", lambda e: e.activation(out=sm[:, 16:20], in_=P[5][:, 0:4], func=AF.Copy), r=[pk(5)], w=["sm2"])
                    S.op("act", lambda e: e.activation(out=sm[:, 24:28], in_=P[5][:, 0:4], func=AF.Exp), r=[pk(5)], w=["sm2"])
                    S.op("dve", lambda e: e.tensor_scalar(out=sm[:, 20:24], in0=sm[:, 16:20], scalar1=-1.0, scalar2=None, op0=ALU.mult), r=["sm2"], w=["sm2"])
                    S.op("dve", lambda e: e.tensor_tensor(out=sm[:, 28:32], in0=P[5][:, 4:8], in1=sm[:, 16:20], op=ALU.subtract), r=[pk(5), "sm2"], w=["sm2"])
                    S.op("act", lambda e: e.activation(out=sm[:, 28:32], in_=sm[:, 28:32], func=AF.Exp), r=["sm2"], w=["sm2"])
                    S.op("dve", (lambda bet: lambda e: e.tensor_tensor(out=sm[:, 32:36], in0=sm[:, 24:28], in1=bet, op=ALU.mult))(bet), r=["sm2", "beta"], w=["sm2"])
                    S.op("dve", lambda e: e.tensor_scalar(out=sm[:, 36:40], in0=sm[:, 28:32], scalar1=ind[:, 0:1], scalar2=None, op0=ALU.mult), r=["sm2", "C"], w=["sm2"])
                    S.op("dve", lambda e: e.tensor_scalar(out=sm[:, 40:44], in0=sm[:, 28:32], scalar1=ind[:, 1:2], scalar2=None, op0=ALU.mult), r=["sm2", "C"], w=["sm2"])
                    S.op("dve", (lambda bet: lambda e: e.tensor_scalar(out=sm[:, 44:48], in0=bet, scalar1=-1.0, scalar2=None, op0=ALU.mult))(bet), r=["beta"], w=["sm2"])

                    mark(4.6)

                    def bc(col):
                        return sm[:, col:col + 4].unsqueeze(2).to_broadcast([128, 4, 128])
                    PTk = PT[:, 0:512].rearrange("p (h d) -> p h d", h=4)
                    PTv = PT[:, 512:1024].rearrange("p (h d) -> p h d", h=4)
                    S.op("dve", (lambda bet: lambda e: e.tensor_tensor(out=Rr[:, :, 0:128], in0=PTv, in1=bet.unsqueeze(2).to_broadcast([128, 4, 128]), op=ALU.mult))(bet),
                         r=[pk(1), "beta"], w=["Rr"])
                    S.op("dve", lambda e: e.tensor_tensor(out=Rr[:, :, 128:256], in0=PTk, in1=bc(32), op=ALU.mult), r=[pk(1), "sm2"], w=["Rr"])
                    S.op("dve", lambda e: e.tensor_tensor(out=kd0[:], in0=PTk, in1=bc(36), op=ALU.mult), r=[pk(1), "sm2"], w=["kd0"])
                    S.op("dve", lambda e: e.tensor_tensor(out=kd1[:], in0=PTk, in1=bc(40), op=ALU.mult), r=[pk(1), "sm2"], w=["kd1"])
                    mark(5)
                    S.op("dve", lambda e: e.tensor_copy(out=ldB[:], in_=ldhl[:].unsqueeze(2).to_broadcast([128, 8, 128])), r=["ldhl"], w=["ldB"])

                    def mmG(e):
                        for h in range(4):
                            e.matmul(P[4][:, h * 128:(h + 1) * 128], lhsT=ldB[:, h, :], rhs=tri_b, start=True, stop=False)
                            ins = e.matmul(P[4][:, h * 128:(h + 1) * 128], lhsT=ldB[:, 4 + h, :], rhs=tri_b, start=False, stop=True)
                        return ins
                    S.op("pe", mmG, r=["ldB", "Cb"], w=[pk(4)])
                    P4v = P[4][:].rearrange("p (h d) -> p h d", h=4)
                    mark(5.2)
                    S.op("dve", lambda e: e.tensor_tensor(out=Ga[:], in0=P4v, in1=mstrict4.rearrange("p (h d) -> p h d", h=4), op=ALU.add), r=[pk(4), "msk4"], w=["Ga"])
                    S.op("dve", lambda e: e.tensor_tensor(out=Gb[:], in0=P4v, in1=minclT4.rearrange("p (h d) -> p h d", h=4), op=ALU.subtract), r=[pk(4), "msk4"], w=["Gb"])
                    S.op("act", lambda e: e.activation(out=egr[:], in_=P4v, func=AF.Exp), r=[pk(4)], w=["egr"])
                    mark(5.4)
                    for h in range(4):
                        S.op("act", (lambda h: lambda e: e.activation(out=Em[:, h, :], in_=Ga[:, h, :], func=AF.Exp, scale=-1.0, bias=sm[:, 16 + h:17 + h]))(h),
                             r=["Ga", "sm2"], w=["Em"])
                        S.op("act", (lambda h: lambda e: e.activation(out=ETm[:, h, :], in_=Gb[:, h, :], func=AF.Exp, bias=sm[:, 20 + h:21 + h]))(h),
                             r=["Gb", "sm2"], w=["ETm"])

                    mark(5.6)
                    def mmKK(e):
                        for h in range(4):
                            e.matmul(P[2][:, h * 128:(h + 1) * 128], lhsT=kn[:, h, :], rhs=kn[:, h, :], start=True, stop=True)
                        for h in range(4):
                            ins = e.matmul(P[3][:, h * 128:(h + 1) * 128], lhsT=kn[:, h, :], rhs=qn[:, h, :], start=True, stop=True)
                        return ins
                    S.op("pe", mmKK, r=["kn", "qn"], w=[pk(2), pk(3)])
                    P2v = P[2][:].rearrange("p (h d) -> p h d", h=4)
                    P3v = P[3][:].rearrange("p (h d) -> p h d", h=4)
                    tAv = tA[:].rearrange("p (h d) -> p h d", h=4)
                    S.op("dve", lambda e: e.tensor_tensor(out=tAv, in0=P2v, in1=bc(44), op=ALU.mult), r=[pk(2), "sm2"], w=["tA"])
                    S.op("dve", lambda e: e.tensor_tensor(out=Nb[:], in0=tAv, in1=Em[:], op=ALU.mult), r=["tA", "Em"], w=["Nb"])
                    S.op("dve", lambda e: e.tensor_tensor(out=qkT[:], in0=P3v, in1=ETm[:], op=ALU.mult), r=[pk(3), "ETm"], w=["qkT"])

                    def trN(e):
                        for h in range(4):
                            ins = e.transpose(PT[:, h * 128:(h + 1) * 128], Nb[:, h, :], ident_b)
                        return ins
                    S.op("pe", trN, r=["Nb", "Cb"], w=[pk(1)])
                    S.op("act", lambda e: e.activation(out=Mb[:], in_=PTk, func=AF.Copy), r=[pk(1)], w=["Mb"])
                    mark(6)
                    for p in range(6):
                        S.op("act", lambda e: e.activation(out=Rb[:], in_=Rr[:], func=AF.Copy), r=["Rr"], w=["Rb"])

                        def mmR(e):
                            for h in range(4):
                                ins = e.matmul(P[6 + h // 2][:, (h % 2) * 256:(h % 2) * 256 + 256], lhsT=Mb[:, h, :], rhs=Rb[:, h, :], start=True, stop=True)
                            return ins
                        S.op("pe", mmR, r=["Mb", "Rb"], w=[pk(6), pk(7)])
                        S.op("dve", lambda e: e.tensor_tensor(out=Rr[:, 0:2, :], in0=Rr[:, 0:2, :], in1=P[6][:].rearrange("p (h d) -> p h d", h=2), op=ALU.add),
                             r=["Rr", pk(6)], w=["Rr"])
                        S.op("dve", lambda e: e.tensor_tensor(out=Rr[:, 2:4, :], in0=Rr[:, 2:4, :], in1=P[7][:].rearrange("p (h d) -> p h d", h=2), op=ALU.add),
                             r=["Rr", pk(7)], w=["Rr"])
                        if p < 5:
                            def mmSq(e):
                                for h in range(4):
                                    e.matmul(P[4][:, h * 128:(h + 1) * 128], lhsT=Mb[:, h, :], rhs=Nb[:, h, :], start=True, stop=True)
                                for h in range(4):
                                    ins = e.matmul(P[5][:, h * 128:(h + 1) * 128], lhsT=Nb[:, h, :], rhs=Mb[:, h, :], start=True, stop=True)
                                return ins
                            S.op("pe", mmSq, r=["Mb", "Nb"], w=[pk(4), pk(5)])
                            S.op("act", lambda e: e.activation(out=Nb[:], in_=P4v, func=AF.Copy), r=[pk(4)], w=["Nb"])
                            S.op("dve", lambda e: e.tensor_copy(out=Mb[:], in_=P[5][:].rearrange("p (h d) -> p h d", h=4)), r=[pk(5)], w=["Mb"])
                    mark(7)
                    S.op("act", lambda e: e.activation(out=wb[:], in_=Rr[:, :, 128:256], func=AF.Copy), r=["Rr"], w=["wb"])

                    def trW(e):
                        for h in range(4):
                            ins = e.transpose(PT[:, h * 128:(h + 1) * 128], wb[:, h, :], ident_b)
                        return ins
                    S.op("pe", trW, r=["wb", "Cb"], w=[pk(1)])
                    S.op("act", lambda e: e.activation(out=wT[:], in_=PTk, func=AF.Copy), r=[pk(1)], w=["wT"])
                    S.op("dve", lambda e: e.tensor_tensor(out=qd[:], in0=qn[:], in1=egr[:], op=ALU.mult), r=["qn", "egr"], w=["qd"])
                    for ch in range(2):
                        kd = kd0 if ch == 0 else kd1
                        kdk = "kd0" if ch == 0 else "kd1"
                        last = 63 if ch == 0 else 127

                        def mmV(e):
                            for h in range(4):
                                ins = e.matmul(P[2][:, h * 128:(h + 1) * 128], lhsT=wT[:, h, :], rhs=Sb[:, h, :], start=True, stop=True)
                            return ins
                        S.op("pe", mmV, r=["wT", "Sb"], w=[pk(2)])
                        S.op("dve", lambda e: e.tensor_tensor(out=vnb[:], in0=Rr[:, :, 0:128], in1=P2v, op=ALU.subtract), r=["Rr", pk(2)], w=["vnb"])

                        def mmO(e, kd=kd):
                            for h in range(4):
                                e.matmul(P[3][:, h * 128:(h + 1) * 128], lhsT=qd[:, h, :], rhs=Sb[:, h, :], start=True, stop=False)
                                e.matmul(P[3][:, h * 128:(h + 1) * 128], lhsT=qkT[:, h, :], rhs=vnb[:, h, :], start=False, stop=True)
                            for h in range(4):
                                ins = e.matmul(P[4][:, h * 128:(h + 1) * 128], lhsT=kd[:, h, :], rhs=vnb[:, h, :], start=True, stop=True)
                            return ins
                        S.op("pe", mmO, r=["qd", "Sb", "qkT", "vnb", kdk], w=[pk(3), pk(4)])
                        if ch == 0:
                            S.op("dve", lambda e: e.tensor_scalar(out=og[:], in0=P3v, scalar1=ind[:, 0:1], scalar2=None, op0=ALU.mult), r=[pk(3), "C"], w=["og"])
                        else:
                            S.op("dve", lambda e: e.scalar_tensor_tensor(out=og[:], in0=P3v, scalar=ind[:, 1:2], in1=og[:], op0=ALU.mult, op1=ALU.add),
                                 r=[pk(3), "C", "og"], w=["og"])
                        for h in range(4):
                            S.op("dve", (lambda h, last: lambda e: e.scalar_tensor_tensor(out=St[:, h, :], in0=St[:, h, :], scalar=egr[:, h, last:last + 1],
                                                                                          in1=P[4][:, h * 128:(h + 1) * 128], op0=ALU.mult, op1=ALU.add))(h, last),
                                 r=["St", "egr", pk(4)], w=["St"])
                        S.op("act", lambda e: e.activation(out=Sb[:], in_=St[:], func=AF.Copy), r=["St"], w=["Sb"])
                    mark(8)
                    ogf = og[:].rearrange("p h d -> p (h d)")
                    S.op("pool", lambda e: e.tensor_tensor(out=tB[:], in0=ogf, in1=ogf, op=ALU.mult), r=["og"], w=["tB"])
                    S.op("dve", lambda e: e.tensor_reduce(out=sm[:, 48:52], in_=tB[:].rearrange("p (h d) -> p h d", h=4), axis=AX.X, op=ALU.add), r=["tB"], w=["sm3"])
                    S.op("act", lambda e: e.activation(out=sm[:, 48:52], in_=sm[:, 48:52], func=AF.Ln, scale=1.0 / 128.0, bias=1e-6), r=["sm3"], w=["sm3"])
                    S.op("act", lambda e: e.activation(out=sm[:, 48:52], in_=sm[:, 48:52], func=AF.Exp, scale=-0.5), r=["sm3"], w=["sm3"])
                    S.op("dve", lambda e: e.tensor_tensor(out=tAv, in0=og[:], in1=bc(48), op=ALU.mult), r=["og", "sm3"], w=["tA"])
                    S.op("dve", lambda e: e.tensor_tensor(out=tA[:], in0=tA[:], in1=gnw[:], op=ALU.mult), r=["tA", "gnw"], w=["tA"])
                    S.op("dve", (lambda t: lambda e: e.tensor_tensor(out=ogb[:], in0=tA[:], in1=zs[:, t, :], op=ALU.mult))(t), r=["tA", "zs"], w=["ogb"])
                    ti = b * 4 + t
                    S.dma("sp", (lambda ti: lambda e: e.dma_start(out=ogs_d[ti, :, :], in_=ogb[:]))(ti), "ogw", r=["ogb"], w=["ogs%d" % ti])
                    if debug:
                        S.dma("sp", (lambda ti: lambda e: e.dma_start(out=dbg["og"][ti, :, :], in_=og[:].rearrange("p h d -> p (h d)")))(ti), "dbg", r=["og"])

        fence()
        S.enabled = "2" in phases
        with contextlib.ExitStack() as es:
            wq2 = sbt(es, "wq2", [128, 8, 1024], BF16)
            wv2 = sbt(es, "wv2", [128, 8, 512], BF16)
            wo = sbt(es, "wo", [128, 8, 1024], BF16)
            xT2s = [sbt(es, "xTc%d" % i, [128, 8, 512], BF16) for i in range(2)]
            KT = sbt(es, "KT", [128, 4, SEQ], BF16)
            Vs = sbt(es, "Vs", [128, 32, 4, 132], BF16)
            pi_ = sbt(es, "pi_", [128, 512], I32)
            pf = sbt(es, "pf", [128, 512])
            ka = sbt(es, "ka", [128, 512])
            ki = sbt(es, "ki", [128, 512], I32)
            cosF = sbt(es, "cosF", [128, 512])
            sinF = sbt(es, "sinF", [128, 512])
            mb = sbt(es, "mb", [128, 512], BF16)
            r1 = sbt(es, "r1", [128, 512])
            r2 = sbt(es, "r2", [128, 512])
            Q0 = sbt(es, "Q0", [128, 4, 512], BF16)
            Q1 = sbt(es, "Q1", [128, 4, 512], BF16)
            pT = [sbt(es, "pT%d" % i, [128, 512], BF16) for i in range(2)]
            den = sbt(es, "den", [128, 16])
            od = sbt(es, "od", [128, 4, 4, 128])
            ot = sbt(es, "ot", [128, 4, 1024], BF16)
            oT = sbt(es, "oT", [128, 8, 128], BF16)
            mixt = sbt(es, "mixt", [128, 1024])
            dnw = sbt(es, "dnw", [128, 512])
            lamv = sbt(es, "lamv", [128, 256])
            lam = sbt(es, "lam", [128, 8])
            PT2 = P[6][:].bitcast(BF16)

            for k in range(8):
                S.dma("pool", (lambda k: lambda e: e.dma_start(out=wq2[:, k, :], in_=wfm_d[k, :, 1536:2560]))(k), "w2", w=["wq2"])
                S.dma("pool", (lambda k: lambda e: e.dma_start(out=wv2[:, k, :], in_=wtm_d[k, :, 512:1024]))(k), "w2", w=["wv2"])
                S.dma("pool", (lambda k: lambda e: e.dma_start(out=wo[:, k, :], in_=wout_d[k, :, :]))(k), "w2", w=["wo"])
            S.dma("sp", lambda e: e.dma_start(out=dnw[:], in_=dnw_d[:, :]), "c2", w=["dnw"])
            S.dma("sp", lambda e: e.dma_start(out=lamv[:], in_=lamv_d[:, :]), "c2", w=["lamv"])
            S.op("dve", lambda e: e.memset(Vs[:].rearrange("p a b c -> p (a b c)"), 1.0), w=["Vs"])
            S.op("dve", lambda e: e.tensor_tensor(out=lamv[:, 0:64], in0=lamv[:, 0:64], in1=lamv[:, 64:128], op=ALU.mult), r=["lamv"], w=["lamv"])
            S.op("dve", lambda e: e.tensor_tensor(out=lamv[:, 128:192], in0=lamv[:, 128:192], in1=lamv[:, 192:256], op=ALU.mult), r=["lamv"], w=["lamv"])
            S.op("dve", lambda e: e.tensor_reduce(out=lam[:, 0:1], in_=lamv[:, 0:64], axis=AX.X, op=ALU.add), r=["lamv"], w=["lam"])
            S.op("dve", lambda e: e.tensor_reduce(out=lam[:, 1:2], in_=lamv[:, 128:192], axis=AX.X, op=ALU.add), r=["lamv"], w=["lam"])
            S.op("act", lambda e: e.activation(out=lam[:, 0:2], in_=lam[:, 0:2], func=AF.Exp), r=["lam"], w=["lam"])
            S.op("dve", lambda e: e.tensor_tensor(out=lam[:, 2:3], in0=lam[:, 1:2], in1=lam[:, 0:1], op=ALU.subtract), r=["lam"], w=["lam"])
            S.op("dve", lambda e: e.tensor_scalar(out=lam[:, 2:3], in0=lam[:, 2:3], scalar1=-LAM_INIT, scalar2=None, op0=ALU.add), r=["lam"], w=["lam"])

            def range_reduce_sin(dst, src_key):
                S.op("dve", lambda e: e.tensor_scalar(out=ki[:], in0=ka[:], scalar1=float(1.0 / (2 * math.pi)), scalar2=None, op0=ALU.mult), r=["ka"], w=["ki"])
                S.op("dve", lambda e: e.tensor_copy(out=r1[:], in_=ki[:]), r=["ki"], w=["r1"])
                S.op("dve", lambda e: e.scalar_tensor_tensor(out=ka[:], in0=r1[:], scalar=float(-2 * math.pi), in1=ka[:], op0=ALU.mult, op1=ALU.add), r=["r1", "ka"], w=["ka"])
                S.op("dve", lambda e: e.tensor_scalar(out=r1[:], in0=ka[:], scalar1=float(math.pi), scalar2=float(-2 * math.pi), op0=ALU.is_gt, op1=ALU.mult), r=["ka"], w=["r1"])
                S.op("dve", lambda e: e.tensor_tensor(out=ka[:], in0=ka[:], in1=r1[:], op=ALU.add), r=["ka", "r1"], w=["ka"])
                S.op("act", lambda e: e.activation(out=dst[:], in_=ka[:], func=AF.Sin), r=["ka"], w=[src_key])

            for b in range(nblk):
                t0 = b * 512
                xT2 = xT2s[b % 2]
                xk = "xTc%d" % (b % 2)
                for bb in ([0, 1] if b == 0 else [b + 1]):
                    if bb < nblk:
                        S.dma("pool", (lambda bb: lambda e: e.dma_start(out=xT2s[bb % 2][:], in_=xT[:, :, bb * 512:bb * 512 + 512].rearrange("k p t -> p k t")))(bb),
                              "x2_%d" % (bb % 2), w=["xTc%d" % (bb % 2)])
                S.dma("sp", (lambda t0: lambda e: e.dma_start(out=pi_[:], in_=posb[:, t0:t0 + 512]))(t0), "pos", w=["pi_"])
                S.op("dve", lambda e: e.tensor_copy(out=pf[:], in_=pi_[:]), r=["pi_"], w=["pf"])
                S.op("dve", lambda e: e.tensor_scalar(out=pf[:], in0=pf[:], scalar1=freq, scalar2=None, op0=ALU.mult), r=["pf", "C"], w=["pf"])
                S.op("dve", lambda e: e.tensor_copy(out=ka[:], in_=pf[:]), r=["pf"], w=["ka"])
                range_reduce_sin(sinF, "sinF")
                S.op("dve", lambda e: e.tensor_scalar(out=ka[:], in0=pf[:], scalar1=float(math.pi / 2), scalar2=None, op0=ALU.add), r=["pf"], w=["ka"])
                range_reduce_sin(cosF, "cosF")
                for which in range(2):
                    for h in range(4):
                        col = which * 512 + h * 128

                        def mm(e, col=col, xT2=xT2):
                            for k in range(8):
                                ins = e.matmul(P[0][:], lhsT=wq2[:, k, col:col + 128], rhs=xT2[:, k, :], start=(k == 0), stop=(k == 7))
                            return ins
                        S.op("pe", mm, r=["wq2", xk], w=[pk(0)])
                        S.op("act", lambda e: e.activation(out=mb[:], in_=P[0][:], func=AF.Copy), r=[pk(0)], w=["mb"])
                        S.op("pe", lambda e: e.matmul(P[1][:], lhsT=perm_b, rhs=mb[:], start=True, stop=True), r=["mb", "Cb"], w=[pk(1)])
                        S.op("dve", lambda e: e.tensor_tensor(out=r1[:], in0=mb[:], in1=cosF[:], op=ALU.mult), r=["mb", "cosF"], w=["r1"])
                        S.op("dve", lambda e: e.tensor_tensor(out=r2[:], in0=P[1][:], in1=sinF[:], op=ALU.mult), r=[pk(1), "sinF"], w=["r2"])
                        if which == 0:
                            S.op("dve", lambda e: e.tensor_tensor(out=r1[:], in0=r1[:], in1=r2[:], op=ALU.add), r=["r1", "r2"], w=["r1"])
                            S.op("dve", (lambda h: lambda e: e.tensor_scalar(out=Q0[:, h, :], in0=r1[:], scalar1=c0, scalar2=None, op0=ALU.mult))(h), r=["r1", "C"], w=["Q0"])
                            S.op("dve", (lambda h: lambda e: e.tensor_scalar(out=Q1[:, h, :], in0=r1[:], scalar1=c1, scalar2=None, op0=ALU.mult))(h), r=["r1", "C"], w=["Q1"])
                        else:
                            S.op("dve", (lambda h, t0: lambda e: e.tensor_tensor(out=KT[:, h, t0:t0 + 512], in0=r1[:], in1=r2[:], op=ALU.add))(h, t0), r=["r1", "r2"], w=["KT"])
                for t in range(4):
                    c0_ = t * 128
                    ti = b * 4 + t

                    def mmv(e, c0_=c0_, xT2=xT2):
                        for k in range(8):
                            ins = e.matmul(P[0][:], lhsT=xT2[:, k, c0_:c0_ + 128], rhs=wv2[:, k, :], start=(k == 0), stop=(k == 7))
                        return ins
                    S.op("pe", mmv, r=["wv2", xk], w=[pk(0)])
                    S.op("act", (lambda ti: lambda e: e.activation(out=Vs[:, ti, :, 0:128], in_=P[0][:].rearrange("p (h d) -> p h d", h=4), func=AF.Copy))(ti),
                         r=[pk(0)], w=["Vs"])
                nkt = 4 * b + 4
                it = 0
                for h in range(4):
                    for c in range(2):
                        Qc = Q0 if c == 0 else Q1
                        qk_ = "Q0" if c == 0 else "Q1"
                        accb = (2, 3) if c == 0 else (4, 5)
                        for kt in range(nkt):
                            j = kt - 4 * b
                            q0 = 128 * j if j >= 0 else 0
                            sp_ = it % 2
                            it += 1
                            S.op("pe", (lambda sp_, h, kt, q0, Qc: lambda e: e.matmul(P[sp_][:, q0:512], lhsT=KT[:, h, kt * 128:(kt + 1) * 128], rhs=Qc[:, h, q0:512],
                                                                                      start=True, stop=True))(sp_, h, kt, q0, Qc), r=["KT", qk_], w=[pk(sp_)])
                            S.op("act", (lambda sp_, q0: lambda e: e.activation(out=pT[sp_][:, q0:512], in_=P[sp_][:, q0:512], func=AF.Exp, scale=0.125))(sp_, q0),
                                 r=[pk(sp_)], w=["pT%d" % sp_])
                            if j >= 0:
                                S.op("pool", (lambda sp_, q0: lambda e: e.tensor_tensor(out=pT[sp_][:, q0:q0 + 128], in0=pT[sp_][:, q0:q0 + 128], in1=causal_b, op=ALU.mult))(sp_, q0),
                                     r=["pT%d" % sp_, "Cb"], w=["pT%d" % sp_])

                            def mmpv(e, sp_=sp_, h=h, kt=kt, j=j, accb=accb, b=b):
                                ins = None
                                for qi in range(max(j, 0), 4):
                                    bank = P[accb[qi // 2]]
                                    o0 = (qi % 2) * 256
                                    ins = e.matmul(bank[:, o0:o0 + 129], lhsT=pT[sp_][:, qi * 128:(qi + 1) * 128], rhs=Vs[:, kt, h, 0:129],
                                                   start=(kt == 0 and qi % 2 == 0), stop=(kt == 4 * b + qi), skip_group_check=True)
                                return ins
                            S.op("pe", mmpv, r=["pT%d" % sp_, "Vs"], w=[pk(accb[0]), pk(accb[1])])
                    for qi in range(4):
                        o0 = (qi % 2) * 256
                        bA = P[2 + qi // 2]
                        bB = P[4 + qi // 2]
                        S.op("dve", (lambda qi, bA, o0: lambda e: e.tensor_copy(out=den[:, qi:qi + 1], in_=bA[:, o0 + 128:o0 + 129]))(qi, bA, o0), r=[pk(2 + qi // 2)], w=["den"])
                        S.op("dve", (lambda qi, bB, o0: lambda e: e.tensor_copy(out=den[:, 4 + qi:5 + qi], in_=bB[:, o0 + 128:o0 + 129]))(qi, bB, o0), r=[pk(4 + qi // 2)], w=["den"])
                    S.op("dve", lambda e: e.reciprocal(out=den[:, 8:16], in_=den[:, 0:8]), r=["den"], w=["den"])
                    S.op("dve", lambda e: e.tensor_scalar(out=den[:, 12:16], in0=den[:, 12:16], scalar1=lam[:, 2:3], scalar2=None, op0=ALU.mult), r=["den", "lam"], w=["den"])
                    for qi in range(4):
                        o0 = (qi % 2) * 256
                        bA = P[2 + qi // 2]
                        bB = P[4 + qi // 2]
                        S.op("dve", (lambda qi, bA, o0, h: lambda e: e.tensor_scalar(out=od[:, qi, h, :], in0=bA[:, o0:o0 + 128], scalar1=den[:, 8 + qi:9 + qi], scalar2=None,
                                                                                      op0=ALU.mult))(qi, bA, o0, h), r=[pk(2 + qi // 2), "den"], w=["od"])
                        S.op("dve", (lambda qi, bB, o0, h: lambda e: e.scalar_tensor_tensor(out=od[:, qi, h, :], in0=bB[:, o0:o0 + 128], scalar=den[:, 12 + qi:13 + qi],
                                                                                             in1=od[:, qi, h, :], op0=ALU.mult, op1=ALU.add))(qi, bB, o0, h),
                             r=[pk(4 + qi // 2), "den", "od"], w=["od"])
                for qi in range(4):
                    ti = b * 4 + qi
                    odf = od[:, qi, :, :].rearrange("p h d -> p (h d)")
                    S.op("pool", (lambda odf: lambda e: e.tensor_tensor(out=r1[:], in0=odf, in1=odf, op=ALU.mult))(odf), r=["od"], w=["r1"])
                    S.op("dve", lambda e: e.tensor_reduce(out=lam[:, 4:8], in_=r1[:].rearrange("p (h d) -> p h d", h=4), axis=AX.X, op=ALU.add), r=["r1"], w=["lam2"])
                    S.op("act", lambda e: e.activation(out=lam[:, 4:8], in_=lam[:, 4:8], func=AF.Ln, scale=1.0 / 128.0, bias=1e-6), r=["lam2"], w=["lam2"])
                    S.op("act", lambda e: e.activation(out=lam[:, 4:8], in_=lam[:, 4:8], func=AF.Exp, scale=-0.5), r=["lam2"], w=["lam2"])
                    S.op("dve", (lambda qi: lambda e: e.tensor_tensor(out=r1[:].rearrange("p (h d) -> p h d", h=4), in0=od[:, qi, :, :],
                                                                      in1=lam[:, 4:8].unsqueeze(2).to_broadcast([128, 4, 128]), op=ALU.mult))(qi), r=["od", "lam2"], w=["r1"])
                    S.op("dve", (lambda qi: lambda e: e.scalar_tensor_tensor(out=ot[:, qi, 512:1024], in0=r1[:], scalar=float(1.0 - LAM_INIT), in1=dnw[:],
                                                                             op0=ALU.mult, op1=ALU.mult))(qi), r=["r1", "dnw"], w=["ot%d" % qi])
                    S.dma("sp", (lambda qi, ti: lambda e: e.dma_start(out=ot[:, qi, 0:512], in_=ogs_d[ti, :, :]))(qi, ti), "ogr", r=["ogs%d" % ti], w=["ot%d" % qi])

                    def trO(e, qi=qi):
                        for k in range(8):
                            ins = e.transpose(PT2[:, k * 128:(k + 1) * 128], ot[:, qi, k * 128:(k + 1) * 128], ident_b)
                        return ins
                    S.op("pe", trO, r=["ot%d" % qi, "Cb"], w=[pk(6)])
                    S.op("act", lambda e: e.activation(out=oT[:], in_=PT2[:, :].rearrange("p (k t) -> p k t", k=8), func=AF.Copy), r=[pk(6)], w=["oT"])
                    for half in range(2):
                        def mmo(e, half=half):
                            for k in range(8):
                                ins = e.matmul(P[6 + half][:], lhsT=oT[:, k, :], rhs=wo[:, k, half * 512:(half + 1) * 512], start=(k == 0), stop=(k == 7))
                            return ins
                        S.op("pe", mmo, r=["oT", "wo"], w=[pk(6 + half)])
                        S.op("act", (lambda half: lambda e: e.activation(out=mixt[:, half * 512:(half + 1) * 512], in_=P[6 + half][:], func=AF.Copy))(half),
                             r=[pk(6 + half)], w=["mixt"])
                    S.dma("sp", (lambda ti: lambda e: e.dma_start(out=mixs_d[ti, :, :], in_=mixt[:]))(ti), "mixw", r=["mixt"], w=["mixs%d" % ti])
                    if debug:
                        S.dma("sp", (lambda ti: lambda e: e.dma_start(out=dbg["mix"][ti, :, :], in_=mixt[:]))(ti), "dbg", r=["mixt"])

        fence()
        S.enabled = "C" in phases
        with contextlib.ExitStack() as es:
            acc = sbt(es, "acc", [128, 16, 1024])
            h1T = sbt(es, "h1T", [128, 8, 2048], BF16)
            gates = sbt(es, "gates", [128, 16, 32])
            m01 = sbt(es, "m01", [128, 2])
            rbb = sbt(es, "rbb", [128, 32])
            bgu = sbt(es, "bgu", [128, 512])
            st = sbt(es, "st", [128, 32])
            S.dma("sp", lambda e: e.dma_start(out=m01[:], in_=m01_d[:, :]), "c3", w=["m01"])
            S.dma("sp", lambda e: e.dma_start(out=rbb[:], in_=rbb_d[:, :]), "c3", w=["rbb"])
            S.dma("sp", lambda e: e.dma_start(out=bgu[:], in_=bgu_d[:, :]), "c3", w=["bgu"])

            def layer_norm(src, srck, dst, dstk, gam, bet, gk, scr, scrk):
                S.op("dve", lambda e: e.tensor_reduce(out=st[:, 0:1], in_=src, axis=AX.X, op=ALU.add), r=[srck], w=["st"])
                S.op("act", lambda e: e.activation(out=scr, in_=src, func=AF.Square), r=[srck], w=[scrk])
                S.op("dve", lambda e: e.tensor_reduce(out=st[:, 1:2], in_=scr, axis=AX.X, op=ALU.add), r=[scrk], w=["st"])
                S.op("dve", lambda e: e.tensor_scalar(out=st[:, 0:2], in0=st[:, 0:2], scalar1=1.0 / 1024.0, scalar2=None, op0=ALU.mult), r=["st"], w=["st"])
                S.op("dve", lambda e: e.tensor_tensor(out=st[:, 2:3], in0=st[:, 0:1], in1=st[:, 0:1], op=ALU.mult), r=["st"], w=["st"])
                S.op("dve", lambda e: e.tensor_tensor(out=st[:, 2:3], in0=st[:, 1:2], in1=st[:, 2:3], op=ALU.subtract), r=["st"], w=["st"])
                S.op("act", lambda e: e.activation(out=st[:, 2:3], in_=st[:, 2:3], func=AF.Ln, bias=1e-5), r=["st"], w=["st"])
                S.op("act", lambda e: e.activation(out=st[:, 2:3], in_=st[:, 2:3], func=AF.Exp, scale=-0.5), r=["st"], w=["st"])
                S.op("dve", lambda e: e.tensor_scalar(out=dst, in0=src, scalar1=st[:, 0:1], scalar2=st[:, 2:3], op0=ALU.subtract, op1=ALU.mult), r=[srck, "st"], w=[dstk])
                S.op("dve", lambda e: e.tensor_tensor(out=dst, in0=dst, in1=gam, op=ALU.mult), r=[dstk, gk], w=[dstk])
                S.op("dve", lambda e: e.tensor_tensor(out=dst, in0=dst, in1=bet, op=ALU.add), r=[dstk, gk], w=[dstk])

            with contextlib.ExitStack() as es2:
                lng = sbt(es2, "lng", [128, 1024])
                lnb = sbt(es2, "lnb", [128, 1024])
                rw = sbt(es2, "rw", [128, 8, 32])
                rwh = sbt(es2, "rwh", [128, 8, 32], BF16)
                rwl = sbt(es2, "rwl", [128, 8, 32], BF16)
                bdn = sbt(es2, "bdn", [128, 1024])
                bdh = sbt(es2, "bdh", [128, 1024], BF16)
                bdl = sbt(es2, "bdl", [128, 1024], BF16)
                h1hl = sbt(es2, "h1hl", [128, 2, 1024], BF16)
                hTl = sbt(es2, "hTl", [128, 8, 128], BF16)
                gpad = sbt(es2, "gpad", [128, 2, 128], BF16)
                xo = sbt(es2, "xo", [128, 1024])
                mA = sbt(es2, "mA", [128, 1024])
                mB = sbt(es2, "mB", [128, 1024])
                h1 = sbt(es2, "h1", [128, 1024])
                scr = sbt(es2, "scr", [128, 1024])
                gT = sbt(es2, "gT", [128, 2, 128], BF16)
                lg = sbt(es2, "lg", [128, 32])
                S.dma("sp", lambda e: e.dma_start(out=lng[:], in_=lnp_d[0, :, :]), "c3", w=["lng"])
                S.dma("sp", lambda e: e.dma_start(out=lnb[:], in_=lnp_d[1, :, :]), "c3", w=["lng"])
                S.dma("sp", lambda e: e.dma_start(out=rw[:], in_=rw_d[:, :, :].rearrange("k p n -> p k n")), "c3", w=["rw"])
                S.dma("sp", lambda e: e.dma_start(out=bdn[:], in_=bdn_d[:, :]), "c3", w=["bdn"])
                S.op("act", lambda e: e.activation(out=rwh[:], in_=rw[:], func=AF.Copy), r=["rw"], w=["rwh"])
                S.op("dve", lambda e: e.tensor_tensor(out=rwl[:], in0=rw[:], in1=rwh[:], op=ALU.subtract), r=["rw", "rwh"], w=["rwl"])
                S.op("act", lambda e: e.activation(out=bdh[:], in_=bdn[:], func=AF.Copy), r=["bdn"], w=["bdh"])
                S.op("dve", lambda e: e.tensor_tensor(out=bdl[:], in0=bdn[:], in1=bdh[:], op=ALU.subtract), r=["bdn", "bdh"], w=["bdl"])
                S.op("dve", lambda e: e.memset(gpad[:].rearrange("p a b -> p (a b)"), 0.0), w=["gpad"])
                for j in range(16):
                    S.dma("sp", (lambda j: lambda e: e.dma_start(out=xo[:], in_=xown_d[j, :, :]))(j), "ldx", w=["xo"])
                    S.dma("sp", (lambda j: lambda e: e.dma_start(out=mA[:], in_=mixs_d[j, :, :]))(j), "ldx", r=["mixs%d" % j], w=["mA"])
                    S.dma("sp", (lambda j: lambda e: e.dma_start(out=mB[:], in_=mixs_d[j + 16, :, :]))(j), "ldx", r=["mixs%d" % (j + 16)], w=["mB"])
                    S.op("dve", lambda e: e.tensor_scalar(out=xo[:], in0=xo[:], scalar1=float(DN_ALPHA), scalar2=None, op0=ALU.mult), r=["xo"], w=["xo"])
                    S.op("dve", lambda e: e.scalar_tensor_tensor(out=xo[:], in0=mA[:], scalar=m01[:, 0:1], in1=xo[:], op0=ALU.mult, op1=ALU.add), r=["mA", "m01", "xo"], w=["xo"])
                    S.op("dve", lambda e: e.scalar_tensor_tensor(out=xo[:], in0=mB[:], scalar=m01[:, 1:2], in1=xo[:], op0=ALU.mult, op1=ALU.add), r=["mB", "m01", "xo"], w=["xo"])
                    layer_norm(xo[:], "xo", h1[:], "h1", lng[:], lnb[:], "lng", scr[:], "scr")
                    if debug:
                        S.dma("sp", (lambda j: lambda e: e.dma_start(out=dbg["h1"][j, :, :], in_=h1[:]))(j), "dbg", r=["h1"])

                    S.op("act", lambda e: e.activation(out=h1hl[:, 0, :], in_=h1[:], func=AF.Copy), r=["h1"], w=["h1hl"])
                    S.op("dve", lambda e: e.tensor_tensor(out=h1hl[:, 1, :], in0=h1[:], in1=h1hl[:, 0, :], op=ALU.subtract), r=["h1", "h1hl"], w=["h1hl"])
                    P0T = P[0][:].bitcast(BF16)
                    P1T = P[1][:].bitcast(BF16)

                    def trh(e):
                        for k in range(8):
                            e.transpose(P0T[:, k * 128:(k + 1) * 128], h1hl[:, 0, k * 128:(k + 1) * 128], ident_b)
                        for k in range(8):
                            ins = e.transpose(P1T[:, k * 128:(k + 1) * 128], h1hl[:, 1, k * 128:(k + 1) * 128], ident_b)
                        return ins
                    S.op("pe", trh, r=["h1hl", "Cb"], w=[pk(0), pk(1)])
                    S.op("act", (lambda j: lambda e: e.activation(out=h1T[:, :, j * 128:(j + 1) * 128], in_=P0T[:, :].rearrange("p (k t) -> p k t", k=8), func=AF.Copy))(j),
                         r=[pk(0)], w=["h1T"])
                    S.op("dve", lambda e: e.tensor_copy(out=hTl[:], in_=P1T[:, :].rearrange("p (k t) -> p k t", k=8)), r=[pk(1)], w=["hTl"])

                    def mmr(e, j=j):
                        n = 0
                        for k in range(8):
                            for (a, bb) in ((h1T[:, k, j * 128:(j + 1) * 128], rwh[:, k, :]), (hTl[:, k, :], rwh[:, k, :]), (h1T[:, k, j * 128:(j + 1) * 128], rwl[:, k, :])):
                                ins = e.matmul(P[2][:, 0:32], lhsT=a, rhs=bb, start=(n == 0), stop=(n == 23))
                                n += 1
                        return ins
                    S.op("pe", mmr, r=["h1T", "hTl", "rwh", "rwl"], w=[pk(2)])
                    S.op("dve", lambda e: e.tensor_tensor(out=lg[:], in0=P[2][:, 0:32], in1=rbb[:], op=ALU.add), r=[pk(2), "rbb"], w=["lg"])
                    S.op("dve", lambda e: e.max(out=st[:, 8:16], in_=lg[:]), r=["lg"], w=["st2"])
                    S.op("dve", lambda e: e.tensor_scalar(out=st[:, 16:17], in0=st[:, 8:9], scalar1=-1.0, scalar2=None, op0=ALU.mult), r=["st2"], w=["st2"])
                    S.op("act", lambda e: e.activation(out=scr[:, 0:32], in_=lg[:], func=AF.Exp, bias=st[:, 16:17]), r=["lg", "st2"], w=["scr"])
                    S.op("dve", lambda e: e.tensor_scalar(out=lg[:], in0=lg[:], scalar1=st[:, 11:12], scalar2=None, op0=ALU.is_ge), r=["lg", "st2"], w=["lg"])
                    S.op("dve", lambda e: e.tensor_tensor(out=lg[:], in0=lg[:], in1=scr[:, 0:32], op=ALU.mult), r=["lg", "scr"], w=["lg"])
                    S.op("dve", lambda e: e.tensor_reduce(out=st[:, 17:18], in_=lg[:], axis=AX.X, op=ALU.add), r=["lg"], w=["st2"])
                    S.op("dve", lambda e: e.reciprocal(out=st[:, 17:18], in_=st[:, 17:18]), r=["st2"], w=["st2"])
                    S.op("dve", (lambda j: lambda e: e.tensor_scalar(out=gates[:, j, :], in0=lg[:], scalar1=st[:, 17:18], scalar2=None, op0=ALU.mult))(j), r=["lg", "st2"], w=["gates"])
                    S.op("act", (lambda j: lambda e: e.activation(out=gpad[:, 0, 0:32], in_=gates[:, j, :], func=AF.Copy))(j), r=["gates"], w=["gpad"])
                    S.op("dve", (lambda j: lambda e: e.tensor_tensor(out=gpad[:, 1, 0:32], in0=gates[:, j, :], in1=gpad[:, 0, 0:32], op=ALU.subtract))(j), r=["gates", "gpad"], w=["gpad"])
                    P3T = P[3][:].bitcast(BF16)

                    def trg(e):
                        e.transpose(P3T[:, 0:128], gpad[:, 0, :], ident_b)
                        return e.transpose(P3T[:, 128:256], gpad[:, 1, :], ident_b)
                    S.op("pe", trg, r=["gpad", "Cb"], w=[pk(3)])
                    S.op("act", lambda e: e.activation(out=gT[:], in_=P3T[:, 0:256].rearrange("p (a t) -> p a t", a=2), func=AF.Copy), r=[pk(3)], w=["gT"])
                    for half in range(2):
                        def mmbd(e, half=half):
                            hs_ = slice(half * 512, (half + 1) * 512)
                            e.matmul(P[4 + half][:], lhsT=gT[:, 0, :], rhs=bdh[:, hs_], start=True, stop=False)
                            e.matmul(P[4 + half][:], lhsT=gT[:, 1, :], rhs=bdh[:, hs_], start=False, stop=False)
                            return e.matmul(P[4 + half][:], lhsT=gT[:, 0, :], rhs=bdl[:, hs_], start=False, stop=True)
                        S.op("pe", mmbd, r=["gT", "bdh", "bdl"], w=[pk(4 + half)])
                        S.op("dve", (lambda half, j: lambda e: e.scalar_tensor_tensor(out=acc[:, j, half * 512:(half + 1) * 512], in0=h1[:, half * 512:(half + 1) * 512],
                                                                                        scalar=float(DN_ALPHA), in1=P[4 + half][:], op0=ALU.mult, op1=ALU.add))(half, j),
                             r=["h1", pk(4 + half)], w=["acc%d" % j])

            fence()
            S.enabled = "D" in phases
            with contextlib.ExitStack() as es2:
                wg = [sbt(es2, "ewg%d" % i, [128, 8, 1024], BF16) for i in range(2)]
                wu = [sbt(es2, "ewu%d" % i, [128, 8, 1024], BF16) for i in range(2)]
                wd = sbt(es2, "wd", [128, 8, 1024], BF16)
                aT = [sbt(es2, "aT%d" % i, [128, 8, 512], BF16) for i in range(2)]
                gsb = sbt(es2, "gsb", [128, 512])
                usb = sbt(es2, "usb", [128, 512], BF16)
                sgs = sbt(es2, "sgs", [128, 512], BF16)
                ucl = sbt(es2, "ucl", [128, 512], BF16)
                it = 0
                def load_gu(ex):
                    wb_ = ex % 2
                    for k in range(8):
                        S.dma("pool", (lambda ex, k, wb_: lambda e: e.dma_start(out=wg[wb_][:, k, :], in_=wgu_d[ex, k, :, 0:1024]))(ex, k, wb_), "wg%d" % wb_, w=["wg%d" % wb_])
                        S.dma("pool", (lambda ex, k, wb_: lambda e: e.dma_start(out=wu[wb_][:, k, :], in_=wgu_d[ex, k, :, 1024:2048]))(ex, k, wb_), "wu%d" % wb_, w=["wu%d" % wb_])

                def load_d(ex):
                    for k in range(8):
                        S.dma("pool", (lambda ex, k: lambda e: e.dma_start(out=wd[:, k, :], in_=wdn_d[ex, k, :, :]))(ex, k), "wd", w=["wd"])
                load_gu(0)
                load_d(0)
                for ex in range(32):
                    wb_ = ex % 2
                    if ex + 1 < 32:
                        load_gu(ex + 1)
                    for tb in range(4):
                        ab = it % 2
                        it += 1
                        for fc in range(8):
                            pg_, pu_ = (0, 1) if fc % 2 == 0 else (2, 3)

                            def mmg(e, fc=fc, tb=tb, wb_=wb_, pg_=pg_, pu_=pu_):
                                for k in range(8):
                                    e.matmul(P[pg_][:], lhsT=wg[wb_][:, k, fc * 128:(fc + 1) * 128], rhs=h1T[:, k, tb * 512:(tb + 1) * 512], start=(k == 0), stop=(k == 7))
                                for k in range(8):
                                    ins = e.matmul(P[pu_][:], lhsT=wu[wb_][:, k, fc * 128:(fc + 1) * 128], rhs=h1T[:, k, tb * 512:(tb + 1) * 512], start=(k == 0), stop=(k == 7))
                                return ins
                            S.op("pe", mmg, r=["wg%d" % wb_, "wu%d" % wb_, "h1T"], w=[pk(pg_), pk(pu_)])
                            bgc = bgu[:, ex * 16 + fc:ex * 16 + fc + 1]
                            buc = bgu[:, ex * 16 + 8 + fc:ex * 16 + 8 + fc + 1]
                            S.op("dve", (lambda pg_, bgc: lambda e: e.tensor_scalar(out=gsb[:], in0=P[pg_][:], scalar1=bgc, scalar2=7.0, op0=ALU.add, op1=ALU.min))(pg_, bgc),
                                 r=[pk(pg_), "bgu"], w=["gsb"])
                            S.op("act", (lambda pu_, buc: lambda e: e.activation(out=usb[:], in_=P[pu_][:], func=AF.Identity, bias=buc))(pu_, buc), r=[pk(pu_), "bgu"], w=["usb"])
                            S.op("act", lambda e: e.activation(out=sgs[:], in_=gsb[:], func=AF.Sigmoid, scale=1.702), r=["gsb"], w=["sgs"])
                            S.op("dve", lambda e: e.tensor_scalar(out=ucl[:], in0=usb[:], scalar1=7.0, scalar2=-7.0, op0=ALU.min, op1=ALU.max), r=["usb"], w=["ucl"])
                            S.op("dve", lambda e: e.tensor_tensor(out=gsb[:], in0=gsb[:], in1=sgs[:], op=ALU.mult), r=["gsb", "sgs"], w=["gsb"])
                            S.op("dve", (lambda ab, fc: lambda e: e.scalar_tensor_tensor(out=aT[ab][:, fc, :], in0=ucl[:], scalar=1.0, in1=gsb[:], op0=ALU.add, op1=ALU.mult))(ab, fc),
                                 r=["ucl", "gsb"], w=["aT%d" % ab])
                        for tt in range(4):
                            j = tb * 4 + tt
                            for half in range(2):
                                pb = 4 + half + 2 * (tt % 2)

                                def mmd(e, ab=ab, tt=tt, half=half, pb=pb):
                                    for fc in range(8):
                                        ins = e.matmul(P[pb][:], lhsT=aT[ab][:, fc, tt * 128:(tt + 1) * 128], rhs=wd[:, fc, half * 512:(half + 1) * 512], start=(fc == 0), stop=(fc == 7))
                                    return ins
                                S.op("pe", mmd, r=["aT%d" % ab, "wd"], w=[pk(pb)])
                                S.op("dve", (lambda j, half, pb, ex: lambda e: e.scalar_tensor_tensor(out=acc[:, j, half * 512:(half + 1) * 512], in0=P[pb][:],
                                                                                                       scalar=gates[:, j, ex:ex + 1], in1=acc[:, j, half * 512:(half + 1) * 512],
                                                                                                       op0=ALU.mult, op1=ALU.add))(j, half, pb, ex),
                                     r=[pk(pb), "gates", "acc%d" % j], w=["acc%d" % j])
                    if ex + 1 < 32:
                        load_d(ex + 1)

            fence()
            S.enabled = "E" in phases
            with contextlib.ExitStack() as es2:
                l2g = sbt(es2, "l2g", [128, 1024])
                l2b = sbt(es2, "l2b", [128, 1024])
                l3g = sbt(es2, "l3g", [128, 1024])
                l3b = sbt(es2, "l3b", [128, 1024])
                pgb = sbt(es2, "pgb", [128, 1024])
                wpg = sbt(es2, "wpg", [128, 8, 1024], BF16)
                plw = sbt(es2, "plw", [128, 2, 1024], BF16)
                ptn = sbt(es2, "ptn", [128, 2, 2048], BF16)
                h2 = sbt(es2, "h2", [128, 1024])
                h2b = sbt(es2, "h2b", [128, 1024], BF16)
                h2T = sbt(es2, "h2T", [128, 8, 128], BF16)
                scrE = sbt(es2, "scr2", [128, 1024])
                gt = sbt(es2, "gt", [128, 1024])
                y3 = sbt(es2, "y3", [128, 1024])
                PT3 = P[0][:].bitcast(BF16)
                S.dma("sp", lambda e: e.dma_start(out=l2g[:], in_=lnp_d[2, :, :]), "c4", w=["l2"])
                S.dma("sp", lambda e: e.dma_start(out=l2b[:], in_=lnp_d[3, :, :]), "c4", w=["l2"])
                S.dma("sp", lambda e: e.dma_start(out=l3g[:], in_=lnp_d[4, :, :]), "c4", w=["l3"])
                S.dma("sp", lambda e: e.dma_start(out=l3b[:], in_=lnp_d[5, :, :]), "c4", w=["l3"])
                S.dma("sp", lambda e: e.dma_start(out=pgb[:], in_=pgb_d[:, :]), "c4", w=["pgb"])
                for k in range(8):
                    S.dma("pool", (lambda k: lambda e: e.dma_start(out=wpg[:, k, :], in_=wpg_d[k, :, :]))(k), "w4", w=["wpg"])
                for k in range(2):
                    S.dma("pool", (lambda k: lambda e: e.dma_start(out=plw[:, k, :], in_=plew_d[k, :, :]))(k), "w4", w=["plw"])
                    S.dma("pool", (lambda k: lambda e: e.dma_start(out=ptn[:, k, :], in_=ptown_d[k, :, :]))(k), "w4", w=["ptn"])
                for j in range(16):
                    if debug:
                        S.dma("sp", (lambda j: lambda e: e.dma_start(out=dbg["acc"][j, :, :], in_=acc[:, j, :]))(j), "dbg", r=["acc%d" % j])
                    layer_norm(acc[:, j, :], "acc%d" % j, h2[:], "h2", l2g[:], l2b[:], "l2", scrE[:], "scr2")
                    S.op("act", lambda e: e.activation(out=h2b[:], in_=h2[:], func=AF.Copy), r=["h2"], w=["h2b"])

                    def trh2(e):
                        for k in range(8):
                            ins = e.transpose(PT3[:, k * 128:(k + 1) * 128], h2b[:, k * 128:(k + 1) * 128], ident_b)
                        return ins
                    S.op("pe", trh2, r=["h2b", "Cb"], w=[pk(0)])
                    S.op("act", lambda e: e.activation(out=h2T[:], in_=PT3[:, :].rearrange("p (k t) -> p k t", k=8), func=AF.Copy), r=[pk(0)], w=["h2T"])
                    for half in range(2):
                        hs_ = slice(half * 512, (half + 1) * 512)

                        def mmgt(e, hs_=hs_, half=half):
                            for k in range(8):
                                ins = e.matmul(P[2 + half][:], lhsT=h2T[:, k, :], rhs=wpg[:, k, hs_], start=(k == 0), stop=(k == 7))
                            return ins
                        S.op("pe", mmgt, r=["h2T", "wpg"], w=[pk(2 + half)])

                        def mmpl(e, hs_=hs_, half=half, j=j):
                            for k in range(2):
                                ins = e.matmul(P[4 + half][:], lhsT=ptn[:, k, j * 128:(j + 1) * 128], rhs=plw[:, k, hs_], start=(k == 0), stop=(k == 1))
                            return ins
                        S.op("pe", mmpl, r=["ptn", "plw"], w=[pk(4 + half)])
                        S.op("dve", (lambda hs_, half: lambda e: e.tensor_tensor(out=gt[:, hs_], in0=P[2 + half][:], in1=pgb[:, hs_], op=ALU.add))(hs_, half),
                             r=[pk(2 + half), "pgb"], w=["gt"])
                        S.op("act", (lambda hs_: lambda e: e.activation(out=gt[:, hs_], in_=gt[:, hs_], func=AF.Sigmoid))(hs_), r=["gt"], w=["gt"])
                        S.op("dve", (lambda hs_, half: lambda e: e.tensor_tensor(out=gt[:, hs_], in0=P[4 + half][:], in1=gt[:, hs_], op=ALU.mult))(hs_, half),
                             r=[pk(4 + half), "gt"], w=["gt"])
                    S.op("dve", lambda e: e.scalar_tensor_tensor(out=y3[:], in0=h2[:], scalar=float(DN_ALPHA), in1=gt[:], op0=ALU.mult, op1=ALU.add), r=["h2", "gt"], w=["y3"])
                    layer_norm(y3[:], "y3", y3[:], "y3", l3g[:], l3b[:], "l3", scrE[:], "scr2")
                    S.dma("sp", (lambda j: lambda e: e.dma_start(out=out_d[j, :, :], in_=y3[:]))(j), "outw", r=["y3"])

        S.emit(final_wait_slots=[x for x in ["outw", "dbg", "ogw", "mixw"] if x in S.dma_slots])
    return nc


def _consts():
    c = np.zeros((128, NCONST), np.float32)
    i = np.arange(128)
    same = (i[:, None] // 64) == (i[None, :] // 64)
    c[:, 0:128] = np.eye(128)
    c[:, 128:256] = ((i[:, None] <= i[None, :]) & same)
    c[:, 256:384] = same
    c[:, 384:512] = np.where((i[None, :] < i[:, None]) & same, 0.0, BIG)
    c[:, 512:640] = np.where((i[:, None] <= i[None, :]) & same, 0.0, BIG)
    c[:, 640:768] = (i[None, :] >= i[:, None])
    c[:, 768:896] = 1.0
    pm = np.zeros((128, 128), np.float32)
    for m in range(128):
        d = m % 64
        if d < 8:
            pm[m + 8, m] = -1.0
        elif d < 16:
            pm[m - 8, m] = 1.0
    c[:, 896:1024] = pm
    c[:, 1024] = (i < 64)
    c[:, 1025] = (i >= 64)
    inv_freq = (500000.0 ** (-np.arange(0, 16, 2, dtype=np.float32) / np.float32(16))).astype(np.float32)
    d = i % 64
    c[:, 1026] = np.where(d < 16, inv_freq[d % 8], 0.0)
    c[:, 1027] = (i < 64)
    c[:, 1028] = (i >= 64)
    return c


def _bc(v, n=128):
    return np.ascontiguousarray(np.broadcast_to(np.asarray(v, np.float32).reshape(1, -1), (n, np.asarray(v).size)))


def _prep(inp):
    f = lambda a: np.ascontiguousarray(np.asarray(a, np.float32))
    x = f(inp["x"])
    w_in = f(inp["w_in"][0])
    O = [0, 512, 1024, 1536, 2048, 2052, 2056, 2568, 3080, 3592]
    gqkv = w_in[:, 0:1536]
    gz = w_in[:, O[3]:O[4]]
    gba = w_in[:, O[4]:O[6]]
    dq = w_in[:, O[6]:O[7]]
    dk = w_in[:, O[7]:O[8]]
    dv = w_in[:, O[8]:O[9]]
    wfm = np.concatenate([gqkv, dq, dk], 1).reshape(8, 128, 2560)
    wtm = np.concatenate([gz, dv, gba], 1).reshape(8, 128, 1032)
    convw = f(inp["conv_w"][0]).T.reshape(12, 128, 4).transpose(1, 0, 2).reshape(128, 48)
    hs = np.concatenate([_bc(inp["a_log"][0]), _bc(inp["dt_bias"][0])], 1)
    gnw4 = _bc(np.tile(f(inp["gdn_norm_w"][0]), 4))
    dnw4 = _bc(np.tile(f(inp["diff_norm_w"][0]), 4))
    lamv = np.concatenate([_bc(inp["lam_q1"][0]), _bc(inp["lam_k1"][0]), _bc(inp["lam_q2"][0]), _bc(inp["lam_k2"][0])], 1)
    shared = dict(
        wfm=np.ascontiguousarray(wfm), wtm=np.ascontiguousarray(wtm), convw=np.ascontiguousarray(convw), hs=np.ascontiguousarray(hs),
        gnw4=gnw4, dnw4=dnw4, lamv=np.ascontiguousarray(lamv),
        wout=f(inp["w_out"][0]).reshape(8, 128, 1024), consts=_consts(),
        msk4=np.ascontiguousarray(np.concatenate([np.tile(_consts()[:, 384:512], (1, 4)), np.tile(_consts()[:, 512:640], (1, 4))], 1)),
        lnp=np.ascontiguousarray(np.stack([_bc(inp[k][0]) for k in ("ln1_g", "ln1_b", "ln2_g", "ln2_b", "ln3_g", "ln3_b")], 0)),
        rw=f(inp["router_w"][0]).reshape(8, 128, 32), rbb=_bc(inp["router_b"][0]),
        wgu=f(inp["w_gu"][0]).reshape(32, 8, 128, 2048),
        bgu=np.ascontiguousarray(f(inp["b_gu"][0]).reshape(32, 16, 128).transpose(2, 0, 1).reshape(128, 512)),
        wdn=f(inp["w_down"][0]).reshape(32, 8, 128, 1024), bdn=np.concatenate([f(inp["b_down"][0]), np.zeros((96, 1024), np.float32)], 0),
        wpg=f(inp["ple_gate_w"][0]).reshape(8, 128, 1024), pgb=_bc(inp["ple_gate_b"][0]),
        plew=f(inp["ple_w"][0]).reshape(2, 128, 1024),
    )
    pos = np.asarray(inp["positions"], np.int32)
    p = f(inp["p"][0])
    maps = []
    for c in range(8):
        b, half = c // 2, c % 2
        m = dict(shared)
        m["xT"] = np.ascontiguousarray(x[b].T).reshape(8, 128, SEQ)
        m["posb"] = np.ascontiguousarray(np.broadcast_to(pos[b][None, :], (128, SEQ)))
        m01 = np.zeros((128, 2), np.float32)
        m01[:, half] = 1.0
        m["m01"] = m01
        m["xown"] = np.ascontiguousarray(x[b, half * 2048:(half + 1) * 2048]).reshape(16, 128, 1024)
        m["ptown"] = np.ascontiguousarray(p[b, half * 2048:(half + 1) * 2048].T).reshape(2, 128, 2048)
        maps.append(m)
    return maps


def _run(inputs, debug=False, phases="12CDE", nblk=NBLK):
    maps = _prep(inputs)
    if "D" not in phases:
        for m in maps:
            m.pop("wgu")
            m.pop("wdn")
    nc = build_program(debug=debug, phases=phases, nblk=nblk)
    res = run_bass_kernel_spmd(nc, maps, core_ids=list(range(8)))
    return res.results


def kernel(**inputs):
    results = _run(inputs, debug=False)
    out = np.zeros((4, SEQ, 1024), np.float32)
    for c in range(8):
        b, half = c // 2, c % 2
        out[b, half * 2048:(half + 1) * 2048] = np.asarray(results[c]["out"], np.float32).reshape(2048, 1024)
    return out
```

```python
import contextlib
import math
import numpy as np
import concourse.bass as bass
import concourse.mybir as mybir
from concourse.bass_utils import run_bass_kernel_spmd

F32 = mybir.dt.float32
BF16 = mybir.dt.bfloat16
I32 = mybir.dt.int32
AF = mybir.ActivationFunctionType
ALU = mybir.AluOpType
AX = mybir.AxisListType

SEQ = 4096
NBLK = 8
DN_ALPHA = 2.0 ** 0.25
LAM_INIT = 0.2
NCONST = 1032
BIG = 30000.0


class Sched:
    STREAMS = ("pe", "act", "dve", "pool", "sp")

    def __init__(self, nc):
        self.nc = nc
        self.ops = {s: [] for s in self.STREAMS}
        self.last_write = {}
        self.readers = {}
        self.dma_slots = {}
        self.sync_same = {"act", "dve", "pool"}

    def _deps_for(self, reads, writes):
        deps = []
        for k in reads:
            t = self.last_write.get(k)
            if t is not None:
                deps.append(t)
        for k in writes:
            t = self.last_write.get(k)
            if t is not None:
                deps.append(t)
            deps.extend(self.readers.get(k, ()))
        return [(d[0], d[1], self.dma_slots[d[1]]) if d[0] == "dma" else d for d in deps]

    def _commit(self, tok, reads, writes):
        for k in reads:
            self.readers.setdefault(k, []).append(tok)
        for k in writes:
            self.last_write[k] = tok
            self.readers[k] = []

    enabled = True

    def fence(self, fn):
        en = self.enabled
        self.enabled = True
        self.op("dve", fn, r=(), w=("__fence__",), _nofence=True)
        self.enabled = en

    def op(self, stream, fn, r=(), w=(), _nofence=False):
        if not self.enabled:
            return
        if not _nofence:
            r = tuple(r) + ("__fence__",)
        w = tuple(w) + tuple(k for k in r if len(k) == 2 and k[0] == "P" and k[1].isdigit())
        deps = self._deps_for(r, w)
        idx = len(self.ops[stream])
        self.ops[stream].append(dict(fn=fn, deps=deps, dma=None))
        self._commit(("eng", stream, idx), r, w)

    def dma(self, stream, fn, slot, r=(), w=()):
        if not self.enabled:
            return
        r = tuple(r) + ("__fence__",)
        deps = self._deps_for(r, w)
        n = self.dma_slots.get(slot, 0) + 1
        self.dma_slots[slot] = n
        self.ops[stream].append(dict(fn=fn, deps=deps, dma=(slot, n)))
        self._commit(("dma", slot, n), r, w)

    def emit(self, final_wait_slots=()):
        nc = self.nc
        milestone = {s: set() for s in self.STREAMS}
        for s in self.STREAMS:
            for o in self.ops[s]:
                for d in o["deps"]:
                    if d[0] == "eng":
                        if d[1] == s and s not in self.sync_same:
                            continue
                        milestone[d[1]].add(d[2])
        mcount = {}
        for s in self.STREAMS:
            c = 0
            arr = []
            for i in range(len(self.ops[s])):
                if i in milestone[s]:
                    c += 1
                arr.append(c)
            mcount[s] = arr
        with contextlib.ExitStack() as es:
            esem = {s: es.enter_context(nc.semaphore("s_" + s)) for s in self.STREAMS}
            dsem = {sl: es.enter_context(nc.semaphore("d_" + str(sl))) for sl in self.dma_slots}
            block = es.enter_context(nc.Block())

            def run_stream(s, eng):
                waited = {}
                for i, o in enumerate(self.ops[s]):
                    need = {}
                    for d in o["deps"]:
                        if d[0] == "eng":
                            if d[1] == s and s not in self.sync_same:
                                continue
                            key = ("e", d[1])
                            val = mcount[d[1]][d[2]]
                        else:
                            key = ("d", d[1])
                            val = 16 * d[2]
                        if val > need.get(key, 0):
                            need[key] = val
                    for key, val in need.items():
                        if waited.get(key, 0) >= val:
                            continue
                        waited[key] = val
                        sem = esem[key[1]] if key[0] == "e" else dsem[key[1]]
                        eng.wait_ge(sem, val)
                    ins = o["fn"](eng)
                    if o["dma"] is not None:
                        ins.then_inc(dsem[o["dma"][0]], 16)
                    elif i in milestone[s]:
                        ins.then_inc(esem[s], 1)
                if s == "sp":
                    for sl in final_wait_slots:
                        eng.wait_ge(dsem[sl], 16 * self.dma_slots[sl])

            block.sync(lambda e: run_stream("sp", e))
            block.tensor(lambda e: run_stream("pe", e))
            block.scalar(lambda e: run_stream("act", e))
            block.vector(lambda e: run_stream("dve", e))
            block.gpsimd(lambda e: run_stream("pool", e))


def build_program(debug=False, phases="12CDE", nblk=NBLK, stop=None):
    nc = bass.Bass("TRN2", target_bir_lowering=False)
    S = Sched(nc)

    def mark(level):
        if stop is not None and level > stop:
            S.enabled = False

    def din(name, shape, dt=F32):
        return nc.dram_tensor(name, list(shape), dt, kind="ExternalInput").ap()

    xT = din("xT", [8, 128, SEQ])
    posb = din("posb", [128, SEQ], I32)
    wfm_d = din("wfm", [8, 128, 2560])
    wtm_d = din("wtm", [8, 128, 1032])
    convw_d = din("convw", [128, 48])
    hs_d = din("hs", [128, 8])
    gnw_d = din("gnw4", [128, 512])
    dnw_d = din("dnw4", [128, 512])
    lamv_d = din("lamv", [128, 256])
    wout_d = din("wout", [8, 128, 1024])
    const_d = din("consts", [128, NCONST])
    msk4_d = din("msk4", [128, 1024])
    m01_d = din("m01", [128, 2])
    xown_d = din("xown", [16, 128, 1024])
    ptown_d = din("ptown", [2, 128, 2048])
    lnp_d = din("lnp", [6, 128, 1024])
    rw_d = din("rw", [8, 128, 32])
    rbb_d = din("rbb", [128, 32])
    wgu_d = din("wgu", [32, 8, 128, 2048]) if "D" in phases else None
    bgu_d = din("bgu", [128, 512])
    wdn_d = din("wdn", [32, 8, 128, 1024]) if "D" in phases else None
    bdn_d = din("bdn", [128, 1024])
    wpg_d = din("wpg", [8, 128, 1024])
    pgb_d = din("pgb", [128, 1024])
    plew_d = din("plew", [2, 128, 1024])
    out_d = nc.dram_tensor("out", [16, 128, 1024], F32, kind="ExternalOutput").ap()
    ogs_d = nc.dram_tensor("ogs", [32, 128, 512], BF16, kind="Internal").ap()
    mixs_d = nc.dram_tensor("mixs", [32, 128, 1024], F32, kind="Internal").ap()
    dbg = {}
    if debug:
        dbg["mix"] = nc.dram_tensor("dbg_mix", [32, 128, 1024], F32, kind="ExternalOutput").ap()
        dbg["og"] = nc.dram_tensor("dbg_og", [32, 128, 512], F32, kind="ExternalOutput").ap()
        dbg["h1"] = nc.dram_tensor("dbg_h1", [16, 128, 1024], F32, kind="ExternalOutput").ap()
        dbg["acc"] = nc.dram_tensor("dbg_acc", [16, 128, 1024], F32, kind="ExternalOutput").ap()

    with contextlib.ExitStack() as top:
        P = [top.enter_context(nc.psum_tensor("P%d" % i, [128, 512], F32)) for i in range(8)]

        def pk(i):
            return "P%d" % i

        def sbt(es, name, shape, dt=F32):
            return es.enter_context(nc.sbuf_tensor("sb_" + name, list(shape), dt))

        C = sbt(top, "C", [128, NCONST])
        ident = C[:, 0:128]
        tri = C[:, 128:256]
        blk = C[:, 256:384]
        mstrict = C[:, 384:512]
        minclT = C[:, 512:640]
        ind = C[:, 1024:1026]
        freq = C[:, 1026:1027]
        c0 = C[:, 1027:1028]
        c1 = C[:, 1028:1029]
        fz = sbt(top, "fz", [128, 8])

        def fence():
            S.fence(lambda e: e.memset(fz[:], 0.0))
        Cb = sbt(top, "Cb", [128, 768], BF16)
        ident_b = Cb[:, 0:128]
        causal_b = Cb[:, 128:256]
        ones_b = Cb[:, 256:384]
        perm_b = Cb[:, 384:512]
        tri_b = Cb[:, 512:640]
        blk_b = Cb[:, 640:768]
        S.dma("sp", lambda e: e.dma_start(out=C[:], in_=const_d[:, :]), "c0", w=["C"])
        S.op("dve", lambda e: e.tensor_copy(out=Cb[:, 0:128], in_=C[:, 0:128]), r=["C"], w=["Cb"])
        S.op("dve", lambda e: e.tensor_copy(out=Cb[:, 128:384], in_=C[:, 640:896]), r=["C"], w=["Cb"])
        S.op("dve", lambda e: e.tensor_copy(out=Cb[:, 384:512], in_=C[:, 896:1024]), r=["C"], w=["Cb"])
        S.op("dve", lambda e: e.tensor_copy(out=Cb[:, 512:768], in_=C[:, 128:384]), r=["C"], w=["Cb"])

        S.enabled = "1" in phases
        with contextlib.ExitStack() as es:
            wg1 = sbt(es, "wg1", [128, 8, 1536], BF16)
            wt1 = sbt(es, "wt1", [128, 8, 520], BF16)
            xTbs = [sbt(es, "xTb%d" % i, [128, 8, 512], BF16) for i in range(2)]
            cin = sbt(es, "cin", [128, 12, 515], BF16)
            cw = sbt(es, "cw", [128, 48])
            hsb = sbt(es, "hsb", [128, 8])
            nega = sbt(es, "nega", [128, 4])
            gnw = sbt(es, "gnw", [128, 512])
            ycv = sbt(es, "ycv", [128, 512])
            gfm = sbt(es, "gfm", [128, 12, 512], BF16)
            zs = sbt(es, "zs", [128, 4, 512], BF16)
            beta = sbt(es, "beta", [128, 4, 4])
            ldt = sbt(es, "ldt", [128, 4, 4])
            sm = sbt(es, "sm", [128, 64])
            sq = sbt(es, "sq", [128, 8, 128], BF16)
            tA = sbt(es, "tA", [128, 512])
            tB = sbt(es, "tB", [128, 512])
            qn = sbt(es, "qn", [128, 4, 128], BF16)
            kn = sbt(es, "kn", [128, 4, 128], BF16)
            Rr = sbt(es, "Rr", [128, 4, 256])
            Rb = sbt(es, "Rb", [128, 4, 256], BF16)
            kd0 = sbt(es, "kd0", [128, 4, 128], BF16)
            kd1 = sbt(es, "kd1", [128, 4, 128], BF16)
            ldB = sbt(es, "ldB", [128, 8, 128], BF16)
            ldhl = sbt(es, "ldhl", [128, 8], BF16)
            msk4 = sbt(es, "msk4", [128, 1024])
            mstrict4 = msk4[:, 0:512]
            minclT4 = msk4[:, 512:1024]
            S.dma("sp", lambda e: e.dma_start(out=msk4[:], in_=msk4_d[:, :]), "c1", w=["msk4"])
            Ga = sbt(es, "Ga", [128, 4, 128])
            Gb = sbt(es, "Gb", [128, 4, 128])
            egr = sbt(es, "egr", [128, 4, 128])
            Em = sbt(es, "Em", [128, 4, 128])
            ETm = sbt(es, "ETm", [128, 4, 128])
            Nb = sbt(es, "Nb", [128, 4, 128], BF16)
            Mb = sbt(es, "Mb", [128, 4, 128], BF16)
            qkT = sbt(es, "qkT", [128, 4, 128], BF16)
            wb = sbt(es, "wb", [128, 4, 128], BF16)
            wT = sbt(es, "wT", [128, 4, 128], BF16)
            qd = sbt(es, "qd", [128, 4, 128], BF16)
            St = sbt(es, "St", [128, 4, 128])
            Sb = sbt(es, "Sb", [128, 4, 128], BF16)
            vnb = sbt(es, "vnb", [128, 4, 128], BF16)
            og = sbt(es, "og", [128, 4, 128])
            ogb = sbt(es, "ogb", [128, 512], BF16)
            PT = P[1][:].bitcast(BF16)

            for k in range(8):
                S.dma("pool", (lambda k: lambda e: e.dma_start(out=wg1[:, k, :], in_=wfm_d[k, :, 0:1536]))(k), "w1", w=["wg1"])
                S.dma("pool", (lambda k: lambda e: e.dma_start(out=wt1[:, k, 0:512], in_=wtm_d[k, :, 0:512]))(k), "w1", w=["wt1"])
                S.dma("pool", (lambda k: lambda e: e.dma_start(out=wt1[:, k, 512:520], in_=wtm_d[k, :, 1024:1032]))(k), "w1", w=["wt1"])
            S.dma("sp", lambda e: e.dma_start(out=cw[:], in_=convw_d[:, :]), "c1", w=["cw"])
            S.dma("sp", lambda e: e.dma_start(out=hsb[:], in_=hs_d[:, :]), "c1", w=["hsb"])
            S.dma("sp", lambda e: e.dma_start(out=gnw[:], in_=gnw_d[:, :]), "c1", w=["gnw"])
            S.op("act", lambda e: e.activation(out=nega[:], in_=hsb[:, 0:4], func=AF.Exp), r=["hsb"], w=["nega"])
            S.op("dve", lambda e: e.tensor_scalar(out=nega[:], in0=nega[:], scalar1=-1.0, scalar2=None, op0=ALU.mult), r=["nega"], w=["nega"])
            S.op("dve", lambda e: e.memset(cin[:].rearrange("p a b -> p (a b)"), 0.0), w=["cin"] + ["cin%d" % i for i in range(12)])
            S.op("dve", lambda e: e.memset(St[:].rearrange("p a b -> p (a b)"), 0.0), w=["St"])
            S.op("dve", lambda e: e.memset(Sb[:].rearrange("p a b -> p (a b)"), 0.0), w=["Sb"])
            S.op("dve", lambda e: e.memset(vnb[:].rearrange("p a b -> p (a b)"), 0.0), w=["vnb"])

            for b in range(nblk):
                t0 = b * 512
                mark(2)
                xTb = xTbs[b % 2]
                xk = "xTb%d" % (b % 2)
                for bb in ([0, 1] if b == 0 else [b + 1]):
                    if bb < nblk:
                        S.dma("pool", (lambda bb: lambda e: e.dma_start(out=xTbs[bb % 2][:], in_=xT[:, :, bb * 512:bb * 512 + 512].rearrange("k p t -> p k t")))(bb),
                              "x1_%d" % (bb % 2), w=["xTb%d" % (bb % 2)])
                for ch in range(12):
                    pb_ = (0, 6, 7)[ch % 3]

                    def mm(e, ch=ch, xTb=xTb, pb_=pb_):
                        for k in range(8):
                            ins = e.matmul(P[pb_][:], lhsT=wg1[:, k, ch * 128:(ch + 1) * 128], rhs=xTb[:, k, :], start=(k == 0), stop=(k == 7))
                        return ins
                    S.op("pe", mm, r=["wg1", xk], w=[pk(pb_)])
                    S.op("act", (lambda ch, pb_: lambda e: e.activation(out=cin[:, ch, 3:515], in_=P[pb_][:], func=AF.Copy))(ch, pb_), r=[pk(pb_)], w=["cin%d" % ch, "cin"])
                    S.op("dve", (lambda ch: lambda e: e.tensor_scalar(out=ycv[:], in0=cin[:, ch, 0:512], scalar1=cw[:, ch * 4:ch * 4 + 1], scalar2=None, op0=ALU.mult))(ch),
                         r=["cin%d" % ch, "cw"], w=["ycv"])
                    for wi in range(1, 4):
                        S.op("dve", (lambda ch, wi: lambda e: e.scalar_tensor_tensor(out=ycv[:], in0=cin[:, ch, wi:wi + 512], scalar=cw[:, ch * 4 + wi:ch * 4 + wi + 1],
                                                                                     in1=ycv[:], op0=ALU.mult, op1=ALU.add))(ch, wi), r=["cin%d" % ch, "cw", "ycv"], w=["ycv"])
                    S.op("act", (lambda ch: lambda e: e.activation(out=gfm[:, ch, :], in_=ycv[:], func=AF.Silu))(ch), r=["ycv"], w=["gfm"])
                    S.op("pool", (lambda ch: lambda e: e.tensor_copy(out=cin[:, ch, 0:3], in_=cin[:, ch, 512:515]))(ch), r=["cin%d" % ch], w=["cin%d" % ch])
                mark(3)
                for t in range(4):
                    c0_ = t * 128

                    pz_ = (0, 6)[t % 2]

                    def mmz(e, c0_=c0_, xTb=xTb, pz_=pz_):
                        for k in range(8):
                            ins = e.matmul(P[pz_][:], lhsT=xTb[:, k, c0_:c0_ + 128], rhs=wt1[:, k, 0:512], start=(k == 0), stop=(k == 7))
                        return ins
                    S.op("pe", mmz, r=["wt1", xk], w=[pk(pz_)])
                    S.op("act", (lambda t, pz_: lambda e: e.activation(out=zs[:, t, :], in_=P[pz_][:], func=AF.Silu))(t, pz_), r=[pk(pz_)], w=["zs"])

                    def mmb(e, c0_=c0_, xTb=xTb):
                        for k in range(8):
                            ins = e.matmul(P[2][:, 0:8], lhsT=xTb[:, k, c0_:c0_ + 128], rhs=wt1[:, k, 512:520], start=(k == 0), stop=(k == 7))
                        return ins
                    S.op("pe", mmb, r=["wt1", xk], w=[pk(2)])
                    S.op("act", lambda e: e.activation(out=sm[:, 0:4], in_=P[2][:, 0:4], func=AF.Exp, scale=-1.0), r=[pk(2)], w=["sm"])
                    S.op("dve", lambda e: e.tensor_scalar(out=sm[:, 0:4], in0=sm[:, 0:4], scalar1=1.0, scalar2=None, op0=ALU.add), r=["sm"], w=["sm"])
                    S.op("dve", (lambda t: lambda e: e.reciprocal(out=beta[:, t, :], in_=sm[:, 0:4]))(t), r=["sm"], w=["beta"])
                    S.op("dve", lambda e: e.tensor_tensor(out=sm[:, 4:8], in0=P[2][:, 4:8], in1=hsb[:, 4:8], op=ALU.add), r=[pk(2), "hsb"], w=["sm"])
                    S.op("dve", lambda e: e.tensor_scalar(out=sm[:, 8:12], in0=sm[:, 4:8], scalar1=-1.0, scalar2=None, op0=ALU.mult), r=["sm"], w=["sm"])
                    S.op("dve", lambda e: e.tensor_tensor(out=sm[:, 8:12], in0=sm[:, 8:12], in1=sm[:, 4:8], op=ALU.max), r=["sm"], w=["sm"])
                    S.op("act", lambda e: e.activation(out=sm[:, 8:12], in_=sm[:, 8:12], func=AF.Exp, scale=-1.0), r=["sm"], w=["sm"])
                    S.op("act", lambda e: e.activation(out=sm[:, 8:12], in_=sm[:, 8:12], func=AF.Ln, bias=1.0), r=["sm"], w=["sm"])
                    S.op("dve", lambda e: e.tensor_scalar(out=sm[:, 4:8], in0=sm[:, 4:8], scalar1=0.0, scalar2=None, op0=ALU.max), r=["sm"], w=["sm"])
                    S.op("dve", lambda e: e.tensor_tensor(out=sm[:, 4:8], in0=sm[:, 4:8], in1=sm[:, 8:12], op=ALU.add), r=["sm"], w=["sm"])
                    S.op("dve", (lambda t: lambda e: e.tensor_tensor(out=ldt[:, t, :], in0=sm[:, 4:8], in1=nega[:], op=ALU.mult))(t), r=["sm", "nega"], w=["ldt"])

                for t in range(4):
                    cs = slice(t * 128, (t + 1) * 128)
                    bet = beta[:, t, :]
                    ld = ldt[:, t, :]
                    mark(4)
                    S.op("pool", (lambda cs: lambda e: e.tensor_tensor(out=sq[:], in0=gfm[:, 0:8, cs], in1=gfm[:, 0:8, cs], op=ALU.mult))(cs), r=["gfm"], w=["sq"])
                    sqf = sq[:].rearrange("p h d -> p (h d)")
                    S.op("pe", lambda e: e.matmul(P[2][:], lhsT=ones_b, rhs=sqf[:, 0:512], start=True, stop=True), r=["sq", "Cb"], w=[pk(2)])
                    S.op("pe", lambda e: e.matmul(P[3][:], lhsT=ones_b, rhs=sqf[:, 512:1024], start=True, stop=True), r=["sq", "Cb"], w=[pk(3)])
                    S.op("act", lambda e: e.activation(out=tA[:], in_=P[2][:], func=AF.Ln, bias=1e-6), r=[pk(2)], w=["tA"])
                    S.op("act", lambda e: e.activation(out=tA[:], in_=tA[:], func=AF.Exp, scale=-0.5), r=["tA"], w=["tA"])
                    S.op("act", lambda e: e.activation(out=tB[:], in_=P[3][:], func=AF.Ln, bias=1e-6), r=[pk(3)], w=["tB"])
                    S.op("act", lambda e: e.activation(out=tB[:], in_=tB[:], func=AF.Exp, scale=-0.5), r=["tB"], w=["tB"])
                    S.op("dve", (lambda cs: lambda e: e.scalar_tensor_tensor(out=qn[:], in0=gfm[:, 0:4, cs], scalar=float(128 ** -0.5),
                                                                             in1=tA[:].rearrange("p (h d) -> p h d", h=4), op0=ALU.mult, op1=ALU.mult))(cs), r=["gfm", "tA"], w=["qn"])
                    S.op("dve", (lambda cs: lambda e: e.tensor_tensor(out=kn[:], in0=gfm[:, 4:8, cs], in1=tB[:].rearrange("p (h d) -> p h d", h=4), op=ALU.mult))(cs),
                         r=["gfm", "tB"], w=["kn"])

                    mark(4.2)
                    def trkv(e, cs=cs):
                        for h in range(4):
                            e.transpose(PT[:, h * 128:(h + 1) * 128], kn[:, h, :], ident_b)
                        for h in range(4):
                            ins = e.transpose(PT[:, 512 + h * 128:512 + (h + 1) * 128], gfm[:, 8 + h, cs], ident_b)
                        return ins
                    S.op("pe", trkv, r=["kn", "gfm", "Cb"], w=[pk(1)])
                    mark(4.4)
                    S.op("act", (lambda ld: lambda e: e.activation(out=ldhl[:, 0:4], in_=ld, func=AF.Copy))(ld), r=["ldt"], w=["ldhl"])
                    S.op("dve", (lambda ld: lambda e: e.tensor_tensor(out=ldhl[:, 4:8], in0=ld, in1=ldhl[:, 0:4], op=ALU.subtract))(ld), r=["ldt", "ldhl"], w=["ldhl"])

                    def mmg2(e):
                        e.matmul(P[5][:, 0:4], lhsT=tri_b, rhs=ldhl[:, 0:4], start=True, stop=False)
                        e.matmul(P[5][:, 0:4], lhsT=tri_b, rhs=ldhl[:, 4:8], start=False, stop=True)
                        e.matmul(P[5][:, 4:8], lhsT=blk_b, rhs=ldhl[:, 0:4], start=True, stop=False)
                        return e.matmul(P[5][:, 4:8], lhsT=blk_b, rhs=ldhl[:, 4:8], start=False, stop=True)
                    S.op("pe", mmg2, r=["Cb", "ldhl"], w=[pk(5)])
                    S.op("act", lambda e: e.activation(out=sm[:, 16:20], in_=P[5][:, 0:4], func=AF.Copy), r=[pk(5)], w=["sm2"])
                    S.op("act", lambda e: e.activation(out=sm[:, 24:28], in_=P[5][:, 0:4], func=AF.Exp), r=[pk(5)], w=["sm2"])
                    S.op("dve", lambda e: e.tensor_scalar(out=sm[:, 20:24], in0=sm[:, 16:20], scalar1=-1.0, scalar2=None, op0=ALU.mult), r=["sm2"], w=["sm2"])
                    S.op("dve", lambda e: e.tensor_tensor(out=sm[:, 28:32], in0=P[5][:, 4:8], in1=sm[:, 16:20], op=ALU.subtract), r=[pk(5), "sm2"], w=["sm2"])
                    S.op("act", lambda e: e.activation(out=sm[:, 28:32], in_=sm[:, 28:32], func=AF.Exp), r=["sm2"], w=["sm2"])
                    S.op("dve", (lambda bet: lambda e: e.tensor_tensor(out=sm[:, 32:36], in0=sm[:, 24:28], in1=bet, op=ALU.mult))(bet), r=["sm2", "beta"], w=["sm2"])
                    S.op("dve", lambda e: e.tensor_scalar(out=sm[:, 36:40], in0=sm[:, 28:32], scalar1=ind[:, 0:1], scalar2=None, op0=ALU.mult), r=["sm2", "C"], w=["sm2"])
                    S.op("dve", lambda e: e.tensor_scalar(out=sm[:, 40:44], in0=sm[:, 28:32], scalar1=ind[:, 1:2], scalar2=None, op0=ALU.mult), r=["sm2", "C"], w=["sm2"])
                    S.op("dve", (lambda bet: lambda e: e.tensor_scalar(out=sm[:, 44:48], in0=bet, scalar1=-1.0, scalar2=None, op0=ALU.mult))(bet), r=["beta"], w=["sm2"])

                    mark(4.6)

                    def bc(col):
                        return sm[:, col:col + 4].unsqueeze(2).to_broadcast([128, 4, 128])
                    PTk = PT[:, 0:512].rearrange("p (h d) -> p h d", h=4)
                    PTv = PT[:, 512:1024].rearrange("p (h d) -> p h d", h=4)
                    S.op("dve", (lambda bet: lambda e: e.tensor_tensor(out=Rr[:, :, 0:128], in0=PTv, in1=bet.unsqueeze(2).to_broadcast([128, 4, 128]), op=ALU.mult))(bet),
                         r=[pk(1), "beta"], w=["Rr"])
                    S.op("dve", lambda e: e.tensor_tensor(out=Rr[:, :, 128:256], in0=PTk, in1=bc(32), op=ALU.mult), r=[pk(1), "sm2"], w=["Rr"])
                    S.op("dve", lambda e: e.tensor_tensor(out=kd0[:], in0=PTk, in1=bc(36), op=ALU.mult), r=[pk(1), "sm2"], w=["kd0"])
                    S.op("dve", lambda e: e.tensor_tensor(out=kd1[:], in0=PTk, in1=bc(40), op=ALU.mult), r=[pk(1), "sm2"], w=["kd1"])
                    mark(5)
                    S.op("dve", lambda e: e.tensor_copy(out=ldB[:], in_=ldhl[:].unsqueeze(2).to_broadcast([128, 8, 128])), r=["ldhl"], w=["ldB"])

                    def mmG(e):
                        for h in range(4):
                            e.matmul(P[4][:, h * 128:(h + 1) * 128], lhsT=ldB[:, h, :], rhs=tri_b, start=True, stop=False)
                            ins = e.matmul(P[4][:, h * 128:(h + 1) * 128], lhsT=ldB[:, 4 + h, :], rhs=tri_b, start=False, stop=True)
                        return ins
                    S.op("pe", mmG, r=["ldB", "Cb"], w=[pk(4)])
                    P4v = P[4][:].rearrange("p (h d) -> p h d", h=4)
                    mark(5.2)
                    S.op("dve", lambda e: e.tensor_tensor(out=Ga[:], in0=P4v, in1=mstrict4.rearrange("p (h d) -> p h d", h=4), op=ALU.add), r=[pk(4), "msk4"], w=["Ga"])
                    S.op("dve", lambda e: e.tensor_tensor(out=Gb[:], in0=P4v, in1=minclT4.rearrange("p (h d) -> p h d", h=4), op=ALU.subtract), r=[pk(4), "msk4"], w=["Gb"])
                    S.op("act", lambda e: e.activation(out=egr[:], in_=P4v, func=AF.Exp), r=[pk(4)], w=["egr"])
                    mark(5.4)
                    for h in range(4):
                        S.op("act", (lambda h: lambda e: e.activation(out=Em[:, h, :], in_=Ga[:, h, :], func=AF.Exp, scale=-1.0, bias=sm[:, 16 + h:17 + h]))(h),
                             r=["Ga", "sm2"], w=["Em"])
                        S.op("act", (lambda h: lambda e: e.activation(out=ETm[:, h, :], in_=Gb[:, h, :], func=AF.Exp, bias=sm[:, 20 + h:21 + h]))(h),
                             r=["Gb", "sm2"], w=["ETm"])

                    mark(5.6)
                    def mmKK(e):
                        for h in range(4):
                            e.matmul(P[2][:, h * 128:(h + 1) * 128], lhsT=kn[:, h, :], rhs=kn[:, h, :], start=True, stop=True)
                        for h in range(4):
                            ins = e.matmul(P[3][:, h * 128:(h + 1) * 128], lhsT=kn[:, h, :], rhs=qn[:, h, :], start=True, stop=True)
                        return ins
                    S.op("pe", mmKK, r=["kn", "qn"], w=[pk(2), pk(3)])
                    P2v = P[2][:].rearrange("p (h d) -> p h d", h=4)
                    P3v = P[3][:].rearrange("p (h d) -> p h d", h=4)
                    tAv = tA[:].rearrange("p (h d) -> p h d", h=4)
                    S.op("dve", lambda e: e.tensor_tensor(out=tAv, in0=P2v, in1=bc(44), op=ALU.mult), r=[pk(2), "sm2"], w=["tA"])
                    S.op("dve", lambda e: e.tensor_tensor(out=Nb[:], in0=tAv, in1=Em[:], op=ALU.mult), r=["tA", "Em"], w=["Nb"])
                    S.op("dve", lambda e: e.tensor_tensor(out=qkT[:], in0=P3v, in1=ETm[:], op=ALU.mult), r=[pk(3), "ETm"], w=["qkT"])

                    def trN(e):
                        for h in range(4):
                            ins = e.transpose(PT[:, h * 128:(h + 1) * 128], Nb[:, h, :], ident_b)
                        return ins
                    S.op("pe", trN, r=["Nb", "Cb"], w=[pk(1)])
                    S.op("act", lambda e: e.activation(out=Mb[:], in_=PTk, func=AF.Copy), r=[pk(1)], w=["Mb"])
                    mark(6)
                    for p in range(6):
                        S.op("act", lambda e: e.activation(out=Rb[:], in_=Rr[:], func=AF.Copy), r=["Rr"], w=["Rb"])

                        def mmR(e):
                            for h in range(4):
                                ins = e.matmul(P[6 + h // 2][:, (h % 2) * 256:(h % 2) * 256 + 256], lhsT=Mb[:, h, :], rhs=Rb[:, h, :], start=True, stop=True)
                            return ins
                        S.op("pe", mmR, r=["Mb", "Rb"], w=[pk(6), pk(7)])
                        S.op("dve", lambda e: e.tensor_tensor(out=Rr[:, 0:2, :], in0=Rr[:, 0:2, :], in1=P[6][:].rearrange("p (h d) -> p h d", h=2), op=ALU.add),
                             r=["Rr", pk(6)], w=["Rr"])
                        S.op("dve", lambda e: e.tensor_tensor(out=Rr[:, 2:4, :], in0=Rr[:, 2:4, :], in1=P[7][:].rearrange("p (h d) -> p h d", h=2), op=ALU.add),
                             r=["Rr", pk(7)], w=["Rr"])
                        if p < 5:
                            def mmSq(e):
                                for h in range(4):
                                    e.matmul(P[4][:, h * 128:(h + 1) * 128], lhsT=Mb[:, h, :], rhs=Nb[:, h, :], start=True, stop=True)
                                for h in range(4):
                                    ins = e.matmul(P[5][:, h * 128:(h + 1) * 128], lhsT=Nb[:, h, :], rhs=Mb[:, h, :], start=True, stop=True)
                                return ins
                            S.op("pe", mmSq, r=["Mb", "Nb"], w=[pk(4), pk(5)])
                            S.op("act", lambda e: e.activation(out=Nb[:], in_=P4v, func=AF.Copy), r=[pk(4)], w=["Nb"])
                            S.op("dve", lambda e: e.tensor_copy(out=Mb[:], in_=P[5][:].rearrange("p (h d) -> p h d", h=4)), r=[pk(5)], w=["Mb"])
                    mark(7)
                    S.op("act", lambda e: e.activation(out=wb[:], in_=Rr[:, :, 128:256], func=AF.Copy), r=["Rr"], w=["wb"])

                    def trW(e):
                        for h in range(4):
                            ins = e.transpose(PT[:, h * 128:(h + 1) * 128], wb[:, h, :], ident_b)
                        return ins
                    S.op("pe", trW, r=["wb", "Cb"], w=[pk(1)])
                    S.op("act", lambda e: e.activation(out=wT[:], in_=PTk, func=AF.Copy), r=[pk(1)], w=["wT"])
                    S.op("dve", lambda e: e.tensor_tensor(out=qd[:], in0=qn[:], in1=egr[:], op=ALU.mult), r=["qn", "egr"], w=["qd"])
                    for ch in range(2):
                        kd = kd0 if ch == 0 else kd1
                        kdk = "kd0" if ch == 0 else "kd1"
                        last = 63 if ch == 0 else 127

                        def mmV(e):
                            for h in range(4):
                                ins = e.matmul(P[2][:, h * 128:(h + 1) * 128], lhsT=wT[:, h, :], rhs=Sb[:, h, :], start=True, stop=True)
                            return ins
                        S.op("pe", mmV, r=["wT", "Sb"], w=[pk(2)])
                        S.op("dve", lambda e: e.tensor_tensor(out=vnb[:], in0=Rr[:, :, 0:128], in1=P2v, op=ALU.subtract), r=["Rr", pk(2)], w=["vnb"])

                        def mmO(e, kd=kd):
                            for h in range(4):
                                e.matmul(P[3][:, h * 128:(h + 1) * 128], lhsT=qd[:, h, :], rhs=Sb[:, h, :], start=True, stop=False)
                                e.matmul(P[3][:, h * 128:(h + 1) * 128], lhsT=qkT[:, h, :], rhs=vnb[:, h, :], start=False, stop=True)
                            for h in range(4):
                                ins = e.matmul(P[4][:, h * 128:(h + 1) * 128], lhsT=kd[:, h, :], rhs=vnb[:, h, :], start=True, stop=True)
                            return ins
                        S.op("pe", mmO, r=["qd", "Sb", "qkT", "vnb", kdk], w=[pk(3), pk(4)])
                        if ch == 0:
                            S.op("dve", lambda e: e.tensor_scalar(out=og[:], in0=P3v, scalar1=ind[:, 0:1], scalar2=None, op0=ALU.mult), r=[pk(3), "C"], w=["og"])
                        else:
                            S.op("dve", lambda e: e.scalar_tensor_tensor(out=og[:], in0=P3v, scalar=ind[:, 1:2], in1=og[:], op0=ALU.mult, op1=ALU.add),
                                 r=[pk(3), "C", "og"], w=["og"])
                        for h in range(4):
                            S.op("dve", (lambda h, last: lambda e: e.scalar_tensor_tensor(out=St[:, h, :], in0=St[:, h, :], scalar=egr[:, h, last:last + 1],
                                                                                          in1=P[4][:, h * 128:(h + 1) * 128], op0=ALU.mult, op1=ALU.add))(h, last),
                                 r=["St", "egr", pk(4)], w=["St"])
                        S.op("act", lambda e: e.activation(out=Sb[:], in_=St[:], func=AF.Copy), r=["St"], w=["Sb"])
                    mark(8)
                    ogf = og[:].rearrange("p h d -> p (h d)")
                    S.op("pool", lambda e: e.tensor_tensor(out=tB[:], in0=ogf, in1=ogf, op=ALU.mult), r=["og"], w=["tB"])
                    S.op("dve", lambda e: e.tensor_reduce(out=sm[:, 48:52], in_=tB[:].rearrange("p (h d) -> p h d", h=4), axis=AX.X, op=ALU.add), r=["tB"], w=["sm3"])
                    S.op("act", lambda e: e.activation(out=sm[:, 48:52], in_=sm[:, 48:52], func=AF.Ln, scale=1.0 / 128.0, bias=1e-6), r=["sm3"], w=["sm3"])
                    S.op("act", lambda e: e.activation(out=sm[:, 48:52], in_=sm[:, 48:52], func=AF.Exp, scale=-0.5), r=["sm3"], w=["sm3"])
                    S.op("dve", lambda e: e.tensor_tensor(out=tAv, in0=og[:], in1=bc(48), op=ALU.mult), r=["og", "sm3"], w=["tA"])
                    S.op("dve", lambda e: e.tensor_tensor(out=tA[:], in0=tA[:], in1=gnw[:], op=ALU.mult), r=["tA", "gnw"], w=["tA"])
                    S.op("dve", (lambda t: lambda e: e.tensor_tensor(out=ogb[:], in0=tA[:], in1=zs[:, t, :], op=ALU.mult))(t), r=["tA", "zs"], w=["ogb"])
                    ti = b * 4 + t
                    S.dma("sp", (lambda ti: lambda e: e.dma_start(out=ogs_d[ti, :, :], in_=ogb[:]))(ti), "ogw", r=["ogb"], w=["ogs%d" % ti])
                    if debug:
                        S.dma("sp", (lambda ti: lambda e: e.dma_start(out=dbg["og"][ti, :, :], in_=og[:].rearrange("p h d -> p (h d)")))(ti), "dbg", r=["og"])

        fence()
        S.enabled = "2" in phases
        with contextlib.ExitStack() as es:
            wq2 = sbt(es, "wq2", [128, 8, 1024], BF16)
            wv2 = sbt(es, "wv2", [128, 8, 512], BF16)
            wo = sbt(es, "wo", [128, 8, 1024], BF16)
            xT2s = [sbt(es, "xTc%d" % i, [128, 8, 512], BF16) for i in range(2)]
            KT = sbt(es, "KT", [128, 4, SEQ], BF16)
            Vs = sbt(es, "Vs", [128, 32, 4, 132], BF16)
            pi_ = sbt(es, "pi_", [128, 512], I32)
            pf = sbt(es, "pf", [128, 512])
            ka = sbt(es, "ka", [128, 512])
            ki = sbt(es, "ki", [128, 512], I32)
            cosF = sbt(es, "cosF", [128, 512])
            sinF = sbt(es, "sinF", [128, 512])
            mb = sbt(es, "mb", [128, 512], BF16)
            r1 = sbt(es, "r1", [128, 512])
            r2 = sbt(es, "r2", [128, 512])
            Q0 = sbt(es, "Q0", [128, 4, 512], BF16)
            Q1 = sbt(es, "Q1", [128, 4, 512], BF16)
            pT = [sbt(es, "pT%d" % i, [128, 512], BF16) for i in range(2)]
            den = sbt(es, "den", [128, 16])
            od = sbt(es, "od", [128, 4, 4, 128])
            ot = sbt(es, "ot", [128, 4, 1024], BF16)
            oT = sbt(es, "oT", [128, 8, 128], BF16)
            mixt = sbt(es, "mixt", [128, 1024])
            dnw = sbt(es, "dnw", [128, 512])
            lamv = sbt(es, "lamv", [128, 256])
            lam = sbt(es, "lam", [128, 8])
            PT2 = P[6][:].bitcast(BF16)

            for k in range(8):
                S.dma("pool", (lambda k: lambda e: e.dma_start(out=wq2[:, k, :], in_=wfm_d[k, :, 1536:2560]))(k), "w2", w=["wq2"])
                S.dma("pool", (lambda k: lambda e: e.dma_start(out=wv2[:, k, :], in_=wtm_d[k, :, 512:1024]))(k), "w2", w=["wv2"])
                S.dma("pool", (lambda k: lambda e: e.dma_start(out=wo[:, k, :], in_=wout_d[k, :, :]))(k), "w2", w=["wo"])
            S.dma("sp", lambda e: e.dma_start(out=dnw[:], in_=dnw_d[:, :]), "c2", w=["dnw"])
            S.dma("sp", lambda e: e.dma_start(out=lamv[:], in_=lamv_d[:, :]), "c2", w=["lamv"])
            S.op("dve", lambda e: e.memset(Vs[:].rearrange("p a b c -> p (a b c)"), 1.0), w=["Vs"])
            S.op("dve", lambda e: e.tensor_tensor(out=lamv[:, 0:64], in0=lamv[:, 0:64], in1=lamv[:, 64:128], op=ALU.mult), r=["lamv"], w=["lamv"])
            S.op("dve", lambda e: e.tensor_tensor(out=lamv[:, 128:192], in0=lamv[:, 128:192], in1=lamv[:, 192:256], op=ALU.mult), r=["lamv"], w=["lamv"])
            S.op("dve", lambda e: e.tensor_reduce(out=lam[:, 0:1], in_=lamv[:, 0:64], axis=AX.X, op=ALU.add), r=["lamv"], w=["lam"])
            S.op("dve", lambda e: e.tensor_reduce(out=lam[:, 1:2], in_=lamv[:, 128:192], axis=AX.X, op=ALU.add), r=["lamv"], w=["lam"])
            S.op("act", lambda e: e.activation(out=lam[:, 0:2], in_=lam[:, 0:2], func=AF.Exp), r=["lam"], w=["lam"])
            S.op("dve", lambda e: e.tensor_tensor(out=lam[:, 2:3], in0=lam[:, 1:2], in1=lam[:, 0:1], op=ALU.subtract), r=["lam"], w=["lam"])
            S.op("dve", lambda e: e.tensor_scalar(out=lam[:, 2:3], in0=lam[:, 2:3], scalar1=-LAM_INIT, scalar2=None, op0=ALU.add), r=["lam"], w=["lam"])

            def range_reduce_sin(dst, src_key):
                S.op("dve", lambda e: e.tensor_scalar(out=ki[:], in0=ka[:], scalar1=float(1.0 / (2 * math.pi)), scalar2=None, op0=ALU.mult), r=["ka"], w=["ki"])
                S.op("dve", lambda e: e.tensor_copy(out=r1[:], in_=ki[:]), r=["ki"], w=["r1"])
                S.op("dve", lambda e: e.scalar_tensor_tensor(out=ka[:], in0=r1[:], scalar=float(-2 * math.pi), in1=ka[:], op0=ALU.mult, op1=ALU.add), r=["r1", "ka"], w=["ka"])
                S.op("dve", lambda e: e.tensor_scalar(out=r1[:], in0=ka[:], scalar1=float(math.pi), scalar2=float(-2 * math.pi), op0=ALU.is_gt, op1=ALU.mult), r=["ka"], w=["r1"])
                S.op("dve", lambda e: e.tensor_tensor(out=ka[:], in0=ka[:], in1=r1[:], op=ALU.add), r=["ka", "r1"], w=["ka"])
                S.op("act", lambda e: e.activation(out=dst[:], in_=ka[:], func=AF.Sin), r=["ka"], w=[src_key])

            for b in range(nblk):
                t0 = b * 512
                xT2 = xT2s[b % 2]
                xk = "xTc%d" % (b % 2)
                for bb in ([0, 1] if b == 0 else [b + 1]):
                    if bb < nblk:
                        S.dma("pool", (lambda bb: lambda e: e.dma_start(out=xT2s[bb % 2][:], in_=xT[:, :, bb * 512:bb * 512 + 512].rearrange("k p t -> p k t")))(bb),
                              "x2_%d" % (bb % 2), w=["xTc%d" % (bb % 2)])
                S.dma("sp", (lambda t0: lambda e: e.dma_start(out=pi_[:], in_=posb[:, t0:t0 + 512]))(t0), "pos", w=["pi_"])
                S.op("dve", lambda e: e.tensor_copy(out=pf[:], in_=pi_[:]), r=["pi_"], w=["pf"])
                S.op("dve", lambda e: e.tensor_scalar(out=pf[:], in0=pf[:], scalar1=freq, scalar2=None, op0=ALU.mult), r=["pf", "C"], w=["pf"])
                S.op("dve", lambda e: e.tensor_copy(out=ka[:], in_=pf[:]), r=["pf"], w=["ka"])
                range_reduce_sin(sinF, "sinF")
                S.op("dve", lambda e: e.tensor_scalar(out=ka[:], in0=pf[:], scalar1=float(math.pi / 2), scalar2=None, op0=ALU.add), r=["pf"], w=["ka"])
                range_reduce_sin(cosF, "cosF")
                for which in range(2):
                    for h in range(4):
                        col = which * 512 + h * 128

                        def mm(e, col=col, xT2=xT2):
                            for k in range(8):
                                ins = e.matmul(P[0][:], lhsT=wq2[:, k, col:col + 128], rhs=xT2[:, k, :], start=(k == 0), stop=(k == 7))
                            return ins
                        S.op("pe", mm, r=["wq2", xk], w=[pk(0)])
                        S.op("act", lambda e: e.activation(out=mb[:], in_=P[0][:], func=AF.Copy), r=[pk(0)], w=["mb"])
                        S.op("pe", lambda e: e.matmul(P[1][:], lhsT=perm_b, rhs=mb[:], start=True, stop=True), r=["mb", "Cb"], w=[pk(1)])
                        S.op("dve", lambda e: e.tensor_tensor(out=r1[:], in0=mb[:], in1=cosF[:], op=ALU.mult), r=["mb", "cosF"], w=["r1"])
                        S.op("dve", lambda e: e.tensor_tensor(out=r2[:], in0=P[1][:], in1=sinF[:], op=ALU.mult), r=[pk(1), "sinF"], w=["r2"])
                        if which == 0:
                            S.op("dve", lambda e: e.tensor_tensor(out=r1[:], in0=r1[:], in1=r2[:], op=ALU.add), r=["r1", "r2"], w=["r1"])
                            S.op("dve", (lambda h: lambda e: e.tensor_scalar(out=Q0[:, h, :], in0=r1[:], scalar1=c0, scalar2=None, op0=ALU.mult))(h), r=["r1", "C"], w=["Q0"])
                            S.op("dve", (lambda h: lambda e: e.tensor_scalar(out=Q1[:, h, :], in0=r1[:], scalar1=c1, scalar2=None, op0=ALU.mult))(h), r=["r1", "C"], w=["Q1"])
                        else:
                            S.op("dve", (lambda h, t0: lambda e: e.tensor_tensor(out=KT[:, h, t0:t0 + 512], in0=r1[:], in1=r2[:], op=ALU.add))(h, t0), r=["r1", "r2"], w=["KT"])
                for t in range(4):
                    c0_ = t * 128
                    ti = b * 4 + t

                    def mmv(e, c0_=c0_, xT2=xT2):
                        for k in range(8):
                            ins = e.matmul(P[0][:], lhsT=xT2[:, k, c0_:c0_ + 128], rhs=wv2[:, k, :], start=(k == 0), stop=(k == 7))
                        return ins
                    S.op("pe", mmv, r=["wv2", xk], w=[pk(0)])
                    S.op("act", (lambda ti: lambda e: e.activation(out=Vs[:, ti, :, 0:128], in_=P[0][:].rearrange("p (h d) -> p h d", h=4), func=AF.Copy))(ti),
                         r=[pk(0)], w=["Vs"])
                nkt = 4 * b + 4
                it = 0
                for h in range(4):
                    for c in range(2):
                        Qc = Q0 if c == 0 else Q1
                        qk_ = "Q0" if c == 0 else "Q1"
                        accb = (2, 3) if c == 0 else (4, 5)
                        for kt in range(nkt):
                            j = kt - 4 * b
                            q0 = 128 * j if j >= 0 else 0
                            sp_ = it % 2
                            it += 1
                            S.op("pe", (lambda sp_, h, kt, q0, Qc: lambda e: e.matmul(P[sp_][:, q0:512], lhsT=KT[:, h, kt * 128:(kt + 1) * 128], rhs=Qc[:, h, q0:512],
                                                                                      start=True, stop=True))(sp_, h, kt, q0, Qc), r=["KT", qk_], w=[pk(sp_)])
                            S.op("act", (lambda sp_, q0: lambda e: e.activation(out=pT[sp_][:, q0:512], in_=P[sp_][:, q0:512], func=AF.Exp, scale=0.125))(sp_, q0),
                                 r=[pk(sp_)], w=["pT%d" % sp_])
                            if j >= 0:
                                S.op("pool", (lambda sp_, q0: lambda e: e.tensor_tensor(out=pT[sp_][:, q0:q0 + 128], in0=pT[sp_][:, q0:q0 + 128], in1=causal_b, op=ALU.mult))(sp_, q0),
                                     r=["pT%d" % sp_, "Cb"], w=["pT%d" % sp_])

                            def mmpv(e, sp_=sp_, h=h, kt=kt, j=j, accb=accb, b=b):
                                ins = None
                                for qi in range(max(j, 0), 4):
                                    bank = P[accb[qi // 2]]
                                    o0 = (qi % 2) * 256
                                    ins = e.matmul(bank[:, o0:o0 + 129], lhsT=pT[sp_][:, qi * 128:(qi + 1) * 128], rhs=Vs[:, kt, h, 0:129],
                                                   start=(kt == 0 and qi % 2 == 0), stop=(kt == 4 * b + qi), skip_group_check=True)
                                return ins
                            S.op("pe", mmpv, r=["pT%d" % sp_, "Vs"], w=[pk(accb[0]), pk(accb[1])])
                    for qi in range(4):
                        o0 = (qi % 2) * 256
                        bA = P[2 + qi // 2]
                        bB = P[4 + qi // 2]
                        S.op("dve", (lambda qi, bA, o0: lambda e: e.tensor_copy(out=den[:, qi:qi + 1], in_=bA[:, o0 + 128:o0 + 129]))(qi, bA, o0), r=[pk(2 + qi // 2)], w=["den"])
                        S.op("dve", (lambda qi, bB, o0: lambda e: e.tensor_copy(out=den[:, 4 + qi:5 + qi], in_=bB[:, o0 + 128:o0 + 129]))(qi, bB, o0), r=[pk(4 + qi // 2)], w=["den"])
                    S.op("dve", lambda e: e.reciprocal(out=den[:, 8:16], in_=den[:, 0:8]), r=["den"], w=["den"])
                    S.op("dve", lambda e: e.tensor_scalar(out=den[:, 12:16], in0=den[:, 12:16], scalar1=lam[:, 2:3], scalar2=None, op0=ALU.mult), r=["den", "lam"], w=["den"])
                    for qi in range(4):
                        o0 = (qi % 2) * 256
                        bA = P[2 + qi // 2]
                        bB = P[4 + qi // 2]
                        S.op("dve", (lambda qi, bA, o0, h: lambda e: e.tensor_scalar(out=od[:, qi, h, :], in0=bA[:, o0:o0 + 128], scalar1=den[:, 8 + qi:9 + qi], scalar2=None,
                                                                                      op0=ALU.mult))(qi, bA, o0, h), r=[pk(2 + qi // 2), "den"], w=["od"])
                        S.op("dve", (lambda qi, bB, o0, h: lambda e: e.scalar_tensor_tensor(out=od[:, qi, h, :], in0=bB[:, o0:o0 + 128], scalar=den[:, 12 + qi:13 + qi],
                                                                                             in1=od[:, qi, h, :], op0=ALU.mult, op1=ALU.add))(qi, bB, o0, h),
                             r=[pk(4 + qi // 2), "den", "od"], w=["od"])
                for qi in range(4):
                    ti = b * 4 + qi
                    odf = od[:, qi, :, :].rearrange("p h d -> p (h d)")
                    S.op("pool", (lambda odf: lambda e: e.tensor_tensor(out=r1[:], in0=odf, in1=odf, op=ALU.mult))(odf), r=["od"], w=["r1"])
                    S.op("dve", lambda e: e.tensor_reduce(out=lam[:, 4:8], in_=r1[:].rearrange("p (h d) -> p h d", h=4), axis=AX.X, op=ALU.add), r=["r1"], w=["lam2"])
                    S.op("act", lambda e: e.activation(out=lam[:, 4:8], in_=lam[:, 4:8], func=AF.Ln, scale=1.0 / 128.0, bias=1e-6), r=["lam2"], w=["lam2"])
                    S.op("act", lambda e: e.activation(out=lam[:, 4:8], in_=lam[:, 4:8], func=AF.Exp, scale=-0.5), r=["lam2"], w=["lam2"])
                    S.op("dve", (lambda qi: lambda e: e.tensor_tensor(out=r1[:].rearrange("p (h d) -> p h d", h=4), in0=od[:, qi, :, :],
                                                                      in1=lam[:, 4:8].unsqueeze(2).to_broadcast([128, 4, 128]), op=ALU.mult))(qi), r=["od", "lam2"], w=["r1"])
                    S.op("dve", (lambda qi: lambda e: e.scalar_tensor_tensor(out=ot[:, qi, 512:1024], in0=r1[:], scalar=float(1.0 - LAM_INIT), in1=dnw[:],
                                                                             op0=ALU.mult, op1=ALU.mult))(qi), r=["r1", "dnw"], w=["ot%d" % qi])
                    S.dma("sp", (lambda qi, ti: lambda e: e.dma_start(out=ot[:, qi, 0:512], in_=ogs_d[ti, :, :]))(qi, ti), "ogr", r=["ogs%d" % ti], w=["ot%d" % qi])

                    def trO(e, qi=qi):
                        for k in range(8):
                            ins = e.transpose(PT2[:, k * 128:(k + 1) * 128], ot[:, qi, k * 128:(k + 1) * 128], ident_b)
                        return ins
                    S.op("pe", trO, r=["ot%d" % qi, "Cb"], w=[pk(6)])
                    S.op("act", lambda e: e.activation(out=oT[:], in_=PT2[:, :].rearrange("p (k t) -> p k t", k=8), func=AF.Copy), r=[pk(6)], w=["oT"])
                    for half in range(2):
                        def mmo(e, half=half):
                            for k in range(8):
                                ins = e.matmul(P[6 + half][:], lhsT=oT[:, k, :], rhs=wo[:, k, half * 512:(half + 1) * 512], start=(k == 0), stop=(k == 7))
                            return ins
                        S.op("pe", mmo, r=["oT", "wo"], w=[pk(6 + half)])
                        S.op("act", (lambda half: lambda e: e.activation(out=mixt[:, half * 512:(half + 1) * 512], in_=P[6 + half][:], func=AF.Copy))(half),
                             r=[pk(6 + half)], w=["mixt"])
                    S.dma("sp", (lambda ti: lambda e: e.dma_start(out=mixs_d[ti, :, :], in_=mixt[:]))(ti), "mixw", r=["mixt"], w=["mixs%d" % ti])
                    if debug:
                        S.dma("sp", (lambda ti: lambda e: e.dma_start(out=dbg["mix"][ti, :, :], in_=mixt[:]))(ti), "dbg", r=["mixt"])

        fence()
        S.enabled = "C" in phases
        with contextlib.ExitStack() as es:
            acc = sbt(es, "acc", [128, 16, 1024])
            h1T = sbt(es, "h1T", [128, 8, 2048], BF16)
            gates = sbt(es, "gates", [128, 16, 32])
            m01 = sbt(es, "m01", [128, 2])
            rbb = sbt(es, "rbb", [128, 32])
            bgu = sbt(es, "bgu", [128, 512])
            st = sbt(es, "st", [128, 32])
            S.dma("sp", lambda e: e.dma_start(out=m01[:], in_=m01_d[:, :]), "c3", w=["m01"])
            S.dma("sp", lambda e: e.dma_start(out=rbb[:], in_=rbb_d[:, :]), "c3", w=["rbb"])
            S.dma("sp", lambda e: e.dma_start(out=bgu[:], in_=bgu_d[:, :]), "c3", w=["bgu"])

            def layer_norm(src, srck, dst, dstk, gam, bet, gk, scr, scrk):
                S.op("dve", lambda e: e.tensor_reduce(out=st[:, 0:1], in_=src, axis=AX.X, op=ALU.add), r=[srck], w=["st"])
                S.op("act", lambda e: e.activation(out=scr, in_=src, func=AF.Square), r=[srck], w=[scrk])
                S.op("dve", lambda e: e.tensor_reduce(out=st[:, 1:2], in_=scr, axis=AX.X, op=ALU.add), r=[scrk], w=["st"])
                S.op("dve", lambda e: e.tensor_scalar(out=st[:, 0:2], in0=st[:, 0:2], scalar1=1.0 / 1024.0, scalar2=None, op0=ALU.mult), r=["st"], w=["st"])
                S.op("dve", lambda e: e.tensor_tensor(out=st[:, 2:3], in0=st[:, 0:1], in1=st[:, 0:1], op=ALU.mult), r=["st"], w=["st"])
                S.op("dve", lambda e: e.tensor_tensor(out=st[:, 2:3], in0=st[:, 1:2], in1=st[:, 2:3], op=ALU.subtract), r=["st"], w=["st"])
                S.op("act", lambda e: e.activation(out=st[:, 2:3], in_=st[:, 2:3], func=AF.Ln, bias=1e-5), r=["st"], w=["st"])
                S.op("act", lambda e: e.activation(out=st[:, 2:3], in_=st[:, 2:3], func=AF.Exp, scale=-0.5), r=["st"], w=["st"])
                S.op("dve", lambda e: e.tensor_scalar(out=dst, in0=src, scalar1=st[:, 0:1], scalar2=st[:, 2:3], op0=ALU.subtract, op1=ALU.mult), r=[srck, "st"], w=[dstk])
                S.op("dve", lambda e: e.tensor_tensor(out=dst, in0=dst, in1=gam, op=ALU.mult), r=[dstk, gk], w=[dstk])
                S.op("dve", lambda e: e.tensor_tensor(out=dst, in0=dst, in1=bet, op=ALU.add), r=[dstk, gk], w=[dstk])

            with contextlib.ExitStack() as es2:
                lng = sbt(es2, "lng", [128, 1024])
                lnb = sbt(es2, "lnb", [128, 1024])
                rw = sbt(es2, "rw", [128, 8, 32])
                rwh = sbt(es2, "rwh", [128, 8, 32], BF16)
                rwl = sbt(es2, "rwl", [128, 8, 32], BF16)
                bdn = sbt(es2, "bdn", [128, 1024])
                bdh = sbt(es2, "bdh", [128, 1024], BF16)
                bdl = sbt(es2, "bdl", [128, 1024], BF16)
                h1hl = sbt(es2, "h1hl", [128, 2, 1024], BF16)
                hTl = sbt(es2, "hTl", [128, 8, 128], BF16)
                gpad = sbt(es2, "gpad", [128, 2, 128], BF16)
                xo = sbt(es2, "xo", [128, 1024])
                mA = sbt(es2, "mA", [128, 1024])
                mB = sbt(es2, "mB", [128, 1024])
                h1 = sbt(es2, "h1", [128, 1024])
                scr = sbt(es2, "scr", [128, 1024])
                gT = sbt(es2, "gT", [128, 2, 128], BF16)
                lg = sbt(es2, "lg", [128, 32])
                S.dma("sp", lambda e: e.dma_start(out=lng[:], in_=lnp_d[0, :, :]), "c3", w=["lng"])
                S.dma("sp", lambda e: e.dma_start(out=lnb[:], in_=lnp_d[1, :, :]), "c3", w=["lng"])
                S.dma("sp", lambda e: e.dma_start(out=rw[:], in_=rw_d[:, :, :].rearrange("k p n -> p k n")), "c3", w=["rw"])
                S.dma("sp", lambda e: e.dma_start(out=bdn[:], in_=bdn_d[:, :]), "c3", w=["bdn"])
                S.op("act", lambda e: e.activation(out=rwh[:], in_=rw[:], func=AF.Copy), r=["rw"], w=["rwh"])
                S.op("dve", lambda e: e.tensor_tensor(out=rwl[:], in0=rw[:], in1=rwh[:], op=ALU.subtract), r=["rw", "rwh"], w=["rwl"])
                S.op("act", lambda e: e.activation(out=bdh[:], in_=bdn[:], func=AF.Copy), r=["bdn"], w=["bdh"])
                S.op("dve", lambda e: e.tensor_tensor(out=bdl[:], in0=bdn[:], in1=bdh[:], op=ALU.subtract), r=["bdn", "bdh"], w=["bdl"])
                S.op("dve", lambda e: e.memset(gpad[:].rearrange("p a b -> p (a b)"), 0.0), w=["gpad"])
                for j in range(16):
                    S.dma("sp", (lambda j: lambda e: e.dma_start(out=xo[:], in_=xown_d[j, :, :]))(j), "ldx", w=["xo"])
                    S.dma("sp", (lambda j: lambda e: e.dma_start(out=mA[:], in_=mixs_d[j, :, :]))(j), "ldx", r=["mixs%d" % j], w=["mA"])
                    S.dma("sp", (lambda j: lambda e: e.dma_start(out=mB[:], in_=mixs_d[j + 16, :, :]))(j), "ldx", r=["mixs%d" % (j + 16)], w=["mB"])
                    S.op("dve", lambda e: e.tensor_scalar(out=xo[:], in0=xo[:], scalar1=float(DN_ALPHA), scalar2=None, op0=ALU.mult), r=["xo"], w=["xo"])
                    S.op("dve", lambda e: e.scalar_tensor_tensor(out=xo[:], in0=mA[:], scalar=m01[:, 0:1], in1=xo[:], op0=ALU.mult, op1=ALU.add), r=["mA", "m01", "xo"], w=["xo"])
                    S.op("dve", lambda e: e.scalar_tensor_tensor(out=xo[:], in0=mB[:], scalar=m01[:, 1:2], in1=xo[:], op0=ALU.mult, op1=ALU.add), r=["mB", "m01", "xo"], w=["xo"])
                    layer_norm(xo[:], "xo", h1[:], "h1", lng[:], lnb[:], "lng", scr[:], "scr")
                    if debug:
                        S.dma("sp", (lambda j: lambda e: e.dma_start(out=dbg["h1"][j, :, :], in_=h1[:]))(j), "dbg", r=["h1"])

                    S.op("act", lambda e: e.activation(out=h1hl[:, 0, :], in_=h1[:], func=AF.Copy), r=["h1"], w=["h1hl"])
                    S.op("dve", lambda e: e.tensor_tensor(out=h1hl[:, 1, :], in0=h1[:], in1=h1hl[:, 0, :], op=ALU.subtract), r=["h1", "h1hl"], w=["h1hl"])
                    P0T = P[0][:].bitcast(BF16)
                    P1T = P[1][:].bitcast(BF16)

                    def trh(e):
                        for k in range(8):
                            e.transpose(P0T[:, k * 128:(k + 1) * 128], h1hl[:, 0, k * 128:(k + 1) * 128], ident_b)
                        for k in range(8):
                            ins = e.transpose(P1T[:, k * 128:(k + 1) * 128], h1hl[:, 1, k * 128:(k + 1) * 128], ident_b)
                        return ins
                    S.op("pe", trh, r=["h1hl", "Cb"], w=[pk(0), pk(1)])
                    S.op("act", (lambda j: lambda e: e.activation(out=h1T[:, :, j * 128:(j + 1) * 128], in_=P0T[:, :].rearrange("p (k t) -> p k t", k=8), func=AF.Copy))(j),
                         r=[pk(0)], w=["h1T"])
                    S.op("dve", lambda e: e.tensor_copy(out=hTl[:], in_=P1T[:, :].rearrange("p (k t) -> p k t", k=8)), r=[pk(1)], w=["hTl"])

                    def mmr(e, j=j):
                        n = 0
                        for k in range(8):
                            for (a, bb) in ((h1T[:, k, j * 128:(j + 1) * 128], rwh[:, k, :]), (hTl[:, k, :], rwh[:, k, :]), (h1T[:, k, j * 128:(j + 1) * 128], rwl[:, k, :])):
                                ins = e.matmul(P[2][:, 0:32], lhsT=a, rhs=bb, start=(n == 0), stop=(n == 23))
                                n += 1
                        return ins
                    S.op("pe", mmr, r=["h1T", "hTl", "rwh", "rwl"], w=[pk(2)])
                    S.op("dve", lambda e: e.tensor_tensor(out=lg[:], in0=P[2][:, 0:32], in1=rbb[:], op=ALU.add), r=[pk(2), "rbb"], w=["lg"])
                    S.op("dve", lambda e: e.max(out=st[:, 8:16], in_=lg[:]), r=["lg"], w=["st2"])
                    S.op("dve", lambda e: e.tensor_scalar(out=st[:, 16:17], in0=st[:, 8:9], scalar1=-1.0, scalar2=None, op0=ALU.mult), r=["st2"], w=["st2"])
                    S.op("act", lambda e: e.activation(out=scr[:, 0:32], in_=lg[:], func=AF.Exp, bias=st[:, 16:17]), r=["lg", "st2"], w=["scr"])
                    S.op("dve", lambda e: e.tensor_scalar(out=lg[:], in0=lg[:], scalar1=st[:, 11:12], scalar2=None, op0=ALU.is_ge), r=["lg", "st2"], w=["lg"])
                    S.op("dve", lambda e: e.tensor_tensor(out=lg[:], in0=lg[:], in1=scr[:, 0:32], op=ALU.mult), r=["lg", "scr"], w=["lg"])
                    S.op("dve", lambda e: e.tensor_reduce(out=st[:, 17:18], in_=lg[:], axis=AX.X, op=ALU.add), r=["lg"], w=["st2"])
                    S.op("dve", lambda e: e.reciprocal(out=st[:, 17:18], in_=st[:, 17:18]), r=["st2"], w=["st2"])
                    S.op("dve", (lambda j: lambda e: e.tensor_scalar(out=gates[:, j, :], in0=lg[:], scalar1=st[:, 17:18], scalar2=None, op0=ALU.mult))(j), r=["lg", "st2"], w=["gates"])
                    S.op("act", (lambda j: lambda e: e.activation(out=gpad[:, 0, 0:32], in_=gates[:, j, :], func=AF.Copy))(j), r=["gates"], w=["gpad"])
                    S.op("dve", (lambda j: lambda e: e.tensor_tensor(out=gpad[:, 1, 0:32], in0=gates[:, j, :], in1=gpad[:, 0, 0:32], op=ALU.subtract))(j), r=["gates", "gpad"], w=["gpad"])
                    P3T = P[3][:].bitcast(BF16)

                    def trg(e):
                        e.transpose(P3T[:, 0:128], gpad[:, 0, :], ident_b)
                        return e.transpose(P3T[:, 128:256], gpad[:, 1, :], ident_b)
                    S.op("pe", trg, r=["gpad", "Cb"], w=[pk(3)])
                    S.op("act", lambda e: e.activation(out=gT[:], in_=P3T[:, 0:256].rearrange("p (a t) -> p a t", a=2), func=AF.Copy), r=[pk(3)], w=["gT"])
                    for half in range(2):
                        def mmbd(e, half=half):
                            hs_ = slice(half * 512, (half + 1) * 512)
                            e.matmul(P[4 + half][:], lhsT=gT[:, 0, :], rhs=bdh[:, hs_], start=True, stop=False)
                            e.matmul(P[4 + half][:], lhsT=gT[:, 1, :], rhs=bdh[:, hs_], start=False, stop=False)
                            return e.matmul(P[4 + half][:], lhsT=gT[:, 0, :], rhs=bdl[:, hs_], start=False, stop=True)
                        S.op("pe", mmbd, r=["gT", "bdh", "bdl"], w=[pk(4 + half)])
                        S.op("dve", (lambda half, j: lambda e: e.scalar_tensor_tensor(out=acc[:, j, half * 512:(half + 1) * 512], in0=h1[:, half * 512:(half + 1) * 512],
                                                                                        scalar=float(DN_ALPHA), in1=P[4 + half][:], op0=ALU.mult, op1=ALU.add))(half, j),
                             r=["h1", pk(4 + half)], w=["acc%d" % j])

            fence()
            S.enabled = "D" in phases
            with contextlib.ExitStack() as es2:
                wg = [sbt(es2, "ewg%d" % i, [128, 8, 1024], BF16) for i in range(2)]
                wu = [sbt(es2, "ewu%d" % i, [128, 8, 1024], BF16) for i in range(2)]
                wd = sbt(es2, "wd", [128, 8, 1024], BF16)
                aT = [sbt(es2, "aT%d" % i, [128, 8, 512], BF16) for i in range(2)]
                gsb = sbt(es2, "gsb", [128, 512])
                usb = sbt(es2, "usb", [128, 512], BF16)
                sgs = sbt(es2, "sgs", [128, 512], BF16)
                ucl = sbt(es2, "ucl", [128, 512], BF16)
                it = 0
                def load_gu(ex):
                    wb_ = ex % 2
                    for k in range(8):
                        S.dma("pool", (lambda ex, k, wb_: lambda e: e.dma_start(out=wg[wb_][:, k, :], in_=wgu_d[ex, k, :, 0:1024]))(ex, k, wb_), "wg%d" % wb_, w=["wg%d" % wb_])
                        S.dma("pool", (lambda ex, k, wb_: lambda e: e.dma_start(out=wu[wb_][:, k, :], in_=wgu_d[ex, k, :, 1024:2048]))(ex, k, wb_), "wu%d" % wb_, w=["wu%d" % wb_])

                def load_d(ex):
                    for k in range(8):
                        S.dma("pool", (lambda ex, k: lambda e: e.dma_start(out=wd[:, k, :], in_=wdn_d[ex, k, :, :]))(ex, k), "wd", w=["wd"])
                load_gu(0)
                load_d(0)
                for ex in range(32):
                    wb_ = ex % 2
                    if ex + 1 < 32:
                        load_gu(ex + 1)
                    for tb in range(4):
                        ab = it % 2
                        it += 1
                        for fc in range(8):
                            pg_, pu_ = (0, 1) if fc % 2 == 0 else (2, 3)

                            def mmg(e, fc=fc, tb=tb, wb_=wb_, pg_=pg_, pu_=pu_):
                                for k in range(8):
                                    e.matmul(P[pg_][:], lhsT=wg[wb_][:, k, fc * 128:(fc + 1) * 128], rhs=h1T[:, k, tb * 512:(tb + 1) * 512], start=(k == 0), stop=(k == 7))
                                for k in range(8):
                                    ins = e.matmul(P[pu_][:], lhsT=wu[wb_][:, k, fc * 128:(fc + 1) * 128], rhs=h1T[:, k, tb * 512:(tb + 1) * 512], start=(k == 0), stop=(k == 7))
                                return ins
                            S.op("pe", mmg, r=["wg%d" % wb_, "wu%d" % wb_, "h1T"], w=[pk(pg_), pk(pu_)])
                            bgc = bgu[:, ex * 16 + fc:ex * 16 + fc + 1]
                            buc = bgu[:, ex * 16 + 8 + fc:ex * 16 + 8 + fc + 1]
                            S.op("dve", (lambda pg_, bgc: lambda e: e.tensor_scalar(out=gsb[:], in0=P[pg_][:], scalar1=bgc, scalar2=7.0, op0=ALU.add, op1=ALU.min))(pg_, bgc),
                                 r=[pk(pg_), "bgu"], w=["gsb"])
                            S.op("act", (lambda pu_, buc: lambda e: e.activation(out=usb[:], in_=P[pu_][:], func=AF.Identity, bias=buc))(pu_, buc), r=[pk(pu_), "bgu"], w=["usb"])
                            S.op("act", lambda e: e.activation(out=sgs[:], in_=gsb[:], func=AF.Sigmoid, scale=1.702), r=["gsb"], w=["sgs"])
                            S.op("dve", lambda e: e.tensor_scalar(out=ucl[:], in0=usb[:], scalar1=7.0, scalar2=-7.0, op0=ALU.min, op1=ALU.max), r=["usb"], w=["ucl"])
                            S.op("dve", lambda e: e.tensor_tensor(out=gsb[:], in0=gsb[:], in1=sgs[:], op=ALU.mult), r=["gsb", "sgs"], w=["gsb"])
                            S.op("dve", (lambda ab, fc: lambda e: e.scalar_tensor_tensor(out=aT[ab][:, fc, :], in0=ucl[:], scalar=1.0, in1=gsb[:], op0=ALU.add, op1=ALU.mult))(ab, fc),
                                 r=["ucl", "gsb"], w=["aT%d" % ab])
                        for tt in range(4):
                            j = tb * 4 + tt
                            for half in range(2):
                                pb = 4 + half + 2 * (tt % 2)

                                def mmd(e, ab=ab, tt=tt, half=half, pb=pb):
                                    for fc in range(8):
                                        ins = e.matmul(P[pb][:], lhsT=aT[ab][:, fc, tt * 128:(tt + 1) * 128], rhs=wd[:, fc, half * 512:(half + 1) * 512], start=(fc == 0), stop=(fc == 7))
                                    return ins
                                S.op("pe", mmd, r=["aT%d" % ab, "wd"], w=[pk(pb)])
                                S.op("dve", (lambda j, half, pb, ex: lambda e: e.scalar_tensor_tensor(out=acc[:, j, half * 512:(half + 1) * 512], in0=P[pb][:],
                                                                                                       scalar=gates[:, j, ex:ex + 1], in1=acc[:, j, half * 512:(half + 1) * 512],
                                                                                                       op0=ALU.mult, op1=ALU.add))(j, half, pb, ex),
                                     r=[pk(pb), "gates", "acc%d" % j], w=["acc%d" % j])
                    if ex + 1 < 32:
                        load_d(ex + 1)

            fence()
            S.enabled = "E" in phases
            with contextlib.ExitStack() as es2:
                l2g = sbt(es2, "l2g", [128, 1024])
                l2b = sbt(es2, "l2b", [128, 1024])
                l3g = sbt(es2, "l3g", [128, 1024])
                l3b = sbt(es2, "l3b", [128, 1024])
                pgb = sbt(es2, "pgb", [128, 1024])
                wpg = sbt(es2, "wpg", [128, 8, 1024], BF16)
                plw = sbt(es2, "plw", [128, 2, 1024], BF16)
                ptn = sbt(es2, "ptn", [128, 2, 2048], BF16)
                h2 = sbt(es2, "h2", [128, 1024])
                h2b = sbt(es2, "h2b", [128, 1024], BF16)
                h2T = sbt(es2, "h2T", [128, 8, 128], BF16)
                scrE = sbt(es2, "scr2", [128, 1024])
                gt = sbt(es2, "gt", [128, 1024])
                y3 = sbt(es2, "y3", [128, 1024])
                PT3 = P[0][:].bitcast(BF16)
                S.dma("sp", lambda e: e.dma_start(out=l2g[:], in_=lnp_d[2, :, :]), "c4", w=["l2"])
                S.dma("sp", lambda e: e.dma_start(out=l2b[:], in_=lnp_d[3, :, :]), "c4", w=["l2"])
                S.dma("sp", lambda e: e.dma_start(out=l3g[:], in_=lnp_d[4, :, :]), "c4", w=["l3"])
                S.dma("sp", lambda e: e.dma_start(out=l3b[:], in_=lnp_d[5, :, :]), "c4", w=["l3"])
                S.dma("sp", lambda e: e.dma_start(out=pgb[:], in_=pgb_d[:, :]), "c4", w=["pgb"])
                for k in range(8):
                    S.dma("pool", (lambda k: lambda e: e.dma_start(out=wpg[:, k, :], in_=wpg_d[k, :, :]))(k), "w4", w=["wpg"])
                for k in range(2):
                    S.dma("pool", (lambda k: lambda e: e.dma_start(out=plw[:, k, :], in_=plew_d[k, :, :]))(k), "w4", w=["plw"])
                    S.dma("pool", (lambda k: lambda e: e.dma_start(out=ptn[:, k, :], in_=ptown_d[k, :, :]))(k), "w4", w=["ptn"])
                for j in range(16):
                    if debug:
                        S.dma("sp", (lambda j: lambda e: e.dma_start(out=dbg["acc"][j, :, :], in_=acc[:, j, :]))(j), "dbg", r=["acc%d" % j])
                    layer_norm(acc[:, j, :], "acc%d" % j, h2[:], "h2", l2g[:], l2b[:], "l2", scrE[:], "scr2")
                    S.op("act", lambda e: e.activation(out=h2b[:], in_=h2[:], func=AF.Copy), r=["h2"], w=["h2b"])

                    def trh2(e):
                        for k in range(8):
                            ins = e.transpose(PT3[:, k * 128:(k + 1) * 128], h2b[:, k * 128:(k + 1) * 128], ident_b)
                        return ins
                    S.op("pe", trh2, r=["h2b", "Cb"], w=[pk(0)])
                    S.op("act", lambda e: e.activation(out=h2T[:], in_=PT3[:, :].rearrange("p (k t) -> p k t", k=8), func=AF.Copy), r=[pk(0)], w=["h2T"])
                    for half in range(2):
                        hs_ = slice(half * 512, (half + 1) * 512)

                        def mmgt(e, hs_=hs_, half=half):
                            for k in range(8):
                                ins = e.matmul(P[2 + half][:], lhsT=h2T[:, k, :], rhs=wpg[:, k, hs_], start=(k == 0), stop=(k == 7))
                            return ins
                        S.op("pe", mmgt, r=["h2T", "wpg"], w=[pk(2 + half)])

                        def mmpl(e, hs_=hs_, half=half, j=j):
                            for k in range(2):
                                ins = e.matmul(P[4 + half][:], lhsT=ptn[:, k, j * 128:(j + 1) * 128], rhs=plw[:, k, hs_], start=(k == 0), stop=(k == 1))
                            return ins
                        S.op("pe", mmpl, r=["ptn", "plw"], w=[pk(4 + half)])
                        S.op("dve", (lambda hs_, half: lambda e: e.tensor_tensor(out=gt[:, hs_], in0=P[2 + half][:], in1=pgb[:, hs_], op=ALU.add))(hs_, half),
                             r=[pk(2 + half), "pgb"], w=["gt"])
                        S.op("act", (lambda hs_: lambda e: e.activation(out=gt[:, hs_], in_=gt[:, hs_], func=AF.Sigmoid))(hs_), r=["gt"], w=["gt"])
                        S.op("dve", (lambda hs_, half: lambda e: e.tensor_tensor(out=gt[:, hs_], in0=P[4 + half][:], in1=gt[:, hs_], op=ALU.mult))(hs_, half),
                             r=[pk(4 + half), "gt"], w=["gt"])
                    S.op("dve", lambda e: e.scalar_tensor_tensor(out=y3[:], in0=h2[:], scalar=float(DN_ALPHA), in1=gt[:], op0=ALU.mult, op1=ALU.add), r=["h2", "gt"], w=["y3"])
                    layer_norm(y3[:], "y3", y3[:], "y3", l3g[:], l3b[:], "l3", scrE[:], "scr2")
                    S.dma("sp", (lambda j: lambda e: e.dma_start(out=out_d[j, :, :], in_=y3[:]))(j), "outw", r=["y3"])

        S.emit(final_wait_slots=[x for x in ["outw", "dbg", "ogw", "mixw"] if x in S.dma_slots])
    return nc


def _consts():
    c = np.zeros((128, NCONST), np.float32)
    i = np.arange(128)
    same = (i[:, None] // 64) == (i[None, :] // 64)
    c[:, 0:128] = np.eye(128)
    c[:, 128:256] = ((i[:, None] <= i[None, :]) & same)
    c[:, 256:384] = same
    c[:, 384:512] = np.where((i[None, :] < i[:, None]) & same, 0.0, BIG)
    c[:, 512:640] = np.where((i[:, None] <= i[None, :]) & same, 0.0, BIG)
    c[:, 640:768] = (i[None, :] >= i[:, None])
    c[:, 768:896] = 1.0
    pm = np.zeros((128, 128), np.float32)
    for m in range(128):
        d = m % 64
        if d < 8:
            pm[m + 8, m] = -1.0
        elif d < 16:
            pm[m - 8, m] = 1.0
    c[:, 896:1024] = pm
    c[:, 1024] = (i < 64)
    c[:, 1025] = (i >= 64)
    inv_freq = (500000.0 ** (-np.arange(0, 16, 2, dtype=np.float32) / np.float32(16))).astype(np.float32)
    d = i % 64
    c[:, 1026] = np.where(d < 16, inv_freq[d % 8], 0.0)
    c[:, 1027] = (i < 64)
    c[:, 1028] = (i >= 64)
    return c


def _bc(v, n=128):
    return np.ascontiguousarray(np.broadcast_to(np.asarray(v, np.float32).reshape(1, -1), (n, np.asarray(v).size)))


def _prep(inp):
    f = lambda a: np.ascontiguousarray(np.asarray(a, np.float32))
    x = f(inp["x"])
    w_in = f(inp["w_in"][0])
    O = [0, 512, 1024, 1536, 2048, 2052, 2056, 2568, 3080, 3592]
    gqkv = w_in[:, 0:1536]
    gz = w_in[:, O[3]:O[4]]
    gba = w_in[:, O[4]:O[6]]
    dq = w_in[:, O[6]:O[7]]
    dk = w_in[:, O[7]:O[8]]
    dv = w_in[:, O[8]:O[9]]
    wfm = np.concatenate([gqkv, dq, dk], 1).reshape(8, 128, 2560)
    wtm = np.concatenate([gz, dv, gba], 1).reshape(8, 128, 1032)
    convw = f(inp["conv_w"][0]).T.reshape(12, 128, 4).transpose(1, 0, 2).reshape(128, 48)
    hs = np.concatenate([_bc(inp["a_log"][0]), _bc(inp["dt_bias"][0])], 1)
    gnw4 = _bc(np.tile(f(inp["gdn_norm_w"][0]), 4))
    dnw4 = _bc(np.tile(f(inp["diff_norm_w"][0]), 4))
    lamv = np.concatenate([_bc(inp["lam_q1"][0]), _bc(inp["lam_k1"][0]), _bc(inp["lam_q2"][0]), _bc(inp["lam_k2"][0])], 1)
    shared = dict(
        wfm=np.ascontiguousarray(wfm), wtm=np.ascontiguousarray(wtm), convw=np.ascontiguousarray(convw), hs=np.ascontiguousarray(hs),
        gnw4=gnw4, dnw4=dnw4, lamv=np.ascontiguousarray(lamv),
        wout=f(inp["w_out"][0]).reshape(8, 128, 1024), consts=_consts(),
        msk4=np.ascontiguousarray(np.concatenate([np.tile(_consts()[:, 384:512], (1, 4)), np.tile(_consts()[:, 512:640], (1, 4))], 1)),
        lnp=np.ascontiguousarray(np.stack([_bc(inp[k][0]) for k in ("ln1_g", "ln1_b", "ln2_g", "ln2_b", "ln3_g", "ln3_b")], 0)),
        rw=f(inp["router_w"][0]).reshape(8, 128, 32), rbb=_bc(inp["router_b"][0]),
        wgu=f(inp["w_gu"][0]).reshape(32, 8, 128, 2048),
        bgu=np.ascontiguousarray(f(inp["b_gu"][0]).reshape(32, 16, 128).transpose(2, 0, 1).reshape(128, 512)),
        wdn=f(inp["w_down"][0]).reshape(32, 8, 128, 1024), bdn=np.concatenate([f(inp["b_down"][0]), np.zeros((96, 1024), np.float32)], 0),
        wpg=f(inp["ple_gate_w"][0]).reshape(8, 128, 1024), pgb=_bc(inp["ple_gate_b"][0]),
        plew=f(inp["ple_w"][0]).reshape(2, 128, 1024),
    )
    pos = np.asarray(inp["positions"], np.int32)
    p = f(inp["p"][0])
    maps = []
    for c in range(8):
        b, half = c // 2, c % 2
        m = dict(shared)
        m["xT"] = np.ascontiguousarray(x[b].T).reshape(8, 128, SEQ)
        m["posb"] = np.ascontiguousarray(np.broadcast_to(pos[b][None, :], (128, SEQ)))
        m01 = np.zeros((128, 2), np.float32)
        m01[:, half] = 1.0
        m["m01"] = m01
        m["xown"] = np.ascontiguousarray(x[b, half * 2048:(half + 1) * 2048]).reshape(16, 128, 1024)
        m["ptown"] = np.ascontiguousarray(p[b, half * 2048:(half + 1) * 2048].T).reshape(2, 128, 2048)
        maps.append(m)
    return maps


def _run(inputs, debug=False, phases="12CDE", nblk=NBLK):
    maps = _prep(inputs)
    if "D" not in phases:
        for m in maps:
            m.pop("wgu")
            m.pop("wdn")
    nc = build_program(debug=debug, phases=phases, nblk=nblk)
    res = run_bass_kernel_spmd(nc, maps, core_ids=list(range(8)))
    return res.results


def kernel(**inputs):
    results = _run(inputs, debug=False)
    out = np.zeros((4, SEQ, 1024), np.float32)
    for c in range(8):
        b, half = c // 2, c % 2
        out[b, half * 2048:(half + 1) * 2048] = np.asarray(results[c]["out"], np.float32).reshape(2048, 1024)
    return out
```
